# Optimizing a Trainium2 kernel written in Bass

```python
import math
import jax, jax.numpy as jnp
from jax import lax
import numpy as np

D_MODEL = 1024
BATCH = 2
SEQ = 8192
DEPTH = 2

GRID_W = 64
CTX_LEN = 256
EPS = 1e-6

CONV_WIDTH = 512
CONV_K = 3
DA_HEADS = 4
DA_HEAD_DIM = 64
DA_V_DIM = 2 * DA_HEAD_DIM
DA_WIDTH = DA_HEADS * DA_V_DIM
EVEN_IN = 3 * CONV_WIDTH + 3 * DA_WIDTH
EVEN_MIX = CONV_WIDTH + DA_WIDTH
Q_BLOCK = 128
ROPE_BASE = 10000.0

LRU_WIDTH = 1024
LRU_BLOCKS = 8
LRU_BLOCK = LRU_WIDTH // LRU_BLOCKS
LRU_CONV_K = 4
LRU_C = 8.0

N_GROUPS = 4
EXPERTS_PER_GROUP = 4
N_EXPERTS = N_GROUPS * EXPERTS_PER_GROUP
D_EXPERT = 512
TOP_K = 2

N_EVEN = (DEPTH + 1) // 2
N_ODD = DEPTH // 2

kernel_name = "hybrid_conv_diffattn_rglru_hmoe_dit"


def rms_norm(x, g):
    xf = x.astype(jnp.float32)
    y = xf * lax.rsqrt(jnp.mean(xf * xf, axis=-1, keepdims=True) + EPS)
    return (y * g.astype(jnp.float32)).astype(x.dtype)


def modulate(h, shift, scale):
    return h * (1.0 + scale) + shift


def axial_rope(n_rows):
    rows = jnp.repeat(jnp.arange(n_rows), GRID_W).astype(jnp.float32)
    cols = jnp.tile(jnp.arange(GRID_W), n_rows).astype(jnp.float32)
    n_freq = DA_HEAD_DIM // 4
    inv = ROPE_BASE ** (-jnp.arange(n_freq, dtype=jnp.float32) / n_freq)
    ang = jnp.concatenate([rows[:, None] * inv, cols[:, None] * inv], axis=-1)
    return jnp.cos(ang), jnp.sin(ang)


def apply_rope(t, cos, sin):
    half = t.shape[-1] // 2
    tf = t.astype(jnp.float32)
    t1, t2 = tf[..., :half], tf[..., half:]
    cs = cos[None, :, None, None, :]
    sn = sin[None, :, None, None, :]
    return jnp.concatenate([t1 * cs - t2 * sn, t2 * cs + t1 * sn], axis=-1).astype(t.dtype)


def depthwise_conv(u, w, b, pad_left, pad_right):
    n = u.shape[1]
    up = jnp.pad(u, ((0, 0), (pad_left, pad_right), (0, 0)))
    y = b
    for k in range(w.shape[0]):
        y = y + up[:, k:k + n] * w[k]
    return y


def diff_attn(q, k, v, lam):
    s = jnp.einsum('bhmqd,bhmkd->bhmqk', q, k, preferred_element_type=jnp.float32) * (DA_HEAD_DIM ** -0.5)
    p = jax.nn.softmax(s, axis=-1)
    w = p[:, :, 0] - lam * p[:, :, 1]
    return jnp.einsum('bhqk,bhkv->bhqv', w, v.astype(jnp.float32)).astype(v.dtype)


def even_mixer(h_lat, h_ctx, w_in, conv_w, conv_b, q_norm, k_norm, lq1, lk1, lq2, lk2,
               sub_norm, w_out, lam_init, cos, sin, with_ctx):
    bsz, n = h_lat.shape[:2]
    c_len = h_ctx.shape[1]
    kv_col = 3 * CONV_WIDTH + DA_WIDTH

    def to_heads_qk(t, g, m):
        return rms_norm(t.reshape(bsz, m, DA_HEADS, 2, DA_HEAD_DIM), g)

    def to_heads_v(t, m):
        return t.reshape(bsz, m, DA_HEADS, DA_V_DIM).transpose(0, 2, 1, 3)

    lam = (jnp.exp(jnp.sum(lq1.astype(jnp.float32) * lk1.astype(jnp.float32)))
           - jnp.exp(jnp.sum(lq2.astype(jnp.float32) * lk2.astype(jnp.float32))) + lam_init)

    z = h_lat @ w_in
    cb, cc, cx, q, k, v = jnp.split(z, 6, axis=-1)
    q_l = apply_rope(to_heads_qk(q, q_norm, n), cos, sin).transpose(0, 2, 3, 1, 4)
    k_l = apply_rope(to_heads_qk(k, k_norm, n), cos, sin).transpose(0, 2, 3, 1, 4)
    v_l = to_heads_v(v, n)

    if with_ctx:
        zc = h_ctx @ w_in
        ccb, ccc, ccx, qc, kc, vc = jnp.split(zc, 6, axis=-1)
    else:
        kc, vc = jnp.split(h_ctx @ w_in[:, kv_col:], 2, axis=-1)
    k_c = to_heads_qk(kc, k_norm, c_len).transpose(0, 2, 3, 1, 4)
    v_c = to_heads_v(vc, c_len)

    out_a = cb * depthwise_conv(cc * cx, conv_w, conv_b, 1, 1)

    k_all = jnp.concatenate([k_l, k_c], axis=3)
    v_all = jnp.concatenate([v_l, v_c], axis=2)
    nb = n // Q_BLOCK
    qb = q_l.reshape(bsz, DA_HEADS, 2, nb, Q_BLOCK, DA_HEAD_DIM).transpose(3, 0, 1, 2, 4, 5)
    o = lax.map(lambda q_blk: diff_attn(q_blk, k_all, v_all, lam), qb)
    o = o.transpose(1, 0, 3, 2, 4).reshape(bsz, n, DA_HEADS, DA_V_DIM)
    out_b = (rms_norm(o, sub_norm) * (1.0 - lam_init)).reshape(bsz, n, DA_WIDTH)
    y_lat = jnp.concatenate([out_a, out_b], axis=-1) @ w_out

    if not with_ctx:
        return y_lat, None
    out_ac = ccb * depthwise_conv(ccc * ccx, conv_w, conv_b, 1, 1)
    q_c = to_heads_qk(qc, q_norm, c_len).transpose(0, 2, 3, 1, 4)
    oc = diff_attn(q_c, k_c, v_c, lam).transpose(0, 2, 1, 3)
    out_bc = (rms_norm(oc, sub_norm) * (1.0 - lam_init)).reshape(bsz, c_len, DA_WIDTH)
    y_ctx = jnp.concatenate([out_ac, out_bc], axis=-1) @ w_out
    return y_lat, y_ctx


def block_diag(u, w, b):
    ub = u.reshape(u.shape[:-1] + (LRU_BLOCKS, LRU_BLOCK))
    return jnp.einsum('bshi,hij->bshj', ub, w).reshape(u.shape) + b


def scan_from(a, b, h0, reverse):
    def combine(e1, e2):
        a1, b1 = e1
        a2, b2 = e2
        return a1 * a2, a2 * b1 + b2
    a_cum, b_cum = lax.associative_scan(combine, (a, b), reverse=reverse, axis=1)
    return a_cum * h0[:, None, :] + b_cum


def rglru_direction(u_lat, u_ctx, conv_w, conv_b, w_a, b_a, w_x, b_x, lam, reverse):
    pads = (0, LRU_CONV_K - 1) if reverse else (LRU_CONV_K - 1, 0)

    def coeffs(u):
        uc = depthwise_conv(u, conv_w, conv_b, pads[0], pads[1]).astype(jnp.float32)
        r = jax.nn.sigmoid(block_diag(uc, w_a.astype(jnp.float32), b_a.astype(jnp.float32)))
        i = jax.nn.sigmoid(block_diag(uc, w_x.astype(jnp.float32), b_x.astype(jnp.float32)))
        log_a = -LRU_C * r * jax.nn.softplus(-lam.astype(jnp.float32))
        return jnp.exp(log_a), jnp.sqrt(-jnp.expm1(2.0 * log_a)) * (i * uc)

    a_c, b_c = coeffs(u_ctx)
    h_ctx = scan_from(a_c, b_c, jnp.zeros((u_ctx.shape[0], LRU_WIDTH), jnp.float32), reverse)
    h0 = h_ctx[:, 0] if reverse else h_ctx[:, -1]
    a_l, b_l = coeffs(u_lat)
    h_lat = scan_from(a_l, b_l, h0, reverse)
    return h_lat, h_ctx


def odd_mixer(h_lat, h_ctx, w_in, conv_w, conv_b, w_a, b_a, w_x, b_x, lam, w_out, with_ctx):
    z = h_lat @ w_in
    y_l = jax.nn.gelu(z[..., :LRU_WIDTH], approximate=True)
    u_l = z[..., LRU_WIDTH:]
    if with_ctx:
        zc = h_ctx @ w_in
        y_c = jax.nn.gelu(zc[..., :LRU_WIDTH], approximate=True)
        u_c = zc[..., LRU_WIDTH:]
    else:
        u_c = h_ctx @ w_in[:, LRU_WIDTH:]
    hf_l, hf_c = rglru_direction(u_l, u_c, conv_w[0], conv_b[0], w_a[0], b_a[0], w_x[0], b_x[0], lam[0], False)
    hb_l, hb_c = rglru_direction(u_l, u_c, conv_w[1], conv_b[1], w_a[1], b_a[1], w_x[1], b_x[1], lam[1], True)
    y_lat = (y_l * (hf_l + hb_l).astype(y_l.dtype)) @ w_out
    if not with_ctx:
        return y_lat, None
    y_ctx = (y_c * (hf_c + hb_c).astype(y_c.dtype)) @ w_out
    return y_lat, y_ctx


def hier_moe(h, w_grp, b_grp, w_rt, b_rt, w_gate, w_up, w_down):
    f32 = jnp.float32
    g_logit = jnp.einsum('bsd,dg->bsg', h, w_grp, preferred_element_type=f32) + b_grp.astype(f32)
    p_grp = jax.nn.softmax(g_logit, axis=-1)
    g_idx = jnp.argmax(g_logit, axis=-1)
    p_sel = jnp.max(p_grp, axis=-1, keepdims=True)
    e_logit = jnp.einsum('bsd,dge->bsge', h, w_rt, preferred_element_type=f32) + b_rt.astype(f32)
    e_sel = jnp.einsum('bsge,bsg->bse', e_logit, jax.nn.one_hot(g_idx, N_GROUPS, dtype=f32))
    top_v, top_i = lax.top_k(e_sel, TOP_K)
    w_sel = jax.nn.softmax(top_v, axis=-1) * p_sel
    eid = g_idx[..., None] * EXPERTS_PER_GROUP + top_i
    gate = jnp.sum(jax.nn.one_hot(eid, N_EXPERTS, dtype=f32) * w_sel[..., None], axis=-2)
    hg = jnp.einsum('bsd,edf->bsef', h, w_gate)
    hu = jnp.einsum('bsd,edf->bsef', h, w_up)
    act = jax.nn.silu(hg) * hu * gate[..., None].astype(h.dtype)
    return jnp.einsum('bsef,efd->bsd', act, w_down)


def setup_inputs(seed: int = 0) -> dict:
    key = jax.random.key(seed)
    ks = iter(jax.random.split(key, 64))
    D = D_MODEL

    def nrm(shape, scale):
        return jax.random.normal(next(ks), shape, jnp.float32) * scale

    def gain(shape):
        return 1.0 + nrm(shape, 0.02)

    u = jax.random.uniform(next(ks), (N_ODD, 2, LRU_WIDTH), jnp.float32, minval=0.9, maxval=0.999)
    s = u ** (1.0 / LRU_C)
    od_lam = jnp.log(s) - jnp.log1p(-s)
    return {
        "x": nrm((BATCH, SEQ, D), 1.0),
        "c": nrm((BATCH, D), 1.0),
        "ctx": nrm((BATCH, CTX_LEN, D), 1.0),
        "c_ctx": nrm((D,), 1.0),
        "ada_w": nrm((DEPTH, D, 6 * D), 0.5 * D ** -0.5),
        "ada_b": nrm((DEPTH, 6 * D), 0.02),
        "norm_mix": gain((DEPTH, D)),
        "norm_ffn": gain((DEPTH, D)),
        "ev_w_in": nrm((N_EVEN, D, EVEN_IN), D ** -0.5),
        "ev_conv_w": nrm((N_EVEN, CONV_K, CONV_WIDTH), CONV_K ** -0.5),
        "ev_conv_b": nrm((N_EVEN, CONV_WIDTH), 0.02),
        "ev_q_norm": gain((N_EVEN, DA_HEAD_DIM)),
        "ev_k_norm": gain((N_EVEN, DA_HEAD_DIM)),
        "ev_lam_q1": nrm((N_EVEN, DA_HEAD_DIM), 0.1),
        "ev_lam_k1": nrm((N_EVEN, DA_HEAD_DIM), 0.1),
        "ev_lam_q2": nrm((N_EVEN, DA_HEAD_DIM), 0.1),
        "ev_lam_k2": nrm((N_EVEN, DA_HEAD_DIM), 0.1),
        "ev_sub_norm": gain((N_EVEN, DA_V_DIM)),
        "ev_w_out": nrm((N_EVEN, EVEN_MIX, D), EVEN_MIX ** -0.5),
        "od_w_in": nrm((N_ODD, D, 2 * LRU_WIDTH), D ** -0.5),
        "od_conv_w": nrm((N_ODD, 2, LRU_CONV_K, LRU_WIDTH), LRU_CONV_K ** -0.5),
        "od_conv_b": nrm((N_ODD, 2, LRU_WIDTH), 0.02),
        "od_w_a": nrm((N_ODD, 2, LRU_BLOCKS, LRU_BLOCK, LRU_BLOCK), LRU_BLOCK ** -0.5),
        "od_b_a": nrm((N_ODD, 2, LRU_WIDTH), 0.02),
        "od_w_x": nrm((N_ODD, 2, LRU_BLOCKS, LRU_BLOCK, LRU_BLOCK), LRU_BLOCK ** -0.5),
        "od_b_x": nrm((N_ODD, 2, LRU_WIDTH), 0.02),
        "od_lam": od_lam,
        "od_w_out": nrm((N_ODD, LRU_WIDTH, D), LRU_WIDTH ** -0.5),
        "moe_w_grp": nrm((DEPTH, D, N_GROUPS), D ** -0.5),
        "moe_b_grp": nrm((DEPTH, N_GROUPS), 0.01),
        "moe_w_rt": nrm((DEPTH, D, N_GROUPS, EXPERTS_PER_GROUP), D ** -0.5),
        "moe_b_rt": nrm((DEPTH, N_GROUPS, EXPERTS_PER_GROUP), 0.01),
        "moe_w_gate": nrm((DEPTH, N_EXPERTS, D, D_EXPERT), D ** -0.5),
        "moe_w_up": nrm((DEPTH, N_EXPERTS, D, D_EXPERT), D ** -0.5),
        "moe_w_down": nrm((DEPTH, N_EXPERTS, D_EXPERT, D), D_EXPERT ** -0.5),
    }


def reference(x, c, ctx, c_ctx, ada_w, ada_b, norm_mix, norm_ffn,
              ev_w_in, ev_conv_w, ev_conv_b, ev_q_norm, ev_k_norm,
              ev_lam_q1, ev_lam_k1, ev_lam_q2, ev_lam_k2, ev_sub_norm, ev_w_out,
              od_w_in, od_conv_w, od_conv_b, od_w_a, od_b_a, od_w_x, od_b_x, od_lam, od_w_out,
              moe_w_grp, moe_b_grp, moe_w_rt, moe_b_rt, moe_w_gate, moe_w_up, moe_w_down):
    n = x.shape[1]
    ROWS = n // GRID_W
    cos, sin = axial_rope(ROWS)
    for l in range(DEPTH):
        with_ctx = l < DEPTH - 1
        mod = jax.nn.silu(c) @ ada_w[l] + ada_b[l]
        mod_c = jax.nn.silu(c_ctx) @ ada_w[l] + ada_b[l]
        sh1, sc1, g1, sh2, sc2, g2 = jnp.split(mod[:, None, :], 6, axis=-1)
        csh1, csc1, cg1, csh2, csc2, cg2 = jnp.split(mod_c, 6, axis=-1)
        h = modulate(rms_norm(x, norm_mix[l]), sh1, sc1)
        hc = modulate(rms_norm(ctx, norm_mix[l]), csh1, csc1)
        j = l // 2
        if l % 2 == 0:
            lam_init = 0.8 - 0.6 * math.exp(-0.3 * l)
            y, yc = even_mixer(h, hc, ev_w_in[j], ev_conv_w[j], ev_conv_b[j], ev_q_norm[j], ev_k_norm[j],
                               ev_lam_q1[j], ev_lam_k1[j], ev_lam_q2[j], ev_lam_k2[j], ev_sub_norm[j],
                               ev_w_out[j], lam_init, cos, sin, with_ctx)
        else:
            y, yc = odd_mixer(h, hc, od_w_in[j], od_conv_w[j], od_conv_b[j], od_w_a[j], od_b_a[j],
                              od_w_x[j], od_b_x[j], od_lam[j], od_w_out[j], with_ctx)
        x = x + g1 * y
        hf = modulate(rms_norm(x, norm_ffn[l]), sh2, sc2)
        x = x + g2 * hier_moe(hf, moe_w_grp[l], moe_b_grp[l], moe_w_rt[l], moe_b_rt[l],
                              moe_w_gate[l], moe_w_up[l], moe_w_down[l])
        if with_ctx:
            ctx = ctx + cg1 * yc
            hfc = modulate(rms_norm(ctx, norm_ffn[l]), csh2, csc2)
            ctx = ctx + cg2 * hier_moe(hfc, moe_w_grp[l], moe_b_grp[l], moe_w_rt[l], moe_b_rt[l],
                                       moe_w_gate[l], moe_w_up[l], moe_w_down[l])
    return x
```

```python
import math
import numpy as np
import ml_dtypes
from contextlib import ExitStack
import concourse.bass as bass
import concourse.mybir as mybir
from concourse.bass_utils import run_bass_kernel_spmd

F32 = mybir.dt.float32
BF16 = mybir.dt.bfloat16
AF = mybir.ActivationFunctionType
ALU = mybir.AluOpType
AX = mybir.AxisListType
NPBF = ml_dtypes.bfloat16

NDSEM = 8
EPS = 1e-6
NCORE = 8
TOK = 2048
CTXL = 256
SEQ = 8192
D = 1024
NKT = (SEQ + CTXL) // 128


class Buf:
    __slots__ = ("lw", "rd")

    def __init__(self):
        self.lw = None
        self.rd = []


class T:
    __slots__ = ("ap", "bufs")

    def __init__(self, ap, bufs):
        self.ap = ap
        self.bufs = bufs

    def __getitem__(self, key):
        return T(self.ap[key], self.bufs)

    def re(self, s, **kw):
        return T(self.ap.rearrange(s, **kw), self.bufs)

    def bc(self, shape):
        return T(self.ap.to_broadcast(shape), self.bufs)


def D_(ap):
    return T(ap, [])


class Prog:
    ENG = ["pe", "act", "dve", "pool", "sp"]

    def __init__(self, nc):
        self.nc = nc
        self.ins = {e: [] for e in self.ENG}
        self.ndma = {e: 0 for e in self.ENG}
        self.sb_off = 16512
        self.sb_max = 0
        self.uid = 0
        self.psum_banks = []
        self.ps_i = 0
        self.barrier_deps = {}
        self.all_dmas_since_barrier = []
        self.pools = {}
        self.held = set()
        self.top = 229312
        self.ncc = 0
        self.init_psum()

    def sb(self, shape, dtype, name=None):
        self.uid += 1
        name = f"{name or 't'}_{self.uid}"
        esz = 4 if dtype == F32 else 2
        free = 1
        for s in shape[1:]:
            free *= s
        nbytes = (free * esz + 31) // 32 * 32
        h = self.nc.alloc_sbuf_tensor_at(name, list(shape), dtype, offset=self.sb_off)
        self.sb_off += nbytes
        self.sb_max = max(self.sb_max, self.sb_off)
        assert self.sb_off <= self.top, f"SBUF overflow {self.sb_off} > {self.top} at {name}"
        return T(h.ap(), [Buf()])

    def sb_top(self, shape, dtype, name=None):
        self.uid += 1
        name = f"{name or 't'}_{self.uid}"
        esz = 4 if dtype == F32 else 2
        free = 1
        for s in shape[1:]:
            free *= s
        nbytes = (free * esz + 31) // 32 * 32
        self.top -= nbytes
        assert self.sb_off <= self.top, f"SBUF overflow (top) {self.sb_off} > {self.top} at {name}"
        h = self.nc.alloc_sbuf_tensor_at(name, list(shape), dtype, offset=self.top)
        return T(h.ap(), [Buf()])

    def top_release(self, t):
        self.barrier()
        self.top = t

    def mark(self):
        return self.sb_off

    def release(self, m):
        self.barrier()
        self.sb_off = m
        for k in [k for k, v in self.pools.items() if v[0] >= m]:
            del self.pools[k]

    def tmp(self, key, shape, dtype, n=2):
        if key not in self.pools:
            off = self.sb_off
            self.pools[key] = [off, [self.sb(shape, dtype, key) for _ in range(n)], 0]
        p = self.pools[key]
        t = p[1][p[2] % len(p[1])]
        p[2] += 1
        return t

    def init_psum(self):
        for i in range(8):
            h = self.nc.alloc_psum_tensor(f"psb{i}", [128, 512], F32)
            self.psum_banks.append(T(h.ap(), [Buf()]))

    def ps(self, hold=False):
        while (self.ps_i % 8) in self.held:
            self.ps_i += 1
        i = self.ps_i % 8
        self.ps_i += 1
        if hold:
            self.held.add(i)
        return self.psum_banks[i]

    def ps_free(self, t):
        for i, b in enumerate(self.psum_banks):
            if b.bufs is t.bufs:
                self.held.discard(i)

    def add(self, eng, fn, reads=(), writes=(), dma=False, cc=False):
        lst = self.ins[eng]
        idx = len(lst)
        if cc:
            j = self.ncc
            self.ncc += 1
            me = ("x", eng, j)
        elif dma:
            j = self.ndma[eng]
            self.ndma[eng] += 1
            me = ("d", eng, j)
        else:
            j = None
            me = ("c", eng, idx)
        deps = set()
        for t in reads:
            for b in t.bufs:
                if b.lw is not None:
                    deps.add(b.lw)
        for t in writes:
            for b in t.bufs:
                if b.lw is not None:
                    deps.add(b.lw)
                deps.update(b.rd)
        deps.discard(me)
        if eng in self.barrier_deps:
            deps |= self.barrier_deps.pop(eng)
        if eng == "pe":
            deps = {d for d in deps if not (d[0] == "c" and d[1] == "pe")}
        for t in reads:
            for b in t.bufs:
                b.rd.append(me)
        for t in writes:
            for b in t.bufs:
                b.lw = me
                b.rd = []
        lst.append(dict(fn=fn, deps=deps, dma=dma, j=j, cc=cc))
        if dma or cc:
            self.all_dmas_since_barrier.append(me)
        return me

    def barrier(self):
        deps = set()
        for e in self.ENG:
            for k in range(len(self.ins[e]) - 1, -1, -1):
                if not self.ins[e][k]["dma"] and not self.ins[e][k]["cc"]:
                    deps.add(("c", e, k))
                    break
        deps |= set(self.all_dmas_since_barrier)
        self.all_dmas_since_barrier = []
        for e in self.ENG:
            self.barrier_deps[e] = set(deps) | self.barrier_deps.get(e, set())

    def mm(self, out, lhsT, rhs, start=True, stop=True, **kw):
        return self.add("pe", lambda e: e.matmul(out.ap, lhsT.ap, rhs.ap, start=start, stop=stop, **kw),
                        reads=[lhsT, rhs], writes=[out])

    def tr(self, out, in_, ident):
        return self.add("pe", lambda e: e.transpose(out.ap, in_.ap, ident.ap), reads=[in_, ident], writes=[out])

    def act(self, out, in_, func, bias=None, scale=None, accum=None):
        reads = [in_]
        kw = {}
        if bias is not None:
            if isinstance(bias, T):
                reads.append(bias)
                kw["bias"] = bias.ap
            else:
                kw["bias"] = bias
        if scale is not None:
            if isinstance(scale, T):
                reads.append(scale)
                kw["scale"] = scale.ap
            else:
                kw["scale"] = scale
        writes = [out]
        if accum is not None:
            writes.append(accum)
            kw["accum_out"] = accum.ap
        return self.add("act", lambda e: e.activation(out.ap, in_.ap, func, **kw), reads=reads, writes=writes)

    def tt(self, out, a, b, op, eng="dve"):
        return self.add(eng, lambda e: e.tensor_tensor(out.ap, a.ap, b.ap, op), reads=[a, b], writes=[out])

    def ts(self, out, a, s1, s2, op0, op1=None, eng="dve"):
        reads = [a]
        v1 = s1.ap if isinstance(s1, T) else s1
        v2 = s2.ap if isinstance(s2, T) else s2
        if isinstance(s1, T):
            reads.append(s1)
        if isinstance(s2, T):
            reads.append(s2)
        if op1 is None:
            return self.add(eng, lambda e: e.tensor_scalar(out.ap, a.ap, v1, None, op0), reads=reads, writes=[out])
        return self.add(eng, lambda e: e.tensor_scalar(out.ap, a.ap, v1, v2, op0, op1), reads=reads, writes=[out])

    def stt(self, out, in0, scalar, in1, op0, op1):
        reads = [in0, in1]
        sv = scalar.ap if isinstance(scalar, T) else scalar
        if isinstance(scalar, T):
            reads.append(scalar)
        return self.add("dve", lambda e: e.scalar_tensor_tensor(out.ap, in0.ap, sv, in1.ap, op0, op1),
                        reads=reads, writes=[out])

    def scan(self, out, d0, d1, init, op0=ALU.mult, op1=ALU.add):
        reads = [d0, d1]
        iv = init.ap if isinstance(init, T) else init
        if isinstance(init, T):
            reads.append(init)
        return self.add("dve", lambda e: e.tensor_tensor_scan(out.ap, d0.ap, d1.ap, iv, op0, op1),
                        reads=reads, writes=[out])

    def copy(self, out, in_, eng="dve"):
        if eng == "act":
            return self.add("act", lambda e: e.copy(out.ap, in_.ap), reads=[in_], writes=[out])
        return self.add(eng, lambda e: e.tensor_copy(out.ap, in_.ap), reads=[in_], writes=[out])

    def recip(self, out, in_):
        return self.add("dve", lambda e: e.reciprocal(out.ap, in_.ap), reads=[in_], writes=[out])

    def reduce(self, out, in_, op, axis=AX.X):
        return self.add("dve", lambda e: e.tensor_reduce(out.ap, in_.ap, axis, op), reads=[in_], writes=[out])

    def memset(self, t, val, eng="dve"):
        return self.add(eng, lambda e: e.memset(t.ap, val), reads=[], writes=[t])

    def dma(self, out, in_, q="sp"):
        return self.add(q, lambda e: e.dma_start(out=out.ap, in_=in_.ap), reads=[in_], writes=[out], dma=True)

    def emit(self, final_reads=()):
        nc = self.nc
        if final_reads:
            self.add("sp", lambda e: e.nop(), reads=list(final_reads), writes=[])
        waits = {e: [] for e in self.ENG}
        marked = {e: set() for e in self.ENG}
        for e in self.ENG:
            seen_c = {}
            seen_d = set()
            for ins in self.ins[e]:
                w = []
                if ins["dma"] and ins["j"] >= NDSEM:
                    pj = ins["j"] - NDSEM
                    if ("d", e, pj) not in seen_d:
                        w.append(("d", e, pj))
                        seen_d.add(("d", e, pj))
                best = {}
                for d in ins["deps"]:
                    if d[0] == "c":
                        if d[2] > best.get(d[1], -1):
                            best[d[1]] = d[2]
                    else:
                        if (d[0], d[1], d[2]) not in seen_d:
                            seen_d.add((d[0], d[1], d[2]))
                            w.append(d)
                for f, i in best.items():
                    if i > seen_c.get(f, -1):
                        seen_c[f] = i
                        marked[f].add(i)
                        w.append(("c", f, i))
                waits[e].append(w)
        val = {e: {} for e in self.ENG}
        for e in self.ENG:
            c = 0
            for i in sorted(marked[e]):
                c += 1
                val[e][i] = c
        self.stats = {e: (len(self.ins[e]), len(marked[e])) for e in self.ENG}
        with ExitStack() as st:
            csem = {e: st.enter_context(nc.semaphore(f"cs_{e}")) for e in self.ENG}
            xsem = [st.enter_context(nc.semaphore(f"xs_{i}")) for i in range(self.ncc)]
            dsem = {e: [st.enter_context(nc.semaphore(f"ds_{e}{i}")) for i in range(NDSEM)]
                    for e in self.ENG if self.ndma[e] > 0}
            block = st.enter_context(nc.Block())

            def run(e, eng):
                for k, ins in enumerate(self.ins[e]):
                    for d in waits[e][k]:
                        if d[0] == "c":
                            eng.wait_ge(csem[d[1]], val[d[1]][d[2]])
                        elif d[0] == "x":
                            eng.wait_ge(xsem[d[2]], 1)
                        else:
                            eng.wait_ge(dsem[d[1]][d[2] % NDSEM], 16 * (d[2] // NDSEM + 1))
                    r = ins["fn"](eng)
                    if ins["cc"]:
                        r.then_inc(xsem[ins["j"]])
                    elif ins["dma"]:
                        r.then_inc(dsem[e][ins["j"] % NDSEM], 16)
                    elif k in marked[e]:
                        r.then_inc(csem[e], 1)

            @block.tensor
            def _(eng):
                run("pe", eng)

            @block.scalar
            def _(eng):
                run("act", eng)

            @block.vector
            def _(eng):
                run("dve", eng)

            @block.gpsimd
            def _(eng):
                run("pool", eng)

            @block.sync
            def _(eng):
                run("sp", eng)


class Ctx:
    def __init__(self, nc, P, dram):
        self.nc, self.P, self.dram = nc, P, dram
        cf = P.sb([128, 384], F32, "cf")
        P.dma(cf, D_(dram["cf"]))
        self.ident = cf[:, 0:128]
        self.perm = cf[:, 128:256]
        self.onesf = cf[:, 256:384]
        cb = P.sb([128, 256], BF16, "cb")
        P.dma(cb, D_(dram["cb"]), q="pool")
        self.ones = cb[:, 0:128]
        self.bones = cb[:, 128:256]
        self.epsb = P.sb([128, 1], F32, "eps")
        P.memset(self.epsb, EPS)

    def rstd_from_ps(self, ps_ss, W, inv_n, name="rstd", n=2):
        P = self.P
        r = P.tmp(f"{name}{W}", [128, W], F32, n=n)
        P.act(r, ps_ss, AF.Ln, bias=self.epsb, scale=inv_n)
        P.act(r, r, AF.Exp, scale=-0.5)
        return r


def norm_mod(C, xt, W, A, Bsh, out_h, out_f32=None):
    P = C.P
    ps = P.ps()
    for c in range(8):
        s = P.tmp(f"sq{W}", [128, W], BF16)
        P.act(s, xt[:, c, :], AF.Square)
        P.mm(ps[:, 0:W], C.ones, s, start=(c == 0), stop=(c == 7))
    rstd = C.rstd_from_ps(ps[:, 0:W], W, 1.0 / D)
    for c in range(8):
        t = P.tmp(f"nt{W}", [128, W], F32)
        P.tt(t, xt[:, c, :], rstd, ALU.mult)
        if out_f32 is not None:
            P.act(out_f32[:, c, :], t, AF.Identity, bias=Bsh[:, c:c + 1], scale=A[:, c:c + 1])
            P.copy(out_h[:, c, :], out_f32[:, c, :], eng="pool")
        else:
            P.act(out_h[:, c, :], t, AF.Identity, bias=Bsh[:, c:c + 1], scale=A[:, c:c + 1])
    return


def compute_mods(C, dram, l_list, cvec, out_mods):
    P = C.P
    m = P.mark()
    sc = P.sb([128, 8, 2], F32, "silu_c")
    P.act(sc, cvec, AF.Silu)
    wbuf = [P.sb([128, 8, 1024], F32, "adaw") for _ in range(2)]
    k = 0
    for li, l in enumerate(l_list):
        ps = P.ps(hold=True)
        bT = P.sb([128, 48], F32, "adab")
        P.dma(bT, D_(dram["ada_bT"][l]))
        for j in range(6):
            w = wbuf[k % 2]
            k += 1
            P.dma(w, D_(dram["ada_w"][l].rearrange("(kc p) f -> p kc f", p=128)[:, :, j * 1024:(j + 1) * 1024]))
            for fc in range(8):
                g = j * 8 + fc
                for kc in range(8):
                    P.mm(ps[:, 2 * g:2 * g + 2], w[:, kc, fc * 128:(fc + 1) * 128], sc[:, kc, :],
                         start=(kc == 0), stop=(kc == 7))
        P.tt(out_mods[li], ps[:, 0:96].re("p (g c) -> p g c", c=2),
             T(bT.ap.unsqueeze(2).to_broadcast([128, 48, 2]), bT.bufs), ALU.add)
        P.ps_free(ps)
    P.release(m)


def din(nc, name, shape, dt=F32):
    return nc.dram_tensor(name, list(shape), dt, kind="ExternalInput").ap()


def dout(nc, name, shape, dt=F32):
    return nc.dram_tensor(name, list(shape), dt, kind="ExternalOutput").ap()


def mod_scalars(C, mods_l, gainT, which, col, name):
    P = C.P
    base = 24 * which
    A = P.sb([128, 8], F32, name)
    P.ts(A, mods_l[:, base + 8:base + 16, col], 1.0, None, ALU.add)
    P.tt(A, A, gainT, ALU.mult)
    Sh = P.sb([128, 8], F32, name + "s")
    P.copy(Sh, mods_l[:, base:base + 8, col])
    G = P.sb([128, 8], F32, name + "g")
    P.copy(G, mods_l[:, base + 16:base + 24, col])
    return A, Sh, G


def qk_norm_rope(C, ps_in, W, gain, cos, sin, out):
    P = C.P
    k_sb = P.tmp(f"qk_k{W}", [128, W], F32, n=2)
    P.copy(k_sb, ps_in, eng="act")
    sq = P.tmp(f"qk_sq{W}", [128, W], BF16)
    P.act(sq, ps_in, AF.Square)
    ps2 = P.ps()
    P.mm(ps2[:, 0:W], C.bones, sq)
    rs = C.rstd_from_ps(ps2[:, 0:W], W, 1.0 / 64.0, name="qk_rs", n=2)
    if cos is None:
        P.stt(out, k_sb, gain, rs, ALU.mult, ALU.mult)
        return
    kh = P.tmp(f"qk_kh{W}", [128, W], F32, n=2)
    P.stt(kh, k_sb, gain, rs, ALU.mult, ALU.mult)
    ps3 = P.ps()
    P.mm(ps3[:, 0:W], C.perm, kh)
    t1 = P.tmp(f"qk_t1{W}", [128, W], F32, n=2)
    P.tt(t1, kh, cos, ALU.mult)
    t2 = P.tmp(f"qk_t2{W}", [128, W], F32, n=2)
    P.tt(t2, ps3[:, 0:W], sin, ALU.mult)
    P.tt(out, t1, t2, ALU.add)


def build_L1():
    nc = bass.Bass("TRN2", target_bir_lowering=False)
    dr = {}
    dr["cf"] = din(nc, "cf", [128, 384])
    dr["cb"] = din(nc, "cb", [128, 256])
    dr["xo"] = din(nc, "xo", [D, TOK])
    dr["xh"] = din(nc, "xh", [D, 2])
    dr["hmask"] = din(nc, "hmask", [128, 2])
    dr["ctx"] = din(nc, "ctx", [D, CTXL])
    dr["cvec"] = din(nc, "cvec", [128, 8, 2])
    dr["ada_w"] = din(nc, "ada_w", [2, D, 6 * D])
    dr["ada_bT"] = din(nc, "ada_bT", [2, 128, 48])
    dr["nmixT"] = din(nc, "nmixT", [2, 128, 8])
    dr["w_in"] = din(nc, "w_in", [D, 3072])
    dr["convw"] = din(nc, "convw", [128, 4, 3])
    dr["convb"] = din(nc, "convb", [128, 4])
    dr["qkn"] = din(nc, "qkn", [128, 2])
    dr["cos"] = din(nc, "cos", [128, TOK])
    dr["sin"] = din(nc, "sin", [128, TOK])
    o_kT = dout(nc, "o_kT", [4, 128, TOK], BF16)
    o_v = dout(nc, "o_v", [TOK, 512], BF16)
    o_kcT = dout(nc, "o_kcT", [4, 128, CTXL], BF16)
    o_vc = dout(nc, "o_vc", [CTXL, 512], BF16)
    o_qT = dout(nc, "o_qT", [4, 128, TOK], BF16)
    o_qcT = dout(nc, "o_qcT", [4, 128, CTXL], BF16)
    o_oa = dout(nc, "o_oa", [4, 128, TOK], BF16)
    o_oac = dout(nc, "o_oac", [4, 128, CTXL], BF16)
    o_mods = dout(nc, "o_mods", [2, 128, 48, 2])

    P = Prog(nc)
    C = Ctx(nc, P, dr)
    outb = T(None, [Buf()])

    cvec = P.sb([128, 8, 2], F32, "cvec")
    P.dma(cvec, D_(dr["cvec"]))
    mods = [P.sb([128, 48, 2], F32, f"mods{l}") for l in range(2)]
    compute_mods(C, dr, [0, 1], cvec, mods)
    for l in range(2):
        P.add("sp", lambda e, l=l: e.dma_start(out=o_mods[l], in_=mods[l].ap), reads=[mods[l]], writes=[outb], dma=True)
    nmix = P.sb([128, 8], F32, "nmix")
    P.dma(nmix, D_(dr["nmixT"][0]))
    A_lat, Sh_lat, _ = mod_scalars(C, mods[0], nmix, 0, 0, "Alat")
    A_ctx, Sh_ctx, _ = mod_scalars(C, mods[0], nmix, 0, 1, "Actx")

    w_in = P.sb([128, 8, 3072], BF16, "w_in")
    for kc in range(8):
        P.dma(w_in[:, kc, :], D_(dr["w_in"][kc * 128:(kc + 1) * 128, :]), q="pool")
    convw = P.sb([128, 4, 3], F32, "convw")
    P.dma(convw, D_(dr["convw"]))
    convb = P.sb([128, 4], F32, "convb")
    P.dma(convb, D_(dr["convb"]))
    qkn = P.sb([128, 2], F32, "qkn")
    P.dma(qkn, D_(dr["qkn"]))
    hmask = P.sb([128, 2], F32, "hmask")
    P.dma(hmask, D_(dr["hmask"]))

    qT = P.sb([128, 4, TOK], BF16, "qT")
    kT = P.sb([128, 4, TOK], BF16, "kT")
    cbs = P.sb([128, 4, TOK], BF16, "cbs")
    ub = P.sb([128, 4, TOK + 2], BF16, "ub")
    qcT = P.sb([128, 4, CTXL], BF16, "qcT")
    kcT = P.sb([128, 4, CTXL], BF16, "kcT")
    cbc = P.sb([128, 4, CTXL], BF16, "cbc")
    ubc = P.sb([128, 4, CTXL + 2], BF16, "ubc")
    P.memset(ubc[:, :, 0:1], 0.0)
    P.memset(ubc[:, :, CTXL + 1:CTXL + 2], 0.0)

    def proj(h, W, f):
        ps = P.ps()
        for kc in range(8):
            P.mm(ps[:, 0:W], w_in[:, kc, f * 128:(f + 1) * 128], h[:, kc, 0:W], start=(kc == 0), stop=(kc == 7))
        return ps[:, 0:W]

    def front_tile(xsrc, W, A, Sh, rope, dst):
        xt = P.tmp(f"xt{W}", [128, 8, W], F32, n=1)
        P.dma(xt, D_(xsrc.rearrange("(c p) t -> p c t", p=128)))
        if rope:
            cs_c = P.tmp("cos_t", [128, W], F32)
            cs_s = P.tmp("sin_t", [128, W], F32)
            P.dma(cs_c, D_(rope[0][:, dst["t0"]:dst["t0"] + W]))
            P.dma(cs_s, D_(rope[1][:, dst["t0"]:dst["t0"] + W]))
        h = P.tmp(f"h{W}", [128, 8, W], BF16)
        norm_mod(C, xt, W, A, Sh, h)
        t0 = dst["t0"]
        if dst.get("cb") is not None:
            for c in range(4):
                ps = proj(h, W, c)
                P.copy(dst["cb"][:, c, t0:t0 + W], ps, eng="act")
        for c in range(4):
            ps_cc = proj(h, W, 4 + c)
            cc = P.tmp(f"cc{W}", [128, W], F32)
            P.copy(cc, ps_cc, eng="act")
            ps_cx = proj(h, W, 8 + c)
            P.tt(dst["u"](c), cc, ps_cx, ALU.mult)
        if dst.get("q") is None:
            return
        for hh in range(4):
            ps = proj(h, W, 12 + hh)
            cs = (cs_c, cs_s) if rope else (None, None)
            qk_norm_rope(C, ps, W, qkn[:, 0:1], cs[0], cs[1], dst["q"][:, hh, t0:t0 + W])
        for hh in range(4):
            ps = proj(h, W, 16 + hh)
            cs = (cs_c, cs_s) if rope else (None, None)
            qk_norm_rope(C, ps, W, qkn[:, 1:2], cs[0], cs[1], dst["k"][:, hh, t0:t0 + W])
        for sub in range(W // 128):
            ps = P.ps()
            for kc in range(8):
                P.mm(ps, h[:, kc, sub * 128:(sub + 1) * 128], w_in[:, kc, 2560:3072], start=(kc == 0), stop=(kc == 7))
            vs = P.tmp("vs", [128, 512], BF16, n=3)
            P.copy(vs, ps, eng="act")
            r0 = t0 + sub * 128
            P.add("sp", lambda e, vs=vs, r0=r0, dv=dst["v_out"]: e.dma_start(out=dv[r0:r0 + 128, :], in_=vs.ap),
                  reads=[vs], writes=[outb], dma=True)

    cos, sin = dr["cos"], dr["sin"]

    m_own = P.mark()
    for t in range(TOK // 512):
        front_tile(dr["xo"][:, t * 512:(t + 1) * 512], 512, A_lat, Sh_lat, (cos, sin),
                   dict(cb=cbs, u=lambda c, t=t: ub[:, c, 1 + t * 512:1 + (t + 1) * 512], q=qT, k=kT, v_out=o_v, t0=t * 512))
    P.release(m_own)
    uh = P.sb([128, 4, 2], F32, "uh")
    front_tile(dr["xh"], 2, A_lat, Sh_lat, None, dict(u=lambda c: uh[:, c, :], t0=0))
    for c in range(4):
        P.tt(ub[:, c, 0:1], uh[:, c, 0:1], hmask[:, 0:1], ALU.mult)
        P.tt(ub[:, c, TOK + 1:TOK + 2], uh[:, c, 1:2], hmask[:, 1:2], ALU.mult)
    front_tile(dr["ctx"], CTXL, A_ctx, Sh_ctx, None,
               dict(cb=cbc, u=lambda c: ubc[:, c, 1:1 + CTXL], q=qcT, k=kcT, v_out=o_vc, t0=0))

    def conv(ubuf, cbt, W, o_dst):
        for c in range(4):
            acc = P.tmp(f"cacc{W}", [128, W], F32)
            P.ts(acc, ubuf[:, c, 1:1 + W], convw[:, c, 1:2], convb[:, c:c + 1], ALU.mult, ALU.add)
            P.stt(acc, ubuf[:, c, 0:W], convw[:, c, 0:1], acc, ALU.mult, ALU.add)
            P.stt(acc, ubuf[:, c, 2:2 + W], convw[:, c, 2:3], acc, ALU.mult, ALU.add)
            oa = P.tmp(f"oa{W}", [128, W], BF16)
            P.tt(oa, acc, cbt[:, c, :], ALU.mult)
            P.add("sp", lambda e, oa=oa, c=c: e.dma_start(out=o_dst[c], in_=oa.ap), reads=[oa], writes=[outb], dma=True)

    conv(ub, cbs, TOK, o_oa)
    conv(ubc, cbc, CTXL, o_oac)
    for (src, dst) in ((qT, o_qT), (kT, o_kT), (qcT, o_qcT), (kcT, o_kcT)):
        P.add("sp", lambda e, src=src, dst=dst: e.dma_start(out=dst.rearrange("h p t -> p h t"), in_=src.ap),
              reads=[src], writes=[outb], dma=True)
    P.emit(final_reads=[outb])
    return nc, P


def host_consts():
    cf = np.zeros((128, 384), np.float32)
    cf[:, 256:384] = 1.0
    cf[np.arange(128), np.arange(128)] = 1.0
    for m in range(128):
        partner = m + 32 if (m % 64) < 32 else m - 32
        cf[partner, 128 + m] = 1.0
    cb = np.zeros((128, 256), np.float32)
    cb[:, 0:128] = 1.0
    for k in range(128):
        for m in range(128):
            if k // 64 == m // 64:
                cb[k, 128 + m] = 1.0
    return cf, cb


def rope_tables():
    n_rows = SEQ // 64
    rows = np.repeat(np.arange(n_rows), 64).astype(np.float32)
    cols = np.tile(np.arange(64), n_rows).astype(np.float32)
    n_freq = 16
    inv = (np.float32(10000.0) ** (-np.arange(n_freq, dtype=np.float32) / np.float32(n_freq))).astype(np.float32)
    ang = np.concatenate([rows[:, None] * inv, cols[:, None] * inv], axis=-1).astype(np.float32)
    cos, sin = np.cos(ang).astype(np.float32), np.sin(ang).astype(np.float32)
    idx = np.arange(128) % 32
    cosT = np.ascontiguousarray(cos[:, idx].T)
    sgn = np.where((np.arange(128) % 64) < 32, -1.0, 1.0).astype(np.float32)
    sinT = np.ascontiguousarray(sin[:, idx].T * sgn[:, None])
    return cosT, sinT


def chunkT(v):
    return np.ascontiguousarray(v.reshape(-1, 128).T)


def moe_layer(C, dr, l, tiles, A2, Sh2, G2, A2c=None, Sh2c=None, G2c=None):
    P = C.P
    m0 = P.mark()
    NT = sum(W // 128 for (_, W, _) in tiles)
    TT = sum(W for (_, W, _) in tiles)
    hf = P.sb([128, 8, TT], BF16, "hf")
    gateT = P.sb([16, TT], F32, "gateT")
    wr = P.sb([128, 8, 20], F32, "wr")
    P.dma(wr, D_(dr["moe_wr"][l].rearrange("(kc p) n -> p kc n", p=128)))
    rb = P.sb([128, 20], F32, "rb")
    P.dma(rb, D_(dr["moe_rb"][l]))
    sel = P.sb([16, 16, 128], F32, "sel")
    P.dma(sel, D_(dr["sel"]))
    def load_w(e):
        wg = P.tmp("wg", [128, 8, 512], BF16)
        wu = P.tmp("wu", [128, 8, 512], BF16)
        wd = P.tmp("wd", [128, 4, 1024], BF16)
        P.dma(wg, D_(dr["moe_wg"][l, e].rearrange("(kc p) f -> p kc f", p=128)), q="pool")
        P.dma(wu, D_(dr["moe_wu"][l, e].rearrange("(kc p) f -> p kc f", p=128)), q="pool")
        P.dma(wd, D_(dr["moe_wd"][l, e].rearrange("(fc p) d -> p fc d", p=128)), q="pool")
        return wg, wu, wd

    wts = {0: load_w(0)}
    m1 = P.mark()
    ps_l = P.ps(hold=True)
    off = 0
    nt = 0
    wg1, wu1 = P.pools["wg"][1][1], P.pools["wu"][1][1]
    al_a = T(P.nc.alloc_sbuf_tensor_at(f"hf32a_{l}_{P.uid}", [128, 4, 512], F32, offset=P.pools["wg"][0] + 8192).ap(), wg1.bufs)
    al_b = T(P.nc.alloc_sbuf_tensor_at(f"hf32b_{l}_{P.uid}", [128, 4, 512], F32, offset=P.pools["wu"][0] + 8192).ap(), wu1.bufs)
    for ti_, (xf, W, is_ctx) in enumerate(tiles):
        A, Sh = (A2c, Sh2c) if is_ctx else (A2, Sh2)
        if ti_ % 2 == 0:
            hf32_t = P.tmp("hf32", [128, 8, 512], F32, n=1)

            def hf32c(c, hf32_t=hf32_t, W=W):
                return hf32_t[:, c, 0:W]
        else:
            def hf32c(c, W=W):
                return (al_a if c < 4 else al_b)[:, c % 4, 0:W]
        ps = P.ps()
        for c in range(8):
            s_ = P.tmp("msq", [128, 512], BF16, n=1)[:, 0:W]
            P.act(s_, xf(c), AF.Square)
            P.mm(ps[:, 0:W], C.ones, s_, start=(c == 0), stop=(c == 7))
        rstd = P.tmp("mrstd", [128, 512], F32, n=1)[:, 0:W]
        P.act(rstd, ps[:, 0:W], AF.Ln, bias=C.epsb, scale=1.0 / D)
        P.act(rstd, rstd, AF.Exp, scale=-0.5)
        ps_t = P.ps()
        for c in range(8):
            t = P.tmp("mnt", [128, 512], F32)[:, 0:W]
            P.tt(t, xf(c), rstd, ALU.mult)
            P.act(hf32c(c), t, AF.Identity, bias=Sh[:, c:c + 1], scale=A[:, c:c + 1])
            P.copy(hf[:, c, off:off + W], hf32c(c), eng="dve")
            P.mm(ps_t[0:20, 0:W], wr[:, c, :], hf32c(c), start=(c == 0), stop=(c == 7))
        lT = P.tmp("mlT", [20, 512], F32, n=1)[:, 0:W]
        P.copy(lT, ps_t[0:20, 0:W], eng="act")
        for sub in range(W // 128):
            P.tr(ps_l[:, nt * 20:nt * 20 + 20], lT[:, sub * 128:(sub + 1) * 128], C.ident[0:20, 0:20])
            nt += 1
        off += W
    Lg = P.sb([128, NT, 20], F32, "Lg")
    P.tt(Lg, ps_l[:, 0:NT * 20].re("p (t k) -> p t k", k=20), T(rb.ap.unsqueeze(1).to_broadcast([128, NT, 20]), rb.bufs), ALU.add)
    gl = Lg[:, :, 0:4]
    el = Lg[:, :, 4:20].re("p t (g e) -> p t g e", e=4)

    def bc3(t, n):
        return T(t.ap.unsqueeze(2).to_broadcast([128, NT, n]), t.bufs)

    gmax = P.sb([128, NT], F32, "gmax")
    P.reduce(gmax, gl, ALU.max)
    oh = P.sb([128, NT, 4], F32, "oh")
    P.tt(oh, gl, bc3(gmax, 4), ALU.is_equal)
    gsh = P.sb([128, NT, 4], F32, "gsh")
    P.tt(gsh, gl, bc3(gmax, 4), ALU.subtract)
    P.act(gsh, gsh, AF.Exp)
    psel = P.sb([128, NT], F32, "psel")
    P.reduce(psel, gsh, ALU.add)
    P.recip(psel, psel)
    e4 = P.sb([128, NT, 4, 4], F32, "e4")
    P.tt(e4, el, T(oh.ap.unsqueeze(3).to_broadcast([128, NT, 4, 4]), oh.bufs), ALU.mult)
    esel = P.sb([128, NT, 4], F32, "esel")
    P.reduce(esel, e4.re("p t g e -> p t e g"), ALU.add)
    mx1 = P.sb([128, NT], F32, "mx1")
    P.reduce(mx1, esel, ALU.max)
    mk1 = P.sb([128, NT, 4], F32, "mk1")
    P.tt(mk1, esel, bc3(mx1, 4), ALU.is_equal)
    es2 = P.sb([128, NT, 4], F32, "es2")
    P.stt(es2, mk1, -1e30, esel, ALU.mult, ALU.add)
    mx2 = P.sb([128, NT], F32, "mx2")
    P.reduce(mx2, es2, ALU.max)
    mk2 = P.sb([128, NT, 4], F32, "mk2")
    P.tt(mk2, es2, bc3(mx2, 4), ALU.is_equal)
    w1 = P.sb([128, NT], F32, "w1")
    P.tt(w1, mx1, mx2, ALU.subtract)
    P.act(w1, w1, AF.Sigmoid)
    P.tt(w1, w1, psel, ALU.mult)
    w2 = P.sb([128, NT], F32, "w2")
    P.tt(w2, psel, w1, ALU.subtract)
    P.tt(mk1, mk1, bc3(w1, 4), ALU.mult)
    P.tt(mk2, mk2, bc3(w2, 4), ALU.mult)
    P.tt(mk1, mk1, mk2, ALU.add)
    gate = e4
    P.tt(gate, T(oh.ap.unsqueeze(3).to_broadcast([128, NT, 4, 4]), oh.bufs),
         T(mk1.ap.unsqueeze(2).to_broadcast([128, NT, 4, 4]), mk1.bufs), ALU.mult)
    gflat = gate.re("p t g e -> p t (g e)")
    for t4 in range(0, NT, 4):
        ps = P.ps()
        n = min(4, NT - t4)
        for i in range(n):
            P.tr(ps[0:16, i * 128:(i + 1) * 128], gflat[:, t4 + i, :], C.ident)
        P.copy(gateT[:, t4 * 128:(t4 + n) * 128], ps[0:16, 0:n * 128])
    P.ps_free(ps_l)
    P.release(m1)

    steps = []
    for e in range(16):
        off = 0
        for (xf, W, is_ctx) in tiles:
            steps.append((e, xf, W, is_ctx, off))
            off += W

    pend = None

    def down(st):
        (e, xf, W, is_ctx, off, actb, wd) = st
        G = G2c if is_ctx else G2
        for dc in range(8):
            ps = P.ps()
            for fc in range(4):
                P.mm(ps[:, 0:W], wd[:, fc, dc * 128:(dc + 1) * 128], actb[:, fc, :], start=(fc == 0), stop=(fc == 3))
            P.stt(xf(dc), ps[:, 0:W], G[:, dc:dc + 1], xf(dc), ALU.mult, ALU.add)

    for si, (e, xf, W, is_ctx, off) in enumerate(steps):
        wg, wu, wd = wts[e]
        psb = P.ps()
        P.mm(psb[:, 0:W], sel[:, e, :], gateT[:, off:off + W])
        gbc = P.tmp(f"gbc{W}", [128, W], F32)
        P.copy(gbc, psb[:, 0:W], eng="act")
        actb = P.tmp(f"actb{W}", [128, 4, W], BF16)
        for fc in range(4):
            ps_g = P.ps()
            for kc in range(8):
                P.mm(ps_g[:, 0:W], wg[:, kc, fc * 128:(fc + 1) * 128], hf[:, kc, off:off + W], start=(kc == 0), stop=(kc == 7))
            ps_u = P.ps()
            for kc in range(8):
                P.mm(ps_u[:, 0:W], wu[:, kc, fc * 128:(fc + 1) * 128], hf[:, kc, off:off + W], start=(kc == 0), stop=(kc == 7))
            sg = P.tmp(f"sg{W}", [128, W], F32)
            P.act(sg, ps_g[:, 0:W], AF.Silu)
            t2 = P.tmp(f"t2{W}", [128, W], F32)
            P.tt(t2, sg, ps_u[:, 0:W], ALU.mult)
            P.tt(actb[:, fc, :], t2, gbc, ALU.mult)
        if pend is not None:
            down(pend)
        pend = (e, xf, W, is_ctx, off, actb, wd)
        if off == 0 and e + 1 < 16:
            wts[e + 1] = load_w(e + 1)
    down(pend)
    P.release(m0)


def moe_drams(nc, dr):
    dr["moe_wr"] = din(nc, "moe_wr", [1, D, 20])
    dr["moe_rb"] = din(nc, "moe_rb", [1, 128, 20])
    dr["sel"] = din(nc, "sel", [16, 16, 128])
    dr["moe_wg"] = din(nc, "moe_wg", [1, 16, D, 512])
    dr["moe_wu"] = din(nc, "moe_wu", [1, 16, D, 512])
    dr["moe_wd"] = din(nc, "moe_wd", [1, 16, 512, D])


def build_L2(debug=False):
    nc = bass.Bass("TRN2", target_bir_lowering=False)
    dr = {}
    dr["cf"] = din(nc, "cf", [128, 384])
    dr["cb"] = din(nc, "cb", [128, 256])
    dr["kT"] = din(nc, "kT", [4, 128, NKT * 128], BF16)
    dr["v"] = din(nc, "v", [4, 128, NKT, 128], BF16)
    dr["qT"] = din(nc, "qT", [4, 128, TOK], BF16)
    dr["qcT"] = din(nc, "qcT", [4, 128, CTXL], BF16)
    dr["oa"] = din(nc, "oa", [4, 128, TOK], BF16)
    dr["oac"] = din(nc, "oac", [4, 128, CTXL], BF16)
    dr["xo"] = din(nc, "xo", [D, TOK])
    dr["ctx"] = din(nc, "ctx", [D, CTXL])
    dr["mods"] = din(nc, "mods", [2, 128, 48, 2])
    dr["lamp"] = din(nc, "lamp", [64, 4])
    dr["subn"] = din(nc, "subn", [128, 1])
    dr["w_out"] = din(nc, "w_out", [D, D])
    dr["nffnT"] = din(nc, "nffnT", [128, 8])
    dr["nmix1T"] = din(nc, "nmix1T", [128, 8])
    dr["od_w_in"] = din(nc, "od_w_in", [D, 2048])
    moe_drams(nc, dr)
    o_x2 = dout(nc, "o_x2", [8, 128, TOK])
    o_yg = dout(nc, "o_yg", [8, 128, TOK], BF16)
    o_u = dout(nc, "o_u", [8, 128, TOK])
    o_uc = dout(nc, "o_uc", [8, 128, CTXL])
    if debug:
        o_x1 = dout(nc, "o_x1", [8, 128, TOK])
        o_c2 = dout(nc, "o_c2", [8, 128, CTXL])

    P = Prog(nc)
    C = Ctx(nc, P, dr)
    outb = T(None, [Buf()])
    LAM_INIT = 0.8 - 0.6 * math.exp(-0.3 * 0)

    mods = [P.sb([128, 48, 2], F32, f"mods{l}") for l in range(2)]
    for l in range(2):
        P.dma(mods[l], D_(dr["mods"][l]))
    nffn = P.sb([128, 8], F32, "nffn")
    P.dma(nffn, D_(dr["nffnT"]))
    nmix1 = P.sb([128, 8], F32, "nmix1")
    P.dma(nmix1, D_(dr["nmix1T"]))
    _, _, G1 = mod_scalars(C, mods[0], nffn, 0, 0, "m0l")
    _, _, G1c = mod_scalars(C, mods[0], nffn, 0, 1, "m0c")
    A2, Sh2, G2 = mod_scalars(C, mods[0], nffn, 1, 0, "f0l")
    A2c, Sh2c, G2c = mod_scalars(C, mods[0], nffn, 1, 1, "f0c")
    A1n, Sh1n, _ = mod_scalars(C, mods[1], nmix1, 0, 0, "m1l")
    A1nc, Sh1nc, _ = mod_scalars(C, mods[1], nmix1, 0, 1, "m1c")

    lamp = P.sb([64, 4], F32, "lamp")
    P.dma(lamp, D_(dr["lamp"]))
    lpr = P.sb([64, 2], F32, "lpr")
    P.tt(lpr[:, 0:1], lamp[:, 0:1], lamp[:, 1:2], ALU.mult)
    P.tt(lpr[:, 1:2], lamp[:, 2:3], lamp[:, 3:4], ALU.mult)
    psl = P.ps()
    P.mm(psl[:, 0:2], C.onesf[0:64, :], lpr)
    lex = P.sb([128, 2], F32, "lex")
    P.act(lex, psl[:, 0:2], AF.Exp)
    nlam = P.sb([128, 1], F32, "nlam")
    P.tt(nlam, lex[:, 1:2], lex[:, 0:1], ALU.subtract)
    P.ts(nlam, nlam, -LAM_INIT, None, ALU.add)
    subn = P.sb([128, 1], F32, "subn")
    P.dma(subn, D_(dr["subn"]))
    P.ts(subn, subn, 1.0 - LAM_INIT, None, ALU.mult)

    xres = P.sb([128, 8, TOK], F32, "xres")
    xc = P.sb([128, 8, CTXL], F32, "xc")
    P.dma(xres, D_(dr["xo"].rearrange("(c p) t -> p c t", p=128)))
    P.dma(xc, D_(dr["ctx"].rearrange("(c p) t -> p c t", p=128)))
    m_mix = P.mark()
    w_out = P.sb([128, 8, D], BF16, "w_out")
    for kc in range(8):
        P.dma(w_out[:, kc, :], D_(dr["w_out"][kc * 128:(kc + 1) * 128, :]), q="pool")
    mix = P.sb([128, 8, TOK], BF16, "mix")
    mixc = P.sb([128, 8, CTXL], BF16, "mixc")
    P.dma(mix[:, 0:4, :], D_(dr["oa"].rearrange("c p t -> p c t")))
    P.dma(mixc[:, 0:4, :], D_(dr["oac"].rearrange("c p t -> p c t")))
    m_att = P.mark()
    qT = P.sb([128, 4, TOK], BF16, "qT")
    P.dma(qT, D_(dr["qT"].rearrange("h p t -> p h t")))
    qcT = P.sb([128, 4, CTXL], BF16, "qcT")
    P.dma(qcT, D_(dr["qcT"].rearrange("h p t -> p h t")))

    SB = [P.psum_banks[i] for i in range(4)]
    ACC = [(P.psum_banks[4], P.psum_banks[5]), (P.psum_banks[6], P.psum_banks[7])]
    P.held.update([0, 1, 2, 3, 4, 5, 6, 7])
    sctr = [0]

    def attn_group(kTh, vh, qsrc, W, kts, m):
        accO, accL = ACC[m]
        lo, hi = m * 64, (m + 1) * 64
        n = len(kts)

        def S(i):
            sb_ = SB[sctr[0] % 4]
            sctr[0] += 1
            kt = kts[i]
            P.mm(sb_[:, 0:W], kTh[lo:hi, kt * 128:(kt + 1) * 128], qsrc[lo:hi, :])
            return sb_

        cur = S(0)
        for i in range(n):
            nxt = S(i + 1) if i + 1 < n else None
            pt = P.tmp("pt", [128, 512], BF16, n=3)[:, 0:W]
            P.act(pt, cur[:, 0:W], AF.Exp, scale=0.125)
            kt = kts[i]
            P.mm(accO[:, 0:W], vh[:, kt, :], pt, start=(i == 0), stop=(i == n - 1))
            P.mm(accL[:, 0:W], C.ones, pt, start=(i == 0), stop=(i == n - 1))
            cur = nxt
        return accO, accL

    def attn_tile(kTh, vh, qsrc_fn, W, kts, dst):
        om = []
        for m in range(2):
            accO, accL = attn_group(kTh, vh, qsrc_fn, W, kts, m)
            rl = P.tmp("rl", [128, 512], F32)[:, 0:W]
            P.recip(rl, accL[:, 0:W])
            o = P.tmp("om", [128, 512], F32, n=4)[:, 0:W]
            P.tt(o, accO[:, 0:W], rl, ALU.mult)
            om.append(o)
        o = P.tmp("od", [128, 512], F32)[:, 0:W]
        P.stt(o, om[1], nlam, om[0], ALU.mult, ALU.add)
        sq = P.tmp("asq", [128, 512], BF16)[:, 0:W]
        P.act(sq, o, AF.Square)
        ps = SB[sctr[0] % 4]
        sctr[0] += 1
        P.mm(ps[:, 0:W], C.ones, sq)
        rstd = P.tmp("arstd", [128, 512], F32)[:, 0:W]
        P.act(rstd, ps[:, 0:W], AF.Sqrt, bias=C.epsb, scale=1.0 / 128.0)
        P.recip(rstd, rstd)
        P.stt(dst, o, subn, rstd, ALU.mult, ALU.mult)

    for hh in range(4):
        kTh = P.tmp("kTh", [128, NKT * 128], BF16, n=1)
        vh = P.tmp("vh", [128, NKT, 128], BF16, n=1)
        P.dma(kTh, D_(dr["kT"][hh]))
        P.dma(vh, D_(dr["v"][hh]))
        for qt in range(TOK // 512):
            attn_tile(kTh, vh, qT[:, hh, qt * 512:(qt + 1) * 512], 512, list(range(NKT)),
                      mix[:, 4 + hh, qt * 512:(qt + 1) * 512])
        attn_tile(kTh, vh, qcT[:, hh, :], CTXL, [NKT - 2, NKT - 1], mixc[:, 4 + hh, :])
    P.held.clear()
    P.ps_i = 0
    P.release(m_att)

    tiles = [(lambda c, t=t: xres[:, c, t * 512:(t + 1) * 512], 512, False) for t in range(TOK // 512)]
    tiles.append((lambda c: xc[:, c, :], CTXL, True))
    for ti, (xf, W, is_ctx) in enumerate(tiles):
        src = mixc if is_ctx else mix[:, :, ti * 512:(ti + 1) * 512]
        G = G1c if is_ctx else G1
        for dc in range(8):
            ps = P.ps()
            for kc in range(8):
                P.mm(ps[:, 0:W], w_out[:, kc, dc * 128:(dc + 1) * 128], src[:, kc, :], start=(kc == 0), stop=(kc == 7))
            P.stt(xf(dc), ps[:, 0:W], G[:, dc:dc + 1], xf(dc), ALU.mult, ALU.add)
    if debug:
        P.add("sp", lambda e: e.dma_start(out=o_x1.rearrange("c p t -> p c t"), in_=xres.ap), reads=[xres], writes=[outb], dma=True)
    P.release(m_mix)
    moe_layer(C, dr, 0, tiles, A2, Sh2, G2, A2c, Sh2c, G2c)
    P.add("sp", lambda e: e.dma_start(out=o_x2.rearrange("c p t -> p c t"), in_=xres.ap), reads=[xres], writes=[outb], dma=True)
    if debug:
        P.add("sp", lambda e: e.dma_start(out=o_c2.rearrange("c p t -> p c t"), in_=xc.ap), reads=[xc], writes=[outb], dma=True)

    w1 = P.sb([128, 8, 2048], BF16, "w1in")
    for kc in range(8):
        P.dma(w1[:, kc, :], D_(dr["od_w_in"][kc * 128:(kc + 1) * 128, :]), q="pool")
    for ti, (xf, W, is_ctx) in enumerate(tiles):
        A, Sh = (A1nc, Sh1nc) if is_ctx else (A1n, Sh1n)
        h1 = P.tmp(f"h1_{W}", [128, 8, W], BF16)
        ps = P.ps()
        for c in range(8):
            s_ = P.tmp(f"sq{W}", [128, W], BF16)
            P.act(s_, xf(c), AF.Square)
            P.mm(ps[:, 0:W], C.ones, s_, start=(c == 0), stop=(c == 7))
        rstd = C.rstd_from_ps(ps[:, 0:W], W, 1.0 / D)
        for c in range(8):
            t = P.tmp(f"nt{W}", [128, W], F32)
            P.tt(t, xf(c), rstd, ALU.mult)
            P.act(h1[:, c, :], t, AF.Identity, bias=Sh[:, c:c + 1], scale=A[:, c:c + 1])
        for f in range(16):
            if is_ctx and f < 8:
                continue
            ps = P.ps()
            for kc in range(8):
                P.mm(ps[:, 0:W], w1[:, kc, f * 128:(f + 1) * 128], h1[:, kc, :], start=(kc == 0), stop=(kc == 7))
            if f < 8:
                yg = P.tmp("yg", [128, W], BF16, n=3)
                P.act(yg, ps[:, 0:W], AF.Gelu_apprx_tanh)
                P.add("sp", lambda e, yg=yg, f=f, ti=ti: e.dma_start(out=o_yg[f, :, ti * 512:(ti + 1) * 512], in_=yg.ap),
                      reads=[yg], writes=[outb], dma=True)
            else:
                uu = P.tmp(f"uu{W}", [128, W], F32, n=3)
                P.copy(uu, ps[:, 0:W], eng="act")
                if is_ctx:
                    P.add("sp", lambda e, uu=uu, f=f: e.dma_start(out=o_uc[f - 8], in_=uu.ap), reads=[uu], writes=[outb], dma=True)
                else:
                    P.add("sp", lambda e, uu=uu, f=f, ti=ti: e.dma_start(out=o_u[f - 8, :, ti * 512:(ti + 1) * 512], in_=uu.ap),
                          reads=[uu], writes=[outb], dma=True)
    P.emit(final_reads=[outb])
    return nc, P


_CACHE = {}


def _get(name, fn):
    if name not in _CACHE:
        _CACHE[name] = fn()
    return _CACHE[name]


def host_sel():
    sel = np.zeros((16, 16, 128), np.float32)
    for e in range(16):
        sel[e, e, :] = 1.0
    return sel


def l1_inputs(r, inp, cf, cb, cosT, sinT):
    b, j = r // 4, r % 4
    s0 = j * TOK
    xT = np.ascontiguousarray(inp["x"][b].T)
    xh = np.zeros((D, 2), np.float32)
    hm = np.zeros((128, 2), np.float32)
    if s0 > 0:
        xh[:, 0] = xT[:, s0 - 1]
        hm[:, 0] = 1
    if s0 + TOK < SEQ:
        xh[:, 1] = xT[:, s0 + TOK]
        hm[:, 1] = 1
    cvec = np.stack([chunkT(inp["c"][b]), chunkT(inp["c_ctx"])], axis=-1)
    return dict(cf=cf, cb=cb, xo=np.ascontiguousarray(xT[:, s0:s0 + TOK]), xh=xh, hmask=hm,
                ctx=np.ascontiguousarray(inp["ctx"][b].T), cvec=np.ascontiguousarray(cvec),
                ada_w=inp["ada_w"], ada_bT=np.ascontiguousarray(inp["ada_b"].reshape(2, 48, 128).transpose(0, 2, 1)),
                nmixT=np.stack([chunkT(inp["norm_mix"][l]) for l in range(2)]),
                w_in=inp["ev_w_in"][0],
                convw=np.ascontiguousarray(inp["ev_conv_w"][0].reshape(3, 4, 128).transpose(2, 1, 0)),
                convb=np.ascontiguousarray(chunkT(inp["ev_conv_b"][0])),
                qkn=np.ascontiguousarray(np.stack([np.tile(inp["ev_q_norm"][0], 2), np.tile(inp["ev_k_norm"][0], 2)], axis=-1)),
                cos=np.ascontiguousarray(cosT[:, s0:s0 + TOK]), sin=np.ascontiguousarray(sinT[:, s0:s0 + TOK]))


def moe_inputs(inp, l):
    wr = np.concatenate([inp["moe_w_grp"][l], inp["moe_w_rt"][l].reshape(D, 16)], axis=1)[None]
    rb = np.concatenate([inp["moe_b_grp"][l], inp["moe_b_rt"][l].reshape(16)])
    rb = np.ascontiguousarray(np.broadcast_to(rb[None, None, :], (1, 128, 20)))
    return dict(moe_wr=np.ascontiguousarray(wr), moe_rb=rb, sel=host_sel(),
                moe_wg=inp["moe_w_gate"][l:l + 1], moe_wu=inp["moe_w_up"][l:l + 1], moe_wd=inp["moe_w_down"][l:l + 1])


def l2_inputs(r, inp, o1, cf, cb, xo_list):
    b = r // 4
    grp = [o1[4 * b + j] for j in range(4)]
    kT = np.concatenate([g["o_kT"] for g in grp] + [grp[0]["o_kcT"]], axis=2)
    v = np.concatenate([g["o_v"] for g in grp] + [grp[0]["o_vc"]], axis=0)
    v = np.ascontiguousarray(v.reshape(NKT, 128, 4, 128).transpose(2, 1, 0, 3))
    d = dict(cf=cf, cb=cb, kT=np.ascontiguousarray(kT), v=v, qT=o1[r]["o_qT"], qcT=o1[r]["o_qcT"],
             oa=o1[r]["o_oa"], oac=o1[r]["o_oac"], xo=xo_list[r], ctx=np.ascontiguousarray(inp["ctx"][b].T),
             mods=o1[r]["o_mods"],
             lamp=np.ascontiguousarray(np.stack([inp["ev_lam_q1"][0], inp["ev_lam_k1"][0], inp["ev_lam_q2"][0], inp["ev_lam_k2"][0]], axis=-1)),
             subn=np.ascontiguousarray(inp["ev_sub_norm"][0].reshape(128, 1)),
             w_out=inp["ev_w_out"][0], nffnT=chunkT(inp["norm_ffn"][0]), nmix1T=chunkT(inp["norm_mix"][1]),
             od_w_in=inp["od_w_in"][0])
    d.update(moe_inputs(inp, 0))
    return d


LRU_PW = [2048]


def lru_params(C, dr):
    P = C.P
    prm = {}
    prm["cw"] = P.sb([128, 8, 2, 4], F32, "lcw")
    P.dma(prm["cw"], D_(dr["l_cw"]))
    for k in ("l_cb", "l_ba", "l_bx", "l_lam"):
        prm[k] = P.sb([128, 8, 2], F32, k)
        P.dma(prm[k], D_(dr[k]))
    prm["wa"] = P.sb([128, 2, 8, 128], BF16, "lwa")
    prm["wx"] = P.sb([128, 2, 8, 128], BF16, "lwx")
    P.dma(prm["wa"], D_(dr["l_wa"]), q="pool")
    P.dma(prm["wx"], D_(dr["l_wx"]), q="pool")
    e = P.sb([128, 8, 2], F32, "l_e")
    P.act(e, prm["l_lam"], AF.Exp, scale=-1.0)
    P.act(e, e, AF.Ln, bias=C.onesf[:, 0:1])
    prm["s1"] = P.sb([128, 8, 2], F32, "l_s1")
    prm["s2"] = P.sb([128, 8, 2], F32, "l_s2")
    P.ts(prm["s1"], e, -8.0, None, ALU.mult)
    P.ts(prm["s2"], e, -16.0, None, ALU.mult)
    return prm


def lru_drams(nc, dr):
    dr["l_cw"] = din(nc, "l_cw", [128, 8, 2, 4])
    for k in ("l_cb", "l_ba", "l_bx", "l_lam"):
        dr[k] = din(nc, k, [128, 8, 2])
    dr["l_wa"] = din(nc, "l_wa", [128, 2, 8, 128])
    dr["l_wx"] = din(nc, "l_wx", [128, 2, 8, 128])


def lru_coeffs(C, prm, ub, W, c, d, nb=2):
    P = C.P
    cw = prm["cw"]
    o0 = 0 if d == 0 else 3
    uc = P.tmp("l_uc", [128, LRU_PW[0]], F32, n=nb)[:, 0:W]
    P.ts(uc, ub[:, o0:o0 + W], cw[:, c, d, 0:1], prm["l_cb"][:, c, d:d + 1], ALU.mult, ALU.add)
    for k in range(1, 4):
        P.stt(uc, ub[:, o0 + k:o0 + k + W], cw[:, c, d, k:k + 1], uc, ALU.mult, ALU.add)
    ucb = P.tmp("l_ucb", [128, LRU_PW[0]], BF16, n=nb)[:, 0:W]
    P.copy(ucb, uc, eng="pool")
    r = P.tmp("l_r", [128, LRU_PW[0]], F32, n=nb)[:, 0:W]
    ig = P.tmp("l_ig", [128, LRU_PW[0]], F32, n=nb)[:, 0:W]
    for t0 in range(0, W, 512):
        w = min(512, W - t0)
        ps = P.ps()
        P.mm(ps[:, 0:w], prm["wa"][:, d, c, :], ucb[:, t0:t0 + w])
        P.act(r[:, t0:t0 + w], ps[:, 0:w], AF.Sigmoid, bias=prm["l_ba"][:, c, d:d + 1])
        ps2 = P.ps()
        P.mm(ps2[:, 0:w], prm["wx"][:, d, c, :], ucb[:, t0:t0 + w])
        P.act(ig[:, t0:t0 + w], ps2[:, 0:w], AF.Sigmoid, bias=prm["l_bx"][:, c, d:d + 1])
    a = P.tmp("l_a", [128, LRU_PW[0]], F32, n=nb)[:, 0:W]
    P.act(a, r, AF.Exp, scale=prm["s1"][:, c, d:d + 1])
    t = P.tmp("l_t", [128, LRU_PW[0]], F32, n=nb)[:, 0:W]
    P.act(t, r, AF.Exp, scale=prm["s2"][:, c, d:d + 1])
    P.act(t, t, AF.Sqrt, bias=C.onesf[:, 0:1], scale=-1.0)
    P.tt(ig, ig, uc, ALU.mult)
    P.tt(ig, ig, t, ALU.mult)
    return a, ig


def lru_scan(C, a, bb, W, d, init, out):
    P = C.P
    if d == 0:
        P.scan(out, a, bb, init)
    else:
        P.scan(out[:, ::-1], a[:, ::-1], bb[:, ::-1], init)


def load_ub(C, dr_u, dr_hp, dr_hn, c, W, key, nb=2):
    P = C.P
    ub = P.tmp(key, [128, W + 6], F32, n=nb)
    if dr_hp is None:
        P.memset(ub[:, 0:3], 0.0)
        P.memset(ub[:, W + 3:W + 6], 0.0)
    else:
        P.dma(ub[:, 0:3], D_(dr_hp[c]))
        P.dma(ub[:, W + 3:W + 6], D_(dr_hn[c]))
    P.dma(ub[:, 3:3 + W], D_(dr_u[c]))
    return ub


def build_L3():
    nc = bass.Bass("TRN2", target_bir_lowering=False)
    dr = {}
    dr["cf"] = din(nc, "cf", [128, 384])
    dr["cb"] = din(nc, "cb", [128, 256])
    dr["u"] = din(nc, "u", [8, 128, TOK])
    dr["uhp"] = din(nc, "uhp", [8, 128, 3])
    dr["uhn"] = din(nc, "uhn", [8, 128, 3])
    dr["uc"] = din(nc, "uc", [8, 128, CTXL])
    lru_drams(nc, dr)
    o_sum = dout(nc, "o_sum", [128, 8, 2, 3])
    P = Prog(nc)
    C = Ctx(nc, P, dr)
    outb = T(None, [Buf()])
    prm = lru_params(C, dr)
    summ = P.sb([128, 8, 2, 3], F32, "summ")
    for c in range(8):
        ubc = load_ub(C, dr["uc"], None, None, c, CTXL, "ubc")
        ub = load_ub(C, dr["u"], dr["uhp"], dr["uhn"], c, TOK, "ub")
        for d in range(2):
            a, bb = lru_coeffs(C, prm, ubc, CTXL, c, d)
            h = P.tmp("l_h", [128, 2048], F32)[:, 0:CTXL]
            lru_scan(C, a, bb, CTXL, d, 0.0, h)
            P.copy(summ[:, c, d, 2:3], h[:, CTXL - 1:CTXL] if d == 0 else h[:, 0:1])
            a, bb = lru_coeffs(C, prm, ub, TOK, c, d)
            h = P.tmp("l_h", [128, 2048], F32)[:, 0:TOK]
            lru_scan(C, a, bb, TOK, d, 0.0, h)
            P.copy(summ[:, c, d, 1:2], h[:, TOK - 1:TOK] if d == 0 else h[:, 0:1])
            P.reduce(summ[:, c, d, 0:1], a, ALU.mult)
    P.add("sp", lambda e: e.dma_start(out=o_sum, in_=summ.ap), reads=[summ], writes=[outb], dma=True)
    P.emit(final_reads=[outb])
    return nc, P


def build_L4():
    nc = bass.Bass("TRN2", target_bir_lowering=False)
    dr = {}
    dr["cf"] = din(nc, "cf", [128, 384])
    dr["cb"] = din(nc, "cb", [128, 256])
    dr["u"] = din(nc, "u", [8, 128, TOK])
    dr["uhp"] = din(nc, "uhp", [8, 128, 3])
    dr["uhn"] = din(nc, "uhn", [8, 128, 3])
    dr["yg"] = din(nc, "yg", [8, 128, TOK], BF16)
    dr["x2"] = din(nc, "x2", [8, 128, TOK])
    dr["summ"] = din(nc, "summ", [4, 128, 8, 2, 3])
    dr["jmask"] = din(nc, "jmask", [128, 2, 4])
    dr["mods"] = din(nc, "mods", [2, 128, 48, 2])
    dr["nffnT"] = din(nc, "nffnT", [128, 8])
    dr["w_out"] = din(nc, "w_out", [D, D])
    lru_drams(nc, dr)
    moe_drams(nc, dr)
    o_out = dout(nc, "o_out", [8, 128, TOK])
    P = Prog(nc)
    C = Ctx(nc, P, dr)
    outb = T(None, [Buf()])
    prm = lru_params(C, dr)
    mods1 = P.sb([128, 48, 2], F32, "mods1")
    P.dma(mods1, D_(dr["mods"][1]))
    nffn = P.sb([128, 8], F32, "nffn")
    P.dma(nffn, D_(dr["nffnT"]))
    _, _, G1 = mod_scalars(C, mods1, nffn, 0, 0, "m1l")
    A2, Sh2, G2 = mod_scalars(C, mods1, nffn, 1, 0, "f1l")
    sm = P.sb([128, 4, 8, 2, 3], F32, "sm")
    for j in range(4):
        P.dma(sm[:, j], D_(dr["summ"][j]))
    jm = P.sb([128, 2, 4], F32, "jm")
    P.dma(jm, D_(dr["jmask"]))
    h0 = P.sb([128, 8, 2], F32, "h0")
    cand = P.sb([128, 8], F32, "cand")
    for d in range(2):
        P.copy(h0[:, :, d], sm[:, 0, :, d, 2])
        order = range(4) if d == 0 else range(3, -1, -1)
        for j in order:
            P.tt(cand, sm[:, j, :, d, 0], h0[:, :, d], ALU.mult)
            P.tt(cand, cand, sm[:, j, :, d, 1], ALU.add)
            P.tt(cand, cand, h0[:, :, d], ALU.subtract)
            P.stt(h0[:, :, d], cand, jm[:, d, j:j + 1], h0[:, :, d], ALU.mult, ALU.add)
    xres = P.sb([128, 8, TOK], F32, "xres")
    P.dma(xres, D_(dr["x2"].rearrange("c p t -> p c t")))
    m_mix = P.mark()
    w_out = P.sb([128, 8, D], BF16, "w_out")
    for kc in range(8):
        P.dma(w_out[:, kc, :], D_(dr["w_out"][kc * 128:(kc + 1) * 128, :]), q="pool")
    mix = P.sb([128, 8, TOK], BF16, "mix")
    m_sc = P.mark()
    for c in range(8):
        ub = load_ub(C, dr["u"], dr["uhp"], dr["uhn"], c, TOK, "ub", nb=1)
        yg = P.tmp("ygl", [128, TOK], BF16, n=1)
        P.dma(yg, D_(dr["yg"][c]))
        hs = []
        for d in range(2):
            a, bb = lru_coeffs(C, prm, ub, TOK, c, d, nb=1)
            h = P.tmp("l_h", [128, 2048], F32)
            lru_scan(C, a, bb, TOK, d, h0[:, c, d:d + 1], h)
            hs.append(h)
        P.tt(hs[0], hs[0], hs[1], ALU.add)
        P.tt(mix[:, c, :], hs[0], yg, ALU.mult)
    P.release(m_sc)
    tiles = [(lambda c, t=t: xres[:, c, t * 512:(t + 1) * 512], 512, False) for t in range(TOK // 512)]
    for ti, (xf, W, _) in enumerate(tiles):
        for dc in range(8):
            ps = P.ps()
            for kc in range(8):
                P.mm(ps[:, 0:W], w_out[:, kc, dc * 128:(dc + 1) * 128], mix[:, kc, ti * 512:(ti + 1) * 512],
                     start=(kc == 0), stop=(kc == 7))
            P.stt(xf(dc), ps[:, 0:W], G1[:, dc:dc + 1], xf(dc), ALU.mult, ALU.add)
    P.release(m_mix)
    moe_layer(C, dr, 0, tiles, A2, Sh2, G2)
    P.add("sp", lambda e: e.dma_start(out=o_out.rearrange("c p t -> p c t"), in_=xres.ap), reads=[xres], writes=[outb], dma=True)
    P.emit(final_reads=[outb])
    return nc, P


def lru_inputs(inp):
    cw = np.ascontiguousarray(inp["od_conv_w"][0].reshape(2, 4, 8, 128).transpose(3, 2, 0, 1))

    def v(a):
        return np.ascontiguousarray(a.reshape(2, 8, 128).transpose(2, 1, 0))

    def w(a):
        return np.ascontiguousarray(a.transpose(2, 0, 1, 3))
    return dict(l_cw=cw, l_cb=v(inp["od_conv_b"][0]), l_ba=v(inp["od_b_a"][0]), l_bx=v(inp["od_b_x"][0]),
                l_lam=v(inp["od_lam"][0]), l_wa=w(inp["od_w_a"][0]), l_wx=w(inp["od_w_x"][0]))


def halo_inputs(r, o2):
    b, j = r // 4, r % 4
    z = np.zeros((8, 128, 3), np.float32)
    hp = np.ascontiguousarray(o2[r - 1]["o_u"][:, :, TOK - 3:TOK]) if j > 0 else z
    hn = np.ascontiguousarray(o2[r + 1]["o_u"][:, :, 0:3]) if j < 3 else z
    return hp, hn


def kernel_unfused(**inp):
    inp = {k: np.asarray(v) for k, v in inp.items()}
    cf, cb = host_consts()
    cosT, sinT = rope_tables()
    cores = list(range(NCORE))
    nc1, _ = _get("L1", build_L1)
    ims1 = [l1_inputs(r, inp, cf, cb, cosT, sinT) for r in cores]
    o1 = run_bass_kernel_spmd(nc1, ims1, core_ids=cores).results
    xo_list = [im["xo"] for im in ims1]
    nc2, _ = _get("L2", build_L2)
    ims2 = [l2_inputs(r, inp, o1, cf, cb, xo_list) for r in cores]
    o2 = run_bass_kernel_spmd(nc2, ims2, core_ids=cores).results
    del ims1, ims2
    lin = lru_inputs(inp)
    nc3, _ = _get("L3", build_L3)
    ims3 = []
    for r in cores:
        hp, hn = halo_inputs(r, o2)
        d = dict(cf=cf, cb=cb, u=o2[r]["o_u"], uhp=hp, uhn=hn, uc=o2[r]["o_uc"])
        d.update(lin)
        ims3.append(d)
    o3 = run_bass_kernel_spmd(nc3, ims3, core_ids=cores).results
    nc4, _ = _get("L4", build_L4)
    ims4 = []
    for r in cores:
        b, j = r // 4, r % 4
        hp, hn = halo_inputs(r, o2)
        summ = np.ascontiguousarray(np.stack([o3[4 * b + jj]["o_sum"] for jj in range(4)], axis=0))
        jm = np.zeros((128, 2, 4), np.float32)
        for jj in range(4):
            jm[:, 0, jj] = 1.0 if jj < j else 0.0
            jm[:, 1, jj] = 1.0 if jj > j else 0.0
        d = dict(cf=cf, cb=cb, u=o2[r]["o_u"], uhp=hp, uhn=hn, yg=o2[r]["o_yg"], x2=o2[r]["o_x2"], summ=summ, jmask=jm,
                 mods=o1[r]["o_mods"], nffnT=chunkT(inp["norm_ffn"][1]), w_out=inp["od_w_out"][0])
        d.update(lin)
        d.update(moe_inputs(inp, 1))
        ims4.append(d)
    o4 = run_bass_kernel_spmd(nc4, ims4, core_ids=cores).results
    out = np.empty((2, SEQ, D), np.float32)
    for r in cores:
        b, j = r // 4, r % 4
        out[b, j * TOK:(j + 1) * TOK, :] = o4[r]["o_out"].reshape(D, TOK).T
    return out


GROUPS = [[0, 1, 2, 3], [4, 5, 6, 7]]
HALF = 512


def qk_stage_a(C, ps_in, W, gain, rope, out):
    P = C.P
    k_sb = P.tmp(f"qk_k{W}", [128, W], F32, n=2)
    P.copy(k_sb, ps_in, eng="act")
    sq = P.tmp(f"qk_sq{W}", [128, W], BF16)
    P.act(sq, ps_in, AF.Square)
    ps2 = P.ps()
    P.mm(ps2[:, 0:W], C.bones, sq)
    rs = C.rstd_from_ps(ps2[:, 0:W], W, 1.0 / 64.0, name="qk_rs", n=2)
    if not rope:
        P.stt(out, k_sb, gain, rs, ALU.mult, ALU.mult)
        return None
    kh = P.tmp(f"qk_kh{W}", [128, W], F32, n=2)
    P.stt(kh, k_sb, gain, rs, ALU.mult, ALU.mult)
    ps3 = P.ps()
    P.mm(ps3[:, 0:W], C.perm, kh)
    return dict(kh=kh, ps3=ps3, out=out)


def qk_stage_b(C, st, W, cos, sin):
    P = C.P
    t1 = P.tmp(f"qk_t1{W}", [128, W], F32, n=2)
    P.tt(t1, st["kh"], cos, ALU.mult)
    t2 = P.tmp(f"qk_t2{W}", [128, W], F32, n=2)
    P.tt(t2, st["ps3"][:, 0:W], sin, ALU.mult)
    P.tt(st["out"], t1, t2, ALU.add)


def build_fused():
    nc = bass.Bass("TRN2", target_bir_lowering=False)
    dr = {}
    dr["cf"] = din(nc, "cf", [128, 384])
    dr["cb"] = din(nc, "cb", [128, 256])
    dr["xo"] = din(nc, "xo", [D, TOK])
    dr["xh"] = din(nc, "xh", [D, 2])
    dr["hmask"] = din(nc, "hmask", [128, 2])
    dr["ctx"] = din(nc, "ctx", [D, CTXL])
    dr["cvec"] = din(nc, "cvec", [128, 8, 2])
    dr["ada_wq"] = din(nc, "ada_wq", [2, D, 1536])
    dr["ada_bq"] = din(nc, "ada_bq", [128, 2, 12])
    dr["nmixT"] = din(nc, "nmixT", [2, 128, 8])
    dr["nffnT"] = din(nc, "nffnT", [2, 128, 8])
    dr["w_in"] = din(nc, "w_in", [D, 3072])
    dr["convw"] = din(nc, "convw", [128, 4, 3])
    dr["convb"] = din(nc, "convb", [128, 4])
    dr["qkn"] = din(nc, "qkn", [128, 2])
    dr["cos"] = din(nc, "cos", [128, TOK])
    dr["sin"] = din(nc, "sin", [128, TOK])
    dr["lamp"] = din(nc, "lamp", [64, 4])
    dr["subn"] = din(nc, "subn", [128, 1])
    dr["w_out"] = din(nc, "w_out", [D, D])
    dr["od_w_in"] = din(nc, "od_w_in", [D, 2048])
    dr["od_w_out"] = din(nc, "od_w_out", [D, D])
    dr["jmask"] = din(nc, "jmask", [128, 2, 4])
    dr["hsel"] = din(nc, "hsel", [128, 2, 4])
    lru_drams(nc, dr)
    dr["moe_wr"] = din(nc, "moe_wr", [2, D, 20])
    dr["moe_rb"] = din(nc, "moe_rb", [2, 128, 20])
    dr["sel"] = din(nc, "sel", [16, 16, 128])
    dr["moe_wg"] = din(nc, "moe_wg", [2, 16, D, 512])
    dr["moe_wu"] = din(nc, "moe_wu", [2, 16, D, 512])
    dr["moe_wd"] = din(nc, "moe_wd", [2, 16, 512, D])
    o_out = dout(nc, "o_out", [8, 128, TOK])
    kv_in = [nc.dram_tensor(f"kv_in{i}", [1024, 512], BF16).ap() for i in range(4)]
    kv_all = [nc.dram_tensor(f"kv_all{i}", [4 * 1024, 512], BF16).ap() for i in range(4)]
    hx_in = nc.dram_tensor("hx_in", [128, 48], F32).ap()
    hx_all = nc.dram_tensor("hx_all", [4 * 128, 48], F32).ap()
    sm_in = nc.dram_tensor("sm_in", [128, 48], F32).ap()
    sm_all = nc.dram_tensor("sm_all", [4 * 128, 48], F32).ap()
    kv_in_t = [T(kv_in[i], [Buf()]) for i in range(4)]
    kv_all_t = [T(kv_all[i], [Buf()]) for i in range(4)]
    hx_in_t, hx_all_t = T(hx_in, [Buf()]), T(hx_all, [Buf()])
    sm_in_t, sm_all_t = T(sm_in, [Buf()]), T(sm_all, [Buf()])
    mq_in = nc.dram_tensor("mq_in", [128, 48], F32).ap()
    mq_all = nc.dram_tensor("mq_all", [4 * 128, 48], F32).ap()
    mq_in_t, mq_all_t = T(mq_in, [Buf()]), T(mq_all, [Buf()])

    P = Prog(nc)
    C = Ctx(nc, P, dr)
    outb = T(None, [Buf()])
    LAM_INIT = 0.8 - 0.6 * math.exp(-0.3 * 0)

    def allgather(src_t, dst_t):
        P.add("pool", lambda e: e.collective_compute("AllGather", ALU.bypass, replica_groups=GROUPS,
                                                     ins=[src_t.ap.opt()], outs=[dst_t.ap.opt()]),
              reads=[src_t], writes=[dst_t], cc=True)

    cvec = P.sb([128, 8, 2], F32, "cvec")
    P.dma(cvec, D_(dr["cvec"]))
    mods = [P.sb([128, 48, 2], F32, f"mods{l}") for l in range(2)]
    m_md = P.mark()
    scv = P.sb([128, 8, 2], F32, "silu_c")
    P.act(scv, cvec, AF.Silu)
    bq = P.sb([128, 2, 12], F32, "bq")
    P.dma(bq, D_(dr["ada_bq"]))
    psm = P.ps(hold=True)
    for l in range(2):
        for hf_ in range(2):
            wq = P.tmp("adawq", [128, 8, 768], F32, n=2)
            P.dma(wq, D_(dr["ada_wq"][l].rearrange("(kc p) f -> p kc f", p=128)[:, :, hf_ * 768:(hf_ + 1) * 768]))
            for fc in range(6):
                g = l * 12 + hf_ * 6 + fc
                for kc in range(8):
                    P.mm(psm[:, 2 * g:2 * g + 2], wq[:, kc, fc * 128:(fc + 1) * 128], scv[:, kc, :],
                         start=(kc == 0), stop=(kc == 7))
    mq = P.sb([128, 2, 12, 2], F32, "mq")
    P.tt(mq, psm[:, 0:48].re("p (l g c) -> p l g c", l=2, c=2),
         T(bq.ap.unsqueeze(3).to_broadcast([128, 2, 12, 2]), bq.bufs), ALU.add)
    P.ps_free(psm)
    P.dma(mq_in_t, mq.re("p l g c -> p (l g c)"))
    allgather(mq_in_t, mq_all_t)
    mqa = P.sb([128, 4, 48], F32, "mqa")
    P.dma(mqa, T(mq_all.rearrange("(r p) f -> p r f", p=128), mq_all_t.bufs))
    for l in range(2):
        P.copy(mods[l].re("p (r i) c -> p r i c", r=4), mqa[:, :, l * 24:(l + 1) * 24].re("p r (i c) -> p r i c", c=2))
    P.release(m_md)
    nmix = [P.sb([128, 8], F32, f"nmix{l}") for l in range(2)]
    nffn = [P.sb([128, 8], F32, f"nffn{l}") for l in range(2)]
    for l in range(2):
        P.dma(nmix[l], D_(dr["nmixT"][l]))
        P.dma(nffn[l], D_(dr["nffnT"][l]))
    A_lat, Sh_lat, G1 = mod_scalars(C, mods[0], nmix[0], 0, 0, "a0l")
    A_ctx, Sh_ctx, G1c = mod_scalars(C, mods[0], nmix[0], 0, 1, "a0c")
    A2, Sh2, G2 = mod_scalars(C, mods[0], nffn[0], 1, 0, "f0l")
    A2c, Sh2c, G2c = mod_scalars(C, mods[0], nffn[0], 1, 1, "f0c")
    A1n, Sh1n, G1n = mod_scalars(C, mods[1], nmix[1], 0, 0, "a1l")
    A1nc, Sh1nc, _ = mod_scalars(C, mods[1], nmix[1], 0, 1, "a1c")
    A2n, Sh2n, G2n = mod_scalars(C, mods[1], nffn[1], 1, 0, "f1l")
    convw = P.sb([128, 4, 3], F32, "convw")
    P.dma(convw, D_(dr["convw"]))
    convb = P.sb([128, 4], F32, "convb")
    P.dma(convb, D_(dr["convb"]))
    qkn = P.sb([128, 2], F32, "qkn")
    P.dma(qkn, D_(dr["qkn"]))
    hmask = P.sb([128, 2], F32, "hmask")
    P.dma(hmask, D_(dr["hmask"]))
    jm = P.sb([128, 2, 4], F32, "jm")
    P.dma(jm, D_(dr["jmask"]))
    hsel = P.sb([128, 2, 4], F32, "hsel")
    P.dma(hsel, D_(dr["hsel"]))
    lamp = P.sb([64, 4], F32, "lamp")
    P.dma(lamp, D_(dr["lamp"]))
    lpr = P.sb([64, 2], F32, "lpr")
    P.tt(lpr[:, 0:1], lamp[:, 0:1], lamp[:, 1:2], ALU.mult)
    P.tt(lpr[:, 1:2], lamp[:, 2:3], lamp[:, 3:4], ALU.mult)
    psl = P.ps()
    P.mm(psl[:, 0:2], C.onesf[0:64, :], lpr)
    lex = P.sb([128, 2], F32, "lex")
    P.act(lex, psl[:, 0:2], AF.Exp)
    nlam = P.sb([128, 1], F32, "nlam")
    P.tt(nlam, lex[:, 1:2], lex[:, 0:1], ALU.subtract)
    P.ts(nlam, nlam, -LAM_INIT, None, ALU.add)
    subn = P.sb([128, 1], F32, "subn")
    P.dma(subn, D_(dr["subn"]))
    P.ts(subn, subn, 1.0 - LAM_INIT, None, ALU.mult)

    m_base = P.mark()
    mix = P.sb([128, 4, TOK], BF16, "mixa")
    mixc = P.sb([128, 4, CTXL], BF16, "mixca")
    qT = P.sb([128, 4, TOK], BF16, "qT")
    qcT = P.sb([128, 4, CTXL], BF16, "qcT")
    kcT = P.sb([128, 4, CTXL], BF16, "kcT")
    vc = P.sb([128, 2, 512], BF16, "vc")
    m_front = P.mark()
    cbs = P.sb([128, 4, TOK], BF16, "cbs")
    ub = P.sb([128, 4, TOK + 2], BF16, "ub")
    cbc = P.sb([128, 4, CTXL], BF16, "cbc")
    ubc = P.sb([128, 4, CTXL + 2], BF16, "ubc")
    P.memset(ubc[:, :, 0:1], 0.0)
    P.memset(ubc[:, :, CTXL + 1:CTXL + 2], 0.0)
    w_in = P.sb([128, 8, 3072], BF16, "w_in")
    for kc in range(8):
        P.dma(w_in[:, kc, :], D_(dr["w_in"][kc * 128:(kc + 1) * 128, :]), q="pool")

    def proj(h, W, f):
        ps = P.ps()
        for kc in range(8):
            P.mm(ps[:, 0:W], w_in[:, kc, f * 128:(f + 1) * 128], h[:, kc, 0:W], start=(kc == 0), stop=(kc == 7))
        return ps[:, 0:W]

    vin_v = [kv_in[t][512:1024, :].rearrange("(h p) (k v) -> p k h v", p=128, v=128) for t in range(4)]

    def front_norm(xsrc, W, A, Sh):
        xt = P.tmp(f"xt{W}", [128, 8, W], F32, n=1)
        P.dma(xt, D_(xsrc.rearrange("(c p) t -> p c t", p=128)))
        h = P.tmp(f"h{W}", [128, 8, W], BF16, n=2)
        norm_mod(C, xt, W, A, Sh, h)
        return h

    def front_tile(xsrc, W, A, Sh, rope, dst, pre_h=None, mid_hook=None):
        t0 = dst["t0"]
        if rope:
            cs_c = P.tmp("cos_t", [128, W], F32, n=1)
            cs_s = P.tmp("sin_t", [128, W], F32, n=1)
            P.dma(cs_c, D_(dr["cos"][:, t0:t0 + W]))
            P.dma(cs_s, D_(dr["sin"][:, t0:t0 + W]))
        h = pre_h if pre_h is not None else front_norm(xsrc, W, A, Sh)
        if dst.get("cb") is not None:
            for c in range(4):
                ps = proj(h, W, c)
                P.copy(dst["cb"][:, c, t0:t0 + W], ps, eng="act")
        for c in range(4):
            ps_cc = proj(h, W, 4 + c)
            cc = P.tmp(f"cc{W}", [128, W], F32)
            P.copy(cc, ps_cc, eng="act")
            ps_cx = proj(h, W, 8 + c)
            P.tt(dst["u"](c), cc, ps_cx, ALU.mult)
        if mid_hook is not None:
            mid_hook()
        if dst.get("q") is None:
            return
        hu = [(12 + hh, 0, hh) for hh in range(4)] + [(16 + hh, 1, hh) for hh in range(4)]

        def unit_out(kind, hh):
            if kind == 0:
                return dst["q"][:, hh, t0:t0 + W], None
            if dst.get("k") is not None:
                return dst["k"][:, hh, t0:t0 + W], None
            kt_ = P.tmp("kt_", [128, W], BF16, n=3)
            return kt_, hh

        def finish(o, hh_store):
            if hh_store is not None:
                nb_ = Buf()
                kv_in_t[t0 // 512].bufs.append(nb_)
                P.add("sp", lambda e, o=o, hh=hh_store, t0=t0: e.dma_start(out=kv_in[t0 // 512][hh * 128:(hh + 1) * 128, :], in_=o.ap),
                      reads=[o], writes=[T(None, [nb_])], dma=True)

        ps_next = proj(h, W, hu[0][0])
        prev = None
        for i, (f_, kind, hh) in enumerate(hu):
            ps_cur = ps_next
            if i + 1 < len(hu):
                ps_next = proj(h, W, hu[i + 1][0])
            o, hs_ = unit_out(kind, hh)
            st = qk_stage_a(C, ps_cur, W, qkn[:, kind:kind + 1], rope, o)
            if prev is not None:
                qk_stage_b(C, prev[0], W, cs_c, cs_s)
                finish(prev[1], prev[2])
            if st is None:
                finish(o, hs_)
                prev = None
            else:
                prev = (st, o, hs_)
        if prev is not None:
            qk_stage_b(C, prev[0], W, cs_c, cs_s)
            finish(prev[1], prev[2])
        for sub in range(W // 128):
            ps = P.ps()
            for kc in range(8):
                P.mm(ps, h[:, kc, sub * 128:(sub + 1) * 128], w_in[:, kc, 2560:3072], start=(kc == 0), stop=(kc == 7))
            if dst.get("v") is not None:
                P.copy(dst["v"][:, sub, :], ps, eng="act")
            else:
                vs = P.tmp("vs", [128, 512], BF16, n=2)
                P.copy(vs, ps, eng="act")
                nb_ = Buf()
                kv_in_t[t0 // 512].bufs.append(nb_)
                P.add("sp", lambda e, vs=vs, sub=sub, t0=t0: e.dma_start(
                    out=vin_v[t0 // 512][:, sub], in_=vs.ap.rearrange("p (h v) -> p h v", v=128)),
                    reads=[vs], writes=[T(None, [nb_])], dma=True)

    def conv_piece(ubuf, cbt, s0, w, dstm):
        for c in range(4):
            acc = P.tmp("cacc", [128, 512], F32, n=1)[:, 0:w]
            P.ts(acc, ubuf[:, c, 1 + s0:1 + s0 + w], convw[:, c, 1:2], convb[:, c:c + 1], ALU.mult, ALU.add)
            P.stt(acc, ubuf[:, c, s0:s0 + w], convw[:, c, 0:1], acc, ALU.mult, ALU.add)
            P.stt(acc, ubuf[:, c, 2 + s0:2 + s0 + w], convw[:, c, 2:3], acc, ALU.mult, ALU.add)
            P.tt(dstm[:, c, s0:s0 + w], acc, cbt[:, c, s0:s0 + w], ALU.mult)

    uh = P.sb([128, 4, 2], F32, "uh")
    m_own = P.mark()
    front_tile(dr["ctx"], CTXL, A_ctx, Sh_ctx, False,
               dict(cb=cbc, u=lambda c: ubc[:, c, 1:1 + CTXL], q=qcT, k=kcT, v=vc, t0=0))
    P.release(m_own)
    nxt_h = [front_norm(dr["xo"][:, 0:512], 512, A_lat, Sh_lat)]
    for t in range(TOK // 512):
        cur_h = nxt_h[0]

        def hook(t=t):
            if t + 1 < TOK // 512:
                nxt_h[0] = front_norm(dr["xo"][:, (t + 1) * 512:(t + 2) * 512], 512, A_lat, Sh_lat)
        front_tile(dr["xo"][:, t * 512:(t + 1) * 512], 512, A_lat, Sh_lat, True,
                   dict(cb=cbs, u=lambda c, t=t: ub[:, c, 1 + t * 512:1 + (t + 1) * 512], q=qT, k=None, v=None, t0=t * 512),
                   pre_h=cur_h, mid_hook=hook)
        allgather(kv_in_t[t], kv_all_t[t])
        if t == 1:
            front_tile(dr["xh"], 2, A_lat, Sh_lat, False, dict(u=lambda c: uh[:, c, :], t0=0))
            for c in range(4):
                P.tt(ub[:, c, 0:1], uh[:, c, 0:1], hmask[:, 0:1], ALU.mult)
                P.tt(ub[:, c, TOK + 1:TOK + 2], uh[:, c, 1:2], hmask[:, 1:2], ALU.mult)
        if t >= 1:
            conv_piece(ub, cbs, (t - 1) * 512, 512, mix)

    conv_piece(ub, cbs, TOK - 512, 512, mix)
    conv_piece(ubc, cbc, 0, CTXL, mixc)
    P.release(m_front)

    top0 = P.top
    xres = P.sb_top([128, 8, TOK], F32, "xres")
    top_x = P.top
    xc = P.sb_top([128, 8, CTXL], F32, "xc")
    xc_off = P.top
    w_out = P.sb([128, 8, D], BF16, "w_out")
    for kc in range(8):
        P.dma(w_out[:, kc, :], D_(dr["w_out"][kc * 128:(kc + 1) * 128, :]), q="pool")
    mixb = P.sb([128, 4, TOK], BF16, "mixb")
    mixcb = P.sb([128, 4, CTXL], BF16, "mixcb")
    m_att = P.mark()
    SB = [P.psum_banks[i] for i in range(4)]
    ACC = [(P.psum_banks[4], P.psum_banks[6]), (P.psum_banks[5], P.psum_banks[7])]
    P.held.update(range(8))
    sctr = [0]

    def attn_group(ksrc, vsrc, qsrc, W, kts, m):
        accO, accL = ACC[m]
        lo, hi = m * 64, (m + 1) * 64
        n = len(kts)

        qz = P.tmp("qz", [128, 512], BF16, n=1)[:, 0:W]
        P.memset(qz, 0.0, eng="pool")
        P.copy(qz[lo:hi, :], qsrc[lo:hi, :], eng="pool")

        def S_(i):
            sb_ = SB[sctr[0] % 4]
            sctr[0] += 1
            P.mm(sb_[:, 0:W], ksrc(kts[i]), qz)
            return sb_

        LOOK = 2
        pend_pt = []
        first_acc = [True]
        sq_ = [S_(i) for i in range(min(LOOK, n))]
        for i in range(n):
            if i + LOOK < n:
                sq_.append(S_(i + LOOK))
            cur = sq_[i]
            pt = P.tmp("pt", [128, 512], BF16, n=6)[:, 0:W]
            P.act(pt, cur[:, 0:W], AF.Exp, scale=0.125)
            P.mm(accO[:, 0:W], vsrc(kts[i]), pt, start=(i == 0), stop=(i == n - 1))
            pend_pt.append(pt)
            if len(pend_pt) == 4 or i == n - 1:
                lvl = list(pend_pt)
                pend_pt = []
                while len(lvl) > 2:
                    nl = []
                    for j2 in range(0, len(lvl) - 1, 2):
                        pp = P.tmp("pp", [128, 512], BF16, n=4)[:, 0:W]
                        P.tt(pp, lvl[j2], lvl[j2 + 1], ALU.add)
                        nl.append(pp)
                    if len(lvl) % 2 == 1:
                        nl.append(lvl[-1])
                    lvl = nl
                if first_acc[0]:
                    lacc = P.tmp("lacc", [128, 512], F32, n=1)[:, 0:W]
                    if len(lvl) == 2:
                        P.tt(lacc, lvl[0], lvl[1], ALU.add)
                    else:
                        P.copy(lacc, lvl[0])
                    first_acc[0] = False
                else:
                    if len(lvl) == 2:
                        pp = P.tmp("pp", [128, 512], BF16, n=4)[:, 0:W]
                        P.tt(pp, lvl[0], lvl[1], ALU.add)
                        lvl = [pp]
                    P.tt(lacc, lacc, lvl[0], ALU.add)
        P.mm(accL[:, 0:W], C.onesf, lacc)
        return accO, accL

    def attn_tile(ksrc, vsrc, qsrc, W, kts, dst):
        om = []
        for m in range(2):
            accO, accL = attn_group(ksrc, vsrc, qsrc, W, kts, m)
            rl = P.tmp("rl", [128, 512], F32, n=1)[:, 0:W]
            P.act(rl, accL[:, 0:W], AF.Ln)
            P.act(rl, rl, AF.Exp, scale=-1.0)
            o = P.tmp("om", [128, 512], F32, n=2)[:, 0:W]
            P.tt(o, accO[:, 0:W], rl, ALU.mult)
            om.append(o)
        o = P.tmp("od", [128, 512], F32, n=1)[:, 0:W]
        P.stt(o, om[1], nlam, om[0], ALU.mult, ALU.add)
        sq = P.tmp("asq", [128, 512], BF16, n=1)[:, 0:W]
        P.act(sq, o, AF.Square)
        ps = SB[sctr[0] % 4]
        sctr[0] += 1
        P.mm(ps[:, 0:W], C.ones, sq)
        rstd = P.tmp("arstd", [128, 512], F32, n=1)[:, 0:W]
        P.act(rstd, ps[:, 0:W], AF.Ln, bias=C.epsb, scale=1.0 / 128.0)
        P.act(rstd, rstd, AF.Exp, scale=-0.5)
        P.stt(dst, o, subn, rstd, ALU.mult, ALU.mult)

    kall_v = [kv_all[t].rearrange("(r s p) c -> s p r c", r=4, p=128) for t in range(4)]
    vall_v = [kv_all[t].rearrange("(r s p) (k v) -> s p r k v", r=4, p=128, v=128) for t in range(4)]
    NK0 = SEQ // 128
    NK0 = SEQ // 128
    KORDER = [r * 16 + t * 4 + kk for t in range(4) for r in range(4) for kk in range(4)] + [NK0, NK0 + 1]
    for hh in range(4):
        kTp = [P.tmp(f"kTh{t}", [128, 4, 512], BF16, n=1) for t in range(4)]
        vhp = [P.tmp(f"vh{t}", [128, 4, 4, 128], BF16, n=1) for t in range(4)]
        for t in range(4):
            P.dma(kTp[t], T(kall_v[t][hh], kv_all_t[t].bufs))
            P.dma(vhp[t], T(vall_v[t][4 + hh], kv_all_t[t].bufs))
        if hh == 0:
            P.dma(xres, D_(dr["xo"].rearrange("(c p) t -> p c t", p=128)))
            P.dma(xc, D_(dr["ctx"].rearrange("(c p) t -> p c t", p=128)))

        def ksrc(kt, hh=hh, kTp=kTp):
            if kt >= NK0:
                return kcT[:, hh, (kt - NK0) * 128:(kt - NK0 + 1) * 128]
            r, t, kk = kt // 16, (kt % 16) // 4, kt % 4
            return kTp[t][:, r, kk * 128:(kk + 1) * 128]

        def vsrc(kt, hh=hh, vhp=vhp):
            if kt >= NK0:
                return vc[:, kt - NK0, hh * 128:(hh + 1) * 128]
            r, t, kk = kt // 16, (kt % 16) // 4, kt % 4
            return vhp[t][:, r, kk, :]

        for qt in range(TOK // 512):
            attn_tile(ksrc, vsrc, qT[:, hh, qt * 512:(qt + 1) * 512], 512, KORDER,
                      mixb[:, hh, qt * 512:(qt + 1) * 512])
        attn_tile(ksrc, vsrc, qcT[:, hh, :], CTXL, [NKT - 2, NKT - 1], mixcb[:, hh, :])
    P.held.clear()
    P.ps_i = 0
    P.release(m_att)

    tiles = [(lambda c, t=t: xres[:, c, t * 512:(t + 1) * 512], 512, False) for t in range(TOK // 512)]
    tiles_c = tiles + [(lambda c: xc[:, c, :], CTXL, True)]
    for ti, (xf, W, is_ctx) in enumerate(tiles_c):
        srcs = (mixc, mixcb) if is_ctx else (mix[:, :, ti * 512:(ti + 1) * 512], mixb[:, :, ti * 512:(ti + 1) * 512])
        G = G1c if is_ctx else G1
        for dc in range(8):
            ps = P.ps()
            for kc in range(8):
                P.mm(ps[:, 0:W], w_out[:, kc, dc * 128:(dc + 1) * 128], srcs[kc // 4][:, kc % 4, :], start=(kc == 0), stop=(kc == 7))
            P.stt(xf(dc), ps[:, 0:W], G[:, dc:dc + 1], xf(dc), ALU.mult, ALU.add)
    P.release(m_base)
    moe_layer(C, dr, 0, tiles_c, A2, Sh2, G2, A2c, Sh2c, G2c)

    yg = P.sb_top([128, 8, TOK], BF16, "yg")
    uu = P.sb_top([128, 8, TOK + 6], BF16, "uu")
    ucx = T(nc.alloc_sbuf_tensor_at("ucx_alias", [128, 8, CTXL + 6], BF16, offset=xc_off).ap(), xc.bufs)
    m_l1 = P.mark()
    w1 = P.sb([128, 8, 2048], BF16, "w1in")
    for kc in range(8):
        P.dma(w1[:, kc, :], D_(dr["od_w_in"][kc * 128:(kc + 1) * 128, :]), q="pool")
    def l1_norm(ti):
        xf, W, is_ctx = tiles_c[ti]
        A, Sh = (A1nc, Sh1nc) if is_ctx else (A1n, Sh1n)
        h1 = P.tmp(f"h1_{W}", [128, 8, W], BF16, n=(1 if is_ctx else 2))
        ps = P.ps()
        for c in range(8):
            s_ = P.tmp(f"sq{W}", [128, W], BF16)
            P.act(s_, xf(c), AF.Square)
            P.mm(ps[:, 0:W], C.ones, s_, start=(c == 0), stop=(c == 7))
        rstd = C.rstd_from_ps(ps[:, 0:W], W, 1.0 / D)
        for c in range(8):
            t = P.tmp(f"nt{W}", [128, W], F32)
            P.tt(t, xf(c), rstd, ALU.mult)
            P.act(h1[:, c, :], t, AF.Identity, bias=Sh[:, c:c + 1], scale=A[:, c:c + 1])
        return h1

    def l1_proj(ti, h1):
        xf, W, is_ctx = tiles_c[ti]
        for f in range(16):
            if is_ctx and f < 8:
                continue
            ps = P.ps()
            for kc in range(8):
                P.mm(ps[:, 0:W], w1[:, kc, f * 128:(f + 1) * 128], h1[:, kc, :], start=(kc == 0), stop=(kc == 7))
            if f < 8:
                P.act(yg[:, f, ti * 512:(ti + 1) * 512], ps[:, 0:W], AF.Gelu_apprx_tanh)
            elif is_ctx:
                P.copy(ucx[:, f - 8, 3:3 + CTXL], ps[:, 0:W], eng="act")
            else:
                P.copy(uu[:, f - 8, 3 + ti * 512:3 + (ti + 1) * 512], ps[:, 0:W], eng="act")

    h1n = l1_norm(0)
    for ti in range(len(tiles_c)):
        h1c = h1n
        if ti + 1 < len(tiles_c):
            h1n = l1_norm(ti + 1)
        l1_proj(ti, h1c)
    P.memset(ucx[:, :, 0:3], 0.0)
    P.memset(ucx[:, :, CTXL + 3:CTXL + 6], 0.0)
    P.release(m_l1)

    prm = lru_params(C, dr)
    hs1 = P.sb([128, 8, 2], F32, "hs1")
    P.ts(hs1, prm["s1"], 0.5, None, ALU.mult)
    hba = P.sb([128, 8, 2], F32, "hba")
    P.ts(hba, prm["l_ba"], 0.5, None, ALU.mult)
    hbx = P.sb([128, 8, 2], F32, "hbx")
    P.ts(hbx, prm["l_bx"], 0.5, None, ALU.mult)
    qtr = P.sb([128, 1], F32, "qtr")
    P.memset(qtr, 0.25)
    hx = P.sb([128, 8, 6], F32, "hx")
    P.copy(hx[:, :, 0:3], uu[:, :, 3:6])
    P.copy(hx[:, :, 3:6], uu[:, :, TOK:TOK + 3])
    P.dma(hx_in_t, hx.re("p c k -> p (c k)"))
    allgather(hx_in_t, hx_all_t)
    hxa = P.sb([128, 4, 8, 6], F32, "hxa")
    P.dma(hxa.re("p r c k -> p r (c k)"), T(hx_all.rearrange("(r p) f -> p r f", p=128), hx_all_t.bufs))
    halo = P.sb([128, 8, 6], F32, "halo")
    P.memset(halo, 0.0)
    for j in range(4):
        P.stt(halo[:, :, 0:3], hxa[:, j, :, 3:6], hsel[:, 0, j:j + 1], halo[:, :, 0:3], ALU.mult, ALU.add)
        P.stt(halo[:, :, 3:6], hxa[:, j, :, 0:3], hsel[:, 1, j:j + 1], halo[:, :, 3:6], ALU.mult, ALU.add)
    P.copy(uu[:, :, 0:3], halo[:, :, 0:3])
    P.copy(uu[:, :, TOK + 3:TOK + 6], halo[:, :, 3:6])

    PW = HALF

    cur_dg = [None, None]

    def get_diag(c):
        if cur_dg[0] != c:
            dg = P.tmp("l_dg", [128, 2, 4, 128], BF16, n=2)
            for d_ in range(2):
                for k in range(4):
                    P.ts(dg[:, d_, k, :], C.ident, prm["cw"][:, c, d_, k:k + 1], None, ALU.mult)
            cur_dg[0], cur_dg[1] = c, dg
        return cur_dg[1]

    def stageA1a(win, W, c, d):
        dg = get_diag(c)
        o0 = 0 if d == 0 else 3
        psc = P.ps()
        for k in range(4):
            P.mm(psc[:, 0:W], dg[:, d, k, :], win[:, o0 + k:o0 + k + W], start=(k == 0), stop=(k == 3))
        uc = P.tmp("l_uc", [128, PW], F32, n=4)[:, 0:W]
        P.ts(uc, psc[:, 0:W], prm["l_cb"][:, c, d:d + 1], None, ALU.add)
        ucb = P.tmp("l_ucb", [128, PW], BF16, n=2)[:, 0:W]
        P.copy(ucb, uc, eng="dve")
        return dict(uc=uc, ucb=ucb)

    def stageA1b(st, W, c, d):
        ps = P.ps()
        P.mm(ps[:, 0:W], prm["wa"][:, d, c, :], st["ucb"])
        ps2 = P.ps()
        P.mm(ps2[:, 0:W], prm["wx"][:, d, c, :], st["ucb"])
        st["ps"], st["ps2"] = ps, ps2

    def stageA2(st, W, c, d):
        tr = P.tmp("l_tr", [128, PW], F32, n=2)[:, 0:W]
        ti = P.tmp("l_ti", [128, PW], F32, n=4)[:, 0:W]
        P.act(tr, st["ps"][:, 0:W], AF.Tanh, bias=hba[:, c, d:d + 1], scale=0.5)
        P.act(ti, st["ps2"][:, 0:W], AF.Tanh, bias=hbx[:, c, d:d + 1], scale=0.5)
        st["tr"], st["ti"] = tr, ti

    def stageEX(st, W, c, d):
        a = P.tmp("l_a", [128, PW], F32, n=3)[:, 0:W]
        P.act(a, st["tr"], AF.Exp, bias=hs1[:, c, d:d + 1], scale=hs1[:, c, d:d + 1])
        t = P.tmp("l_t", [128, PW], F32, n=3)[:, 0:W]
        P.act(t, st["tr"], AF.Exp, bias=prm["s1"][:, c, d:d + 1], scale=prm["s1"][:, c, d:d + 1])
        st["a"], st["t"] = a, t

    def stageSQ(st, W):
        P.act(st["t"], st["t"], AF.Sqrt, bias=qtr, scale=-0.25)

    def stageB2(st, W, c, d, init, out, want_A=None):
        uc, ti, a, t = st["uc"], st["ti"], st["a"], st["t"]
        P.stt(ti, ti, 1.0, uc, ALU.add, ALU.mult)
        P.tt(ti, ti, t, ALU.mult)
        if d == 0:
            P.scan(out, a, ti, init)
        else:
            P.scan(out[:, ::-1], a[:, ::-1], ti[:, ::-1], init)
        if want_A is not None:
            P.reduce(want_A, a, ALU.mult)

    def run_units(units):
        n = len(units)
        if n == 0:
            return
        sts = [None] * n

        def A1(i):
            u = units[i]
            sts[i] = stageA1a(u["win"], u["W"], u["c"], u["d"])

        def A1b(i):
            u = units[i]
            stageA1b(sts[i], u["W"], u["c"], u["d"])

        def A2(i):
            u = units[i]
            stageA2(sts[i], u["W"], u["c"], u["d"])

        def EX(i):
            u = units[i]
            stageEX(sts[i], u["W"], u["c"], u["d"])

        def B2(i):
            u = units[i]
            stageB2(sts[i], u["W"], u["c"], u["d"], u["init"](), u["out"], u.get("want_A"))
            if u.get("after"):
                u["after"]()
            sts[i] = None

        A1(0)
        if n > 1:
            A1(1)
        A1b(0)
        if n > 1:
            A1b(1)
        A2(0)
        if n > 1:
            A2(1)
        i = 0
        while i < n:
            two = i + 1 < n
            for j in (i + 2, i + 3):
                if j < n:
                    A1(j)
            for j in (i + 2, i + 3):
                if j < n:
                    A1b(j)
            EX(i)
            if two:
                EX(i + 1)
            stageSQ(sts[i], units[i]["W"])
            if two:
                stageSQ(sts[i + 1], units[i + 1]["W"])
            for j in (i + 2, i + 3):
                if j < n:
                    A2(j)
            B2(i)
            if two:
                B2(i + 1)
            i += 2

    NQ = TOK // HALF
    summ = P.sb([128, 8, 2, 3], F32, "summ")
    apq = P.sb([128, 8, 2, NQ], F32, "apq")
    carry = {}
    units = []
    for c in range(8):
        for d in range(2):
            hcx = P.tmp("l_hx", [128, PW], F32, n=2)[:, 0:CTXL]

            def after_ctx(c=c, d=d, hcx=hcx):
                P.copy(summ[:, c, d, 2:3], hcx[:, CTXL - 1:CTXL] if d == 0 else hcx[:, 0:1])
            units.append(dict(win=ucx[:, c, :], W=CTXL, c=c, d=d, init=lambda: 0.0, out=hcx, after=after_ctx))
            order = list(range(NQ)) if d == 0 else list(range(NQ - 1, -1, -1))
            for n_, k in enumerate(order):
                ho = P.tmp("l_hx", [128, PW], F32, n=2)[:, 0:HALF]
                key = (c, d)

                def init_fn(key=key, n_=n_):
                    return 0.0 if n_ == 0 else carry[key]

                def after_q(key=key, ho=ho, d=d, n_=n_, c=c):
                    if n_ == NQ - 1:
                        P.copy(summ[:, c, d, 1:2], ho[:, HALF - 1:HALF] if d == 0 else ho[:, 0:1])
                    else:
                        cr = P.tmp("l_cr", [128, 1], F32, n=4)
                        P.copy(cr, ho[:, HALF - 1:HALF] if d == 0 else ho[:, 0:1])
                        carry[key] = cr
                units.append(dict(win=uu[:, c, k * HALF:k * HALF + HALF + 6], W=HALF, c=c, d=d, init=init_fn, out=ho,
                                  after=after_q, want_A=apq[:, c, d, k:k + 1]))
    run_units(units)
    P.tt(summ[:, :, :, 0], apq[:, :, :, 0], apq[:, :, :, 1], ALU.mult)
    for k in range(2, NQ):
        P.tt(summ[:, :, :, 0], summ[:, :, :, 0], apq[:, :, :, k], ALU.mult)
    P.dma(sm_in_t, summ.re("p c d k -> p (c d k)"))
    allgather(sm_in_t, sm_all_t)
    sm = P.sb([128, 4, 8, 2, 3], F32, "sm")
    P.dma(sm.re("p r c d k -> p r (c d k)"), T(sm_all.rearrange("(r p) f -> p r f", p=128), sm_all_t.bufs))
    h0 = P.sb([128, 8, 2], F32, "h0")
    cand = P.sb([128, 8], F32, "cand")
    for d in range(2):
        P.copy(h0[:, :, d], summ[:, :, d, 2])
        order = range(4) if d == 0 else range(3, -1, -1)
        for j in order:
            P.tt(cand, sm[:, j, :, d, 0], h0[:, :, d], ALU.mult)
            P.tt(cand, cand, sm[:, j, :, d, 1], ALU.add)
            P.tt(cand, cand, h0[:, :, d], ALU.subtract)
            P.stt(h0[:, :, d], cand, jm[:, d, j:j + 1], h0[:, :, d], ALU.mult, ALU.add)
    hst = [T(None, None)] * NQ
    hfull = [P.sb([128, HALF], F32, f"hfull{k}") for k in range(NQ)]
    units = []
    carry2 = {}
    for c in range(8):
        dirs = [0, 1] if c % 2 == 0 else [1, 0]
        for di, d in enumerate(dirs):
            order = list(range(NQ)) if d == 0 else list(range(NQ - 1, -1, -1))
            for n_, k in enumerate(order):
                out = hfull[k] if di == 0 else P.tmp("l_hx", [128, PW], F32, n=2)[:, 0:HALF]
                key = (c, d)

                def init_fn(key=key, n_=n_, c=c, d=d):
                    return h0[:, c, d:d + 1] if n_ == 0 else carry2[key]

                def after_q(key=key, out=out, d=d, di=di, k=k, c=c):
                    cr = P.tmp("l_cr", [128, 1], F32, n=4)
                    P.copy(cr, out[:, HALF - 1:HALF] if d == 0 else out[:, 0:1])
                    carry2[key] = cr
                    if di == 1:
                        P.tt(out, out, hfull[k], ALU.add)
                        P.tt(yg[:, c, k * HALF:(k + 1) * HALF], out, yg[:, c, k * HALF:(k + 1) * HALF], ALU.mult)
                units.append(dict(win=uu[:, c, k * HALF:k * HALF + HALF + 6], W=HALF, c=c, d=d, init=init_fn, out=out, after=after_q))
    run_units(units)
    P.release(m_base)
    w_out1 = P.sb([128, 8, D], BF16, "w_out1")
    for kc in range(8):
        P.dma(w_out1[:, kc, :], D_(dr["od_w_out"][kc * 128:(kc + 1) * 128, :]), q="pool")
    for ti, (xf, W, _) in enumerate(tiles):
        for dc in range(8):
            ps = P.ps()
            for kc in range(8):
                P.mm(ps[:, 0:W], w_out1[:, kc, dc * 128:(dc + 1) * 128], yg[:, kc, ti * 512:(ti + 1) * 512],
                     start=(kc == 0), stop=(kc == 7))
            P.stt(xf(dc), ps[:, 0:W], G1n[:, dc:dc + 1], xf(dc), ALU.mult, ALU.add)
    P.release(m_base)
    P.top_release(top_x)
    moe_layer(C, dr, 1, tiles, A2n, Sh2n, G2n)
    P.add("sp", lambda e: e.dma_start(out=o_out.rearrange("c p t -> p c t"), in_=xres.ap), reads=[xres], writes=[outb], dma=True)
    LRU_PW[0] = 2048
    P.emit(final_reads=[outb])
    return nc, P


def fused_inputs(r, inp, cf, cb, cosT, sinT, lin):
    b, j = r // 4, r % 4
    d = l1_inputs(r, inp, cf, cb, cosT, sinT)
    del d["ada_w"], d["ada_bT"]
    d["ada_wq"] = np.ascontiguousarray(inp["ada_w"][:, :, 1536 * j:1536 * (j + 1)])
    d["ada_bq"] = np.ascontiguousarray(inp["ada_b"].reshape(2, 48, 128)[:, 12 * j:12 * (j + 1), :].transpose(2, 0, 1))
    jm = np.zeros((128, 2, 4), np.float32)
    hs = np.zeros((128, 2, 4), np.float32)
    for jj in range(4):
        jm[:, 0, jj] = 1.0 if jj < j else 0.0
        jm[:, 1, jj] = 1.0 if jj > j else 0.0
        hs[:, 0, jj] = 1.0 if jj == j - 1 else 0.0
        hs[:, 1, jj] = 1.0 if jj == j + 1 else 0.0
    d.update(dict(
        nffnT=np.stack([chunkT(inp["norm_ffn"][l]) for l in range(2)]),
        lamp=np.ascontiguousarray(np.stack([inp["ev_lam_q1"][0], inp["ev_lam_k1"][0], inp["ev_lam_q2"][0], inp["ev_lam_k2"][0]], axis=-1)),
        subn=np.ascontiguousarray(inp["ev_sub_norm"][0].reshape(128, 1)),
        w_out=inp["ev_w_out"][0], od_w_in=inp["od_w_in"][0], od_w_out=inp["od_w_out"][0], jmask=jm, hsel=hs))
    d.update(lin)
    m0, m1 = moe_inputs(inp, 0), moe_inputs(inp, 1)
    d.update(dict(moe_wr=np.concatenate([m0["moe_wr"], m1["moe_wr"]], 0), moe_rb=np.concatenate([m0["moe_rb"], m1["moe_rb"]], 0),
                  sel=m0["sel"], moe_wg=inp["moe_w_gate"], moe_wu=inp["moe_w_up"], moe_wd=inp["moe_w_down"]))
    return d


def kernel(**inp):
    inp = {k: np.asarray(v) for k, v in inp.items()}
    cf, cb = host_consts()
    cosT, sinT = rope_tables()
    cores = list(range(NCORE))
    lin = lru_inputs(inp)
    ncf, _ = _get("F", build_fused)
    ims = [fused_inputs(r, inp, cf, cb, cosT, sinT, lin) for r in cores]
    res = run_bass_kernel_spmd(ncf, ims, core_ids=cores).results
    out = np.empty((2, SEQ, D), np.float32)
    for r in cores:
        b, j = r // 4, r % 4
        out[b, j * TOK:(j + 1) * TOK, :] = res[r]["o_out"].reshape(D, TOK).T
    return out
```

```python
import math
import numpy as np
import ml_dtypes
from contextlib import ExitStack
import concourse.bass as bass
import concourse.mybir as mybir
from concourse.bass_utils import run_bass_kernel_spmd

F32 = mybir.dt.float32
BF16 = mybir.dt.bfloat16
AF = mybir.ActivationFunctionType
ALU = mybir.AluOpType
AX = mybir.AxisListType
NPBF = ml_dtypes.bfloat16

NDSEM = 8
EPS = 1e-6
NCORE = 8
TOK = 2048
CTXL = 256
SEQ = 8192
D = 1024
NKT = (SEQ + CTXL) // 128


class Buf:
    __slots__ = ("lw", "rd")

    def __init__(self):
        self.lw = None
        self.rd = []


class T:
    __slots__ = ("ap", "bufs")

    def __init__(self, ap, bufs):
        self.ap = ap
        self.bufs = bufs

    def __getitem__(self, key):
        return T(self.ap[key], self.bufs)

    def re(self, s, **kw):
        return T(self.ap.rearrange(s, **kw), self.bufs)

    def bc(self, shape):
        return T(self.ap.to_broadcast(shape), self.bufs)


def D_(ap):
    return T(ap, [])


class Prog:
    ENG = ["pe", "act", "dve", "pool", "sp"]

    def __init__(self, nc):
        self.nc = nc
        self.ins = {e: [] for e in self.ENG}
        self.ndma = {e: 0 for e in self.ENG}
        self.sb_off = 16512
        self.sb_max = 0
        self.uid = 0
        self.psum_banks = []
        self.ps_i = 0
        self.barrier_deps = {}
        self.all_dmas_since_barrier = []
        self.pools = {}
        self.held = set()
        self.top = 229312
        self.ncc = 0
        self.init_psum()

    def sb(self, shape, dtype, name=None):
        self.uid += 1
        name = f"{name or 't'}_{self.uid}"
        esz = 4 if dtype == F32 else 2
        free = 1
        for s in shape[1:]:
            free *= s
        nbytes = (free * esz + 31) // 32 * 32
        h = self.nc.alloc_sbuf_tensor_at(name, list(shape), dtype, offset=self.sb_off)
        self.sb_off += nbytes
        self.sb_max = max(self.sb_max, self.sb_off)
        assert self.sb_off <= self.top, f"SBUF overflow {self.sb_off} > {self.top} at {name}"
        return T(h.ap(), [Buf()])

    def sb_top(self, shape, dtype, name=None):
        self.uid += 1
        name = f"{name or 't'}_{self.uid}"
        esz = 4 if dtype == F32 else 2
        free = 1
        for s in shape[1:]:
            free *= s
        nbytes = (free * esz + 31) // 32 * 32
        self.top -= nbytes
        assert self.sb_off <= self.top, f"SBUF overflow (top) {self.sb_off} > {self.top} at {name}"
        h = self.nc.alloc_sbuf_tensor_at(name, list(shape), dtype, offset=self.top)
        return T(h.ap(), [Buf()])

    def top_release(self, t):
        self.barrier()
        self.top = t

    def mark(self):
        return self.sb_off

    def release(self, m):
        self.barrier()
        self.sb_off = m
        for k in [k for k, v in self.pools.items() if v[0] >= m]:
            del self.pools[k]

    def tmp(self, key, shape, dtype, n=2):
        if key not in self.pools:
            off = self.sb_off
            self.pools[key] = [off, [self.sb(shape, dtype, key) for _ in range(n)], 0]
        p = self.pools[key]
        t = p[1][p[2] % len(p[1])]
        p[2] += 1
        return t

    def init_psum(self):
        for i in range(8):
            h = self.nc.alloc_psum_tensor(f"psb{i}", [128, 512], F32)
            self.psum_banks.append(T(h.ap(), [Buf()]))

    def ps(self, hold=False):
        while (self.ps_i % 8) in self.held:
            self.ps_i += 1
        i = self.ps_i % 8
        self.ps_i += 1
        if hold:
            self.held.add(i)
        return self.psum_banks[i]

    def ps_free(self, t):
        for i, b in enumerate(self.psum_banks):
            if b.bufs is t.bufs:
                self.held.discard(i)

    def add(self, eng, fn, reads=(), writes=(), dma=False, cc=False):
        lst = self.ins[eng]
        idx = len(lst)
        if cc:
            j = self.ncc
            self.ncc += 1
            me = ("x", eng, j)
        elif dma:
            j = self.ndma[eng]
            self.ndma[eng] += 1
            me = ("d", eng, j)
        else:
            j = None
            me = ("c", eng, idx)
        deps = set()
        for t in reads:
            for b in t.bufs:
                if b.lw is not None:
                    deps.add(b.lw)
        for t in writes:
            for b in t.bufs:
                if b.lw is not None:
                    deps.add(b.lw)
                deps.update(b.rd)
        deps.discard(me)
        if eng in self.barrier_deps:
            deps |= self.barrier_deps.pop(eng)
        if eng == "pe":
            deps = {d for d in deps if not (d[0] == "c" and d[1] == "pe")}
        for t in reads:
            for b in t.bufs:
                b.rd.append(me)
        for t in writes:
            for b in t.bufs:
                b.lw = me
                b.rd = []
        lst.append(dict(fn=fn, deps=deps, dma=dma, j=j, cc=cc))
        if dma or cc:
            self.all_dmas_since_barrier.append(me)
        return me

    def barrier(self):
        deps = set()
        for e in self.ENG:
            for k in range(len(self.ins[e]) - 1, -1, -1):
                if not self.ins[e][k]["dma"] and not self.ins[e][k]["cc"]:
                    deps.add(("c", e, k))
                    break
        deps |= set(self.all_dmas_since_barrier)
        self.all_dmas_since_barrier = []
        for e in self.ENG:
            self.barrier_deps[e] = set(deps) | self.barrier_deps.get(e, set())

    def mm(self, out, lhsT, rhs, start=True, stop=True, **kw):
        return self.add("pe", lambda e: e.matmul(out.ap, lhsT.ap, rhs.ap, start=start, stop=stop, **kw),
                        reads=[lhsT, rhs], writes=[out])

    def tr(self, out, in_, ident):
        return self.add("pe", lambda e: e.transpose(out.ap, in_.ap, ident.ap), reads=[in_, ident], writes=[out])

    def act(self, out, in_, func, bias=None, scale=None, accum=None):
        reads = [in_]
        kw = {}
        if bias is not None:
            if isinstance(bias, T):
                reads.append(bias)
                kw["bias"] = bias.ap
            else:
                kw["bias"] = bias
        if scale is not None:
            if isinstance(scale, T):
                reads.append(scale)
                kw["scale"] = scale.ap
            else:
                kw["scale"] = scale
        writes = [out]
        if accum is not None:
            writes.append(accum)
            kw["accum_out"] = accum.ap
        return self.add("act", lambda e: e.activation(out.ap, in_.ap, func, **kw), reads=reads, writes=writes)

    def tt(self, out, a, b, op, eng="dve"):
        return self.add(eng, lambda e: e.tensor_tensor(out.ap, a.ap, b.ap, op), reads=[a, b], writes=[out])

    def ts(self, out, a, s1, s2, op0, op1=None, eng="dve"):
        reads = [a]
        v1 = s1.ap if isinstance(s1, T) else s1
        v2 = s2.ap if isinstance(s2, T) else s2
        if isinstance(s1, T):
            reads.append(s1)
        if isinstance(s2, T):
            reads.append(s2)
        if op1 is None:
            return self.add(eng, lambda e: e.tensor_scalar(out.ap, a.ap, v1, None, op0), reads=reads, writes=[out])
        return self.add(eng, lambda e: e.tensor_scalar(out.ap, a.ap, v1, v2, op0, op1), reads=reads, writes=[out])

    def stt(self, out, in0, scalar, in1, op0, op1):
        reads = [in0, in1]
        sv = scalar.ap if isinstance(scalar, T) else scalar
        if isinstance(scalar, T):
            reads.append(scalar)
        return self.add("dve", lambda e: e.scalar_tensor_tensor(out.ap, in0.ap, sv, in1.ap, op0, op1),
                        reads=reads, writes=[out])

    def scan(self, out, d0, d1, init, op0=ALU.mult, op1=ALU.add):
        reads = [d0, d1]
        iv = init.ap if isinstance(init, T) else init
        if isinstance(init, T):
            reads.append(init)
        return self.add("dve", lambda e: e.tensor_tensor_scan(out.ap, d0.ap, d1.ap, iv, op0, op1),
                        reads=reads, writes=[out])

    def copy(self, out, in_, eng="dve"):
        if eng == "act":
            return self.add("act", lambda e: e.copy(out.ap, in_.ap), reads=[in_], writes=[out])
        return self.add(eng, lambda e: e.tensor_copy(out.ap, in_.ap), reads=[in_], writes=[out])

    def recip(self, out, in_):
        return self.add("dve", lambda e: e.reciprocal(out.ap, in_.ap), reads=[in_], writes=[out])

    def reduce(self, out, in_, op, axis=AX.X):
        return self.add("dve", lambda e: e.tensor_reduce(out.ap, in_.ap, axis, op), reads=[in_], writes=[out])

    def memset(self, t, val, eng="dve"):
        return self.add(eng, lambda e: e.memset(t.ap, val), reads=[], writes=[t])

    def dma(self, out, in_, q="sp"):
        return self.add(q, lambda e: e.dma_start(out=out.ap, in_=in_.ap), reads=[in_], writes=[out], dma=True)

    def emit(self, final_reads=()):
        nc = self.nc
        if final_reads:
            self.add("sp", lambda e: e.nop(), reads=list(final_reads), writes=[])
        waits = {e: [] for e in self.ENG}
        marked = {e: set() for e in self.ENG}
        for e in self.ENG:
            seen_c = {}
            seen_d = set()
            for ins in self.ins[e]:
                w = []
                if ins["dma"] and ins["j"] >= NDSEM:
                    pj = ins["j"] - NDSEM
                    if ("d", e, pj) not in seen_d:
                        w.append(("d", e, pj))
                        seen_d.add(("d", e, pj))
                best = {}
                for d in ins["deps"]:
                    if d[0] == "c":
                        if d[2] > best.get(d[1], -1):
                            best[d[1]] = d[2]
                    else:
                        if (d[0], d[1], d[2]) not in seen_d:
                            seen_d.add((d[0], d[1], d[2]))
                            w.append(d)
                for f, i in best.items():
                    if i > seen_c.get(f, -1):
                        seen_c[f] = i
                        marked[f].add(i)
                        w.append(("c", f, i))
                waits[e].append(w)
        val = {e: {} for e in self.ENG}
        for e in self.ENG:
            c = 0
            for i in sorted(marked[e]):
                c += 1
                val[e][i] = c
        self.stats = {e: (len(self.ins[e]), len(marked[e])) for e in self.ENG}
        with ExitStack() as st:
            csem = {e: st.enter_context(nc.semaphore(f"cs_{e}")) for e in self.ENG}
            xsem = [st.enter_context(nc.semaphore(f"xs_{i}")) for i in range(self.ncc)]
            dsem = {e: [st.enter_context(nc.semaphore(f"ds_{e}{i}")) for i in range(NDSEM)]
                    for e in self.ENG if self.ndma[e] > 0}
            block = st.enter_context(nc.Block())

            def run(e, eng):
                for k, ins in enumerate(self.ins[e]):
                    for d in waits[e][k]:
                        if d[0] == "c":
                            eng.wait_ge(csem[d[1]], val[d[1]][d[2]])
                        elif d[0] == "x":
                            eng.wait_ge(xsem[d[2]], 1)
                        else:
                            eng.wait_ge(dsem[d[1]][d[2] % NDSEM], 16 * (d[2] // NDSEM + 1))
                    r = ins["fn"](eng)
                    if ins["cc"]:
                        r.then_inc(xsem[ins["j"]])
                    elif ins["dma"]:
                        r.then_inc(dsem[e][ins["j"] % NDSEM], 16)
                    elif k in marked[e]:
                        r.then_inc(csem[e], 1)

            @block.tensor
            def _(eng):
                run("pe", eng)

            @block.scalar
            def _(eng):
                run("act", eng)

            @block.vector
            def _(eng):
                run("dve", eng)

            @block.gpsimd
            def _(eng):
                run("pool", eng)

            @block.sync
            def _(eng):
                run("sp", eng)


class Ctx:
    def __init__(self, nc, P, dram):
        self.nc, self.P, self.dram = nc, P, dram
        cf = P.sb([128, 384], F32, "cf")
        P.dma(cf, D_(dram["cf"]))
        self.ident = cf[:, 0:128]
        self.perm = cf[:, 128:256]
        self.onesf = cf[:, 256:384]
        cb = P.sb([128, 256], BF16, "cb")
        P.dma(cb, D_(dram["cb"]), q="pool")
        self.ones = cb[:, 0:128]
        self.bones = cb[:, 128:256]
        self.epsb = P.sb([128, 1], F32, "eps")
        P.memset(self.epsb, EPS)

    def rstd_from_ps(self, ps_ss, W, inv_n, name="rstd", n=2):
        P = self.P
        r = P.tmp(f"{name}{W}", [128, W], F32, n=n)
        P.act(r, ps_ss, AF.Ln, bias=self.epsb, scale=inv_n)
        P.act(r, r, AF.Exp, scale=-0.5)
        return r


def norm_mod(C, xt, W, A, Bsh, out_h, out_f32=None):
    P = C.P
    ps = P.ps()
    for c in range(8):
        s = P.tmp(f"sq{W}", [128, W], BF16)
        P.act(s, xt[:, c, :], AF.Square)
        P.mm(ps[:, 0:W], C.ones, s, start=(c == 0), stop=(c == 7))
    rstd = C.rstd_from_ps(ps[:, 0:W], W, 1.0 / D)
    for c in range(8):
        t = P.tmp(f"nt{W}", [128, W], F32)
        P.tt(t, xt[:, c, :], rstd, ALU.mult)
        if out_f32 is not None:
            P.act(out_f32[:, c, :], t, AF.Identity, bias=Bsh[:, c:c + 1], scale=A[:, c:c + 1])
            P.copy(out_h[:, c, :], out_f32[:, c, :], eng="pool")
        else:
            P.act(out_h[:, c, :], t, AF.Identity, bias=Bsh[:, c:c + 1], scale=A[:, c:c + 1])
    return


def compute_mods(C, dram, l_list, cvec, out_mods):
    P = C.P
    m = P.mark()
    sc = P.sb([128, 8, 2], F32, "silu_c")
    P.act(sc, cvec, AF.Silu)
    wbuf = [P.sb([128, 8, 1024], F32, "adaw") for _ in range(2)]
    k = 0
    for li, l in enumerate(l_list):
        ps = P.ps(hold=True)
        bT = P.sb([128, 48], F32, "adab")
        P.dma(bT, D_(dram["ada_bT"][l]))
        for j in range(6):
            w = wbuf[k % 2]
            k += 1
            P.dma(w, D_(dram["ada_w"][l].rearrange("(kc p) f -> p kc f", p=128)[:, :, j * 1024:(j + 1) * 1024]))
            for fc in range(8):
                g = j * 8 + fc
                for kc in range(8):
                    P.mm(ps[:, 2 * g:2 * g + 2], w[:, kc, fc * 128:(fc + 1) * 128], sc[:, kc, :],
                         start=(kc == 0), stop=(kc == 7))
        P.tt(out_mods[li], ps[:, 0:96].re("p (g c) -> p g c", c=2),
             T(bT.ap.unsqueeze(2).to_broadcast([128, 48, 2]), bT.bufs), ALU.add)
        P.ps_free(ps)
    P.release(m)


def din(nc, name, shape, dt=F32):
    return nc.dram_tensor(name, list(shape), dt, kind="ExternalInput").ap()


def dout(nc, name, shape, dt=F32):
    return nc.dram_tensor(name, list(shape), dt, kind="ExternalOutput").ap()


def mod_scalars(C, mods_l, gainT, which, col, name):
    P = C.P
    base = 24 * which
    A = P.sb([128, 8], F32, name)
    P.ts(A, mods_l[:, base + 8:base + 16, col], 1.0, None, ALU.add)
    P.tt(A, A, gainT, ALU.mult)
    Sh = P.sb([128, 8], F32, name + "s")
    P.copy(Sh, mods_l[:, base:base + 8, col])
    G = P.sb([128, 8], F32, name + "g")
    P.copy(G, mods_l[:, base + 16:base + 24, col])
    return A, Sh, G


def qk_norm_rope(C, ps_in, W, gain, cos, sin, out):
    P = C.P
    k_sb = P.tmp(f"qk_k{W}", [128, W], F32, n=2)
    P.copy(k_sb, ps_in, eng="act")
    sq = P.tmp(f"qk_sq{W}", [128, W], BF16)
    P.act(sq, ps_in, AF.Square)
    ps2 = P.ps()
    P.mm(ps2[:, 0:W], C.bones, sq)
    rs = C.rstd_from_ps(ps2[:, 0:W], W, 1.0 / 64.0, name="qk_rs", n=2)
    if cos is None:
        P.stt(out, k_sb, gain, rs, ALU.mult, ALU.mult)
        return
    kh = P.tmp(f"qk_kh{W}", [128, W], F32, n=2)
    P.stt(kh, k_sb, gain, rs, ALU.mult, ALU.mult)
    ps3 = P.ps()
    P.mm(ps3[:, 0:W], C.perm, kh)
    t1 = P.tmp(f"qk_t1{W}", [128, W], F32, n=2)
    P.tt(t1, kh, cos, ALU.mult)
    t2 = P.tmp(f"qk_t2{W}", [128, W], F32, n=2)
    P.tt(t2, ps3[:, 0:W], sin, ALU.mult)
    P.tt(out, t1, t2, ALU.add)


def build_L1():
    nc = bass.Bass("TRN2", target_bir_lowering=False)
    dr = {}
    dr["cf"] = din(nc, "cf", [128, 384])
    dr["cb"] = din(nc, "cb", [128, 256])
    dr["xo"] = din(nc, "xo", [D, TOK])
    dr["xh"] = din(nc, "xh", [D, 2])
    dr["hmask"] = din(nc, "hmask", [128, 2])
    dr["ctx"] = din(nc, "ctx", [D, CTXL])
    dr["cvec"] = din(nc, "cvec", [128, 8, 2])
    dr["ada_w"] = din(nc, "ada_w", [2, D, 6 * D])
    dr["ada_bT"] = din(nc, "ada_bT", [2, 128, 48])
    dr["nmixT"] = din(nc, "nmixT", [2, 128, 8])
    dr["w_in"] = din(nc, "w_in", [D, 3072])
    dr["convw"] = din(nc, "convw", [128, 4, 3])
    dr["convb"] = din(nc, "convb", [128, 4])
    dr["qkn"] = din(nc, "qkn", [128, 2])
    dr["cos"] = din(nc, "cos", [128, TOK])
    dr["sin"] = din(nc, "sin", [128, TOK])
    o_kT = dout(nc, "o_kT", [4, 128, TOK], BF16)
    o_v = dout(nc, "o_v", [TOK, 512], BF16)
    o_kcT = dout(nc, "o_kcT", [4, 128, CTXL], BF16)
    o_vc = dout(nc, "o_vc", [CTXL, 512], BF16)
    o_qT = dout(nc, "o_qT", [4, 128, TOK], BF16)
    o_qcT = dout(nc, "o_qcT", [4, 128, CTXL], BF16)
    o_oa = dout(nc, "o_oa", [4, 128, TOK], BF16)
    o_oac = dout(nc, "o_oac", [4, 128, CTXL], BF16)
    o_mods = dout(nc, "o_mods", [2, 128, 48, 2])

    P = Prog(nc)
    C = Ctx(nc, P, dr)
    outb = T(None, [Buf()])

    cvec = P.sb([128, 8, 2], F32, "cvec")
    P.dma(cvec, D_(dr["cvec"]))
    mods = [P.sb([128, 48, 2], F32, f"mods{l}") for l in range(2)]
    compute_mods(C, dr, [0, 1], cvec, mods)
    for l in range(2):
        P.add("sp", lambda e, l=l: e.dma_start(out=o_mods[l], in_=mods[l].ap), reads=[mods[l]], writes=[outb], dma=True)
    nmix = P.sb([128, 8], F32, "nmix")
    P.dma(nmix, D_(dr["nmixT"][0]))
    A_lat, Sh_lat, _ = mod_scalars(C, mods[0], nmix, 0, 0, "Alat")
    A_ctx, Sh_ctx, _ = mod_scalars(C, mods[0], nmix, 0, 1, "Actx")

    w_in = P.sb([128, 8, 3072], BF16, "w_in")
    for kc in range(8):
        P.dma(w_in[:, kc, :], D_(dr["w_in"][kc * 128:(kc + 1) * 128, :]), q="pool")
    convw = P.sb([128, 4, 3], F32, "convw")
    P.dma(convw, D_(dr["convw"]))
    convb = P.sb([128, 4], F32, "convb")
    P.dma(convb, D_(dr["convb"]))
    qkn = P.sb([128, 2], F32, "qkn")
    P.dma(qkn, D_(dr["qkn"]))
    hmask = P.sb([128, 2], F32, "hmask")
    P.dma(hmask, D_(dr["hmask"]))

    qT = P.sb([128, 4, TOK], BF16, "qT")
    kT = P.sb([128, 4, TOK], BF16, "kT")
    cbs = P.sb([128, 4, TOK], BF16, "cbs")
    ub = P.sb([128, 4, TOK + 2], BF16, "ub")
    qcT = P.sb([128, 4, CTXL], BF16, "qcT")
    kcT = P.sb([128, 4, CTXL], BF16, "kcT")
    cbc = P.sb([128, 4, CTXL], BF16, "cbc")
    ubc = P.sb([128, 4, CTXL + 2], BF16, "ubc")
    P.memset(ubc[:, :, 0:1], 0.0)
    P.memset(ubc[:, :, CTXL + 1:CTXL + 2], 0.0)

    def proj(h, W, f):
        ps = P.ps()
        for kc in range(8):
            P.mm(ps[:, 0:W], w_in[:, kc, f * 128:(f + 1) * 128], h[:, kc, 0:W], start=(kc == 0), stop=(kc == 7))
        return ps[:, 0:W]

    def front_tile(xsrc, W, A, Sh, rope, dst):
        xt = P.tmp(f"xt{W}", [128, 8, W], F32, n=1)
        P.dma(xt, D_(xsrc.rearrange("(c p) t -> p c t", p=128)))
        if rope:
            cs_c = P.tmp("cos_t", [128, W], F32)
            cs_s = P.tmp("sin_t", [128, W], F32)
            P.dma(cs_c, D_(rope[0][:, dst["t0"]:dst["t0"] + W]))
            P.dma(cs_s, D_(rope[1][:, dst["t0"]:dst["t0"] + W]))
        h = P.tmp(f"h{W}", [128, 8, W], BF16)
        norm_mod(C, xt, W, A, Sh, h)
        t0 = dst["t0"]
        if dst.get("cb") is not None:
            for c in range(4):
                ps = proj(h, W, c)
                P.copy(dst["cb"][:, c, t0:t0 + W], ps, eng="act")
        for c in range(4):
            ps_cc = proj(h, W, 4 + c)
            cc = P.tmp(f"cc{W}", [128, W], F32)
            P.copy(cc, ps_cc, eng="act")
            ps_cx = proj(h, W, 8 + c)
            P.tt(dst["u"](c), cc, ps_cx, ALU.mult)
        if dst.get("q") is None:
            return
        for hh in range(4):
            ps = proj(h, W, 12 + hh)
            cs = (cs_c, cs_s) if rope else (None, None)
            qk_norm_rope(C, ps, W, qkn[:, 0:1], cs[0], cs[1], dst["q"][:, hh, t0:t0 + W])
        for hh in range(4):
            ps = proj(h, W, 16 + hh)
            cs = (cs_c, cs_s) if rope else (None, None)
            qk_norm_rope(C, ps, W, qkn[:, 1:2], cs[0], cs[1], dst["k"][:, hh, t0:t0 + W])
        for sub in range(W // 128):
            ps = P.ps()
            for kc in range(8):
                P.mm(ps, h[:, kc, sub * 128:(sub + 1) * 128], w_in[:, kc, 2560:3072], start=(kc == 0), stop=(kc == 7))
            vs = P.tmp("vs", [128, 512], BF16, n=3)
            P.copy(vs, ps, eng="act")
            r0 = t0 + sub * 128
            P.add("sp", lambda e, vs=vs, r0=r0, dv=dst["v_out"]: e.dma_start(out=dv[r0:r0 + 128, :], in_=vs.ap),
                  reads=[vs], writes=[outb], dma=True)

    cos, sin = dr["cos"], dr["sin"]

    m_own = P.mark()
    for t in range(TOK // 512):
        front_tile(dr["xo"][:, t * 512:(t + 1) * 512], 512, A_lat, Sh_lat, (cos, sin),
                   dict(cb=cbs, u=lambda c, t=t: ub[:, c, 1 + t * 512:1 + (t + 1) * 512], q=qT, k=kT, v_out=o_v, t0=t * 512))
    P.release(m_own)
    uh = P.sb([128, 4, 2], F32, "uh")
    front_tile(dr["xh"], 2, A_lat, Sh_lat, None, dict(u=lambda c: uh[:, c, :], t0=0))
    for c in range(4):
        P.tt(ub[:, c, 0:1], uh[:, c, 0:1], hmask[:, 0:1], ALU.mult)
        P.tt(ub[:, c, TOK + 1:TOK + 2], uh[:, c, 1:2], hmask[:, 1:2], ALU.mult)
    front_tile(dr["ctx"], CTXL, A_ctx, Sh_ctx, None,
               dict(cb=cbc, u=lambda c: ubc[:, c, 1:1 + CTXL], q=qcT, k=kcT, v_out=o_vc, t0=0))

    def conv(ubuf, cbt, W, o_dst):
        for c in range(4):
            acc = P.tmp(f"cacc{W}", [128, W], F32)
            P.ts(acc, ubuf[:, c, 1:1 + W], convw[:, c, 1:2], convb[:, c:c + 1], ALU.mult, ALU.add)
            P.stt(acc, ubuf[:, c, 0:W], convw[:, c, 0:1], acc, ALU.mult, ALU.add)
            P.stt(acc, ubuf[:, c, 2:2 + W], convw[:, c, 2:3], acc, ALU.mult, ALU.add)
            oa = P.tmp(f"oa{W}", [128, W], BF16)
            P.tt(oa, acc, cbt[:, c, :], ALU.mult)
            P.add("sp", lambda e, oa=oa, c=c: e.dma_start(out=o_dst[c], in_=oa.ap), reads=[oa], writes=[outb], dma=True)

    conv(ub, cbs, TOK, o_oa)
    conv(ubc, cbc, CTXL, o_oac)
    for (src, dst) in ((qT, o_qT), (kT, o_kT), (qcT, o_qcT), (kcT, o_kcT)):
        P.add("sp", lambda e, src=src, dst=dst: e.dma_start(out=dst.rearrange("h p t -> p h t"), in_=src.ap),
              reads=[src], writes=[outb], dma=True)
    P.emit(final_reads=[outb])
    return nc, P


def host_consts():
    cf = np.zeros((128, 384), np.float32)
    cf[:, 256:384] = 1.0
    cf[np.arange(128), np.arange(128)] = 1.0
    for m in range(128):
        partner = m + 32 if (m % 64) < 32 else m - 32
        cf[partner, 128 + m] = 1.0
    cb = np.zeros((128, 256), np.float32)
    cb[:, 0:128] = 1.0
    for k in range(128):
        for m in range(128):
            if k // 64 == m // 64:
                cb[k, 128 + m] = 1.0
    return cf, cb


def rope_tables():
    n_rows = SEQ // 64
    rows = np.repeat(np.arange(n_rows), 64).astype(np.float32)
    cols = np.tile(np.arange(64), n_rows).astype(np.float32)
    n_freq = 16
    inv = (np.float32(10000.0) ** (-np.arange(n_freq, dtype=np.float32) / np.float32(n_freq))).astype(np.float32)
    ang = np.concatenate([rows[:, None] * inv, cols[:, None] * inv], axis=-1).astype(np.float32)
    cos, sin = np.cos(ang).astype(np.float32), np.sin(ang).astype(np.float32)
    idx = np.arange(128) % 32
    cosT = np.ascontiguousarray(cos[:, idx].T)
    sgn = np.where((np.arange(128) % 64) < 32, -1.0, 1.0).astype(np.float32)
    sinT = np.ascontiguousarray(sin[:, idx].T * sgn[:, None])
    return cosT, sinT


def chunkT(v):
    return np.ascontiguousarray(v.reshape(-1, 128).T)


def moe_layer(C, dr, l, tiles, A2, Sh2, G2, A2c=None, Sh2c=None, G2c=None):
    P = C.P
    m0 = P.mark()
    NT = sum(W // 128 for (_, W, _) in tiles)
    TT = sum(W for (_, W, _) in tiles)
    hf = P.sb([128, 8, TT], BF16, "hf")
    gateT = P.sb([16, TT], F32, "gateT")
    wr = P.sb([128, 8, 20], F32, "wr")
    P.dma(wr, D_(dr["moe_wr"][l].rearrange("(kc p) n -> p kc n", p=128)))
    rb = P.sb([128, 20], F32, "rb")
    P.dma(rb, D_(dr["moe_rb"][l]))
    sel = P.sb([16, 16, 128], F32, "sel")
    P.dma(sel, D_(dr["sel"]))
    def load_w(e):
        wg = P.tmp("wg", [128, 8, 512], BF16)
        wu = P.tmp("wu", [128, 8, 512], BF16)
        wd = P.tmp("wd", [128, 4, 1024], BF16)
        P.dma(wg, D_(dr["moe_wg"][l, e].rearrange("(kc p) f -> p kc f", p=128)), q="pool")
        P.dma(wu, D_(dr["moe_wu"][l, e].rearrange("(kc p) f -> p kc f", p=128)), q="pool")
        P.dma(wd, D_(dr["moe_wd"][l, e].rearrange("(fc p) d -> p fc d", p=128)), q="pool")
        return wg, wu, wd

    wts = {0: load_w(0)}
    m1 = P.mark()
    ps_l = P.ps(hold=True)
    off = 0
    nt = 0
    wg1, wu1 = P.pools["wg"][1][1], P.pools["wu"][1][1]
    al_a = P.nc.alloc_sbuf_tensor_at(f"hf32a_{l}_{P.uid}", [128, 4, 512], F32, offset=P.pools["wg"][0] + 8192).ap()
    al_b = P.nc.alloc_sbuf_tensor_at(f"hf32b_{l}_{P.uid}", [128, 4, 512], F32, offset=P.pools["wu"][0] + 8192).ap()
    hf32_main = P.tmp("hf32", [128, 8, 512], F32, n=1)
    stage = [[], []]
    for c in range(8):
        stage[0].append(T(hf32_main.ap[:, c, :], [Buf()]))
        b_ = Buf()
        (wg1 if c < 4 else wu1).bufs.append(b_)
        stage[1].append(T((al_a if c < 4 else al_b)[:, c % 4, :], [b_]))
    for ti_, (xf, W, is_ctx) in enumerate(tiles):
        A, Sh = (A2c, Sh2c) if is_ctx else (A2, Sh2)

        def hf32c(c, ti_=ti_, W=W):
            return stage[ti_ % 2][c][:, 0:W]
        ps = P.ps()
        for c in range(8):
            s_ = P.tmp("msq", [128, 512], BF16, n=1)[:, 0:W]
            P.act(s_, xf(c), AF.Square)
            P.mm(ps[:, 0:W], C.ones, s_, start=(c == 0), stop=(c == 7))
        rstd = P.tmp("mrstd", [128, 512], F32, n=1)[:, 0:W]
        P.act(rstd, ps[:, 0:W], AF.Ln, bias=C.epsb, scale=1.0 / D)
        P.act(rstd, rstd, AF.Exp, scale=-0.5)
        ps_t = P.ps()
        for c in range(8):
            t = P.tmp("mnt", [128, 512], F32)[:, 0:W]
            P.tt(t, xf(c), rstd, ALU.mult)
            P.act(hf32c(c), t, AF.Identity, bias=Sh[:, c:c + 1], scale=A[:, c:c + 1])
            P.copy(hf[:, c, off:off + W], hf32c(c), eng="dve")
            P.mm(ps_t[0:20, 0:W], wr[:, c, :], hf32c(c), start=(c == 0), stop=(c == 7))
        lT = P.tmp("mlT", [20, 512], F32, n=1)[:, 0:W]
        P.copy(lT, ps_t[0:20, 0:W], eng="act")
        for sub in range(W // 128):
            P.tr(ps_l[:, nt * 20:nt * 20 + 20], lT[:, sub * 128:(sub + 1) * 128], C.ident[0:20, 0:20])
            nt += 1
        off += W
    Lg = P.sb([128, NT, 20], F32, "Lg")
    P.tt(Lg, ps_l[:, 0:NT * 20].re("p (t k) -> p t k", k=20), T(rb.ap.unsqueeze(1).to_broadcast([128, NT, 20]), rb.bufs), ALU.add)
    gl = Lg[:, :, 0:4]
    el = Lg[:, :, 4:20].re("p t (g e) -> p t g e", e=4)

    def bc3(t, n):
        return T(t.ap.unsqueeze(2).to_broadcast([128, NT, n]), t.bufs)

    gmax = P.sb([128, NT], F32, "gmax")
    P.reduce(gmax, gl, ALU.max)
    oh = P.sb([128, NT, 4], F32, "oh")
    P.tt(oh, gl, bc3(gmax, 4), ALU.is_equal)
    gsh = P.sb([128, NT, 4], F32, "gsh")
    P.tt(gsh, gl, bc3(gmax, 4), ALU.subtract)
    P.act(gsh, gsh, AF.Exp)
    psel = P.sb([128, NT], F32, "psel")
    P.reduce(psel, gsh, ALU.add)
    P.recip(psel, psel)
    e4 = P.sb([128, NT, 4, 4], F32, "e4")
    P.tt(e4, el, T(oh.ap.unsqueeze(3).to_broadcast([128, NT, 4, 4]), oh.bufs), ALU.mult)
    esel = P.sb([128, NT, 4], F32, "esel")
    P.reduce(esel, e4.re("p t g e -> p t e g"), ALU.add)
    mx1 = P.sb([128, NT], F32, "mx1")
    P.reduce(mx1, esel, ALU.max)
    mk1 = P.sb([128, NT, 4], F32, "mk1")
    P.tt(mk1, esel, bc3(mx1, 4), ALU.is_equal)
    es2 = P.sb([128, NT, 4], F32, "es2")
    P.stt(es2, mk1, -1e30, esel, ALU.mult, ALU.add)
    mx2 = P.sb([128, NT], F32, "mx2")
    P.reduce(mx2, es2, ALU.max)
    mk2 = P.sb([128, NT, 4], F32, "mk2")
    P.tt(mk2, es2, bc3(mx2, 4), ALU.is_equal)
    w1 = P.sb([128, NT], F32, "w1")
    P.tt(w1, mx1, mx2, ALU.subtract)
    P.act(w1, w1, AF.Sigmoid)
    P.tt(w1, w1, psel, ALU.mult)
    w2 = P.sb([128, NT], F32, "w2")
    P.tt(w2, psel, w1, ALU.subtract)
    P.tt(mk1, mk1, bc3(w1, 4), ALU.mult)
    P.tt(mk2, mk2, bc3(w2, 4), ALU.mult)
    P.tt(mk1, mk1, mk2, ALU.add)
    gate = e4
    P.tt(gate, T(oh.ap.unsqueeze(3).to_broadcast([128, NT, 4, 4]), oh.bufs),
         T(mk1.ap.unsqueeze(2).to_broadcast([128, NT, 4, 4]), mk1.bufs), ALU.mult)
    gflat = gate.re("p t g e -> p t (g e)")
    for t4 in range(0, NT, 4):
        ps = P.ps()
        n = min(4, NT - t4)
        for i in range(n):
            P.tr(ps[0:16, i * 128:(i + 1) * 128], gflat[:, t4 + i, :], C.ident)
        P.copy(gateT[:, t4 * 128:(t4 + n) * 128], ps[0:16, 0:n * 128])
    P.ps_free(ps_l)
    P.release(m1)

    steps = []
    for e in range(16):
        off = 0
        for (xf, W, is_ctx) in tiles:
            steps.append((e, xf, W, is_ctx, off))
            off += W

    pend = None

    def down(st):
        (e, xf, W, is_ctx, off, actb, wd) = st
        G = G2c if is_ctx else G2
        for dc in range(8):
            ps = P.ps()
            for fc in range(4):
                P.mm(ps[:, 0:W], wd[:, fc, dc * 128:(dc + 1) * 128], actb[:, fc, :], start=(fc == 0), stop=(fc == 3))
            P.stt(xf(dc), ps[:, 0:W], G[:, dc:dc + 1], xf(dc), ALU.mult, ALU.add)

    for si, (e, xf, W, is_ctx, off) in enumerate(steps):
        wg, wu, wd = wts[e]
        psb = P.ps()
        P.mm(psb[:, 0:W], sel[:, e, :], gateT[:, off:off + W])
        gbc = P.tmp(f"gbc{W}", [128, W], F32)
        P.copy(gbc, psb[:, 0:W], eng="act")
        actb = P.tmp(f"actb{W}", [128, 4, W], BF16)
        for fc in range(4):
            ps_g = P.ps()
            for kc in range(8):
                P.mm(ps_g[:, 0:W], wg[:, kc, fc * 128:(fc + 1) * 128], hf[:, kc, off:off + W], start=(kc == 0), stop=(kc == 7))
            ps_u = P.ps()
            for kc in range(8):
                P.mm(ps_u[:, 0:W], wu[:, kc, fc * 128:(fc + 1) * 128], hf[:, kc, off:off + W], start=(kc == 0), stop=(kc == 7))
            sg = P.tmp(f"sg{W}", [128, W], F32)
            P.act(sg, ps_g[:, 0:W], AF.Silu)
            t2 = P.tmp(f"t2{W}", [128, W], F32)
            P.tt(t2, sg, ps_u[:, 0:W], ALU.mult)
            P.tt(actb[:, fc, :], t2, gbc, ALU.mult)
        if pend is not None:
            down(pend)
        pend = (e, xf, W, is_ctx, off, actb, wd)
        if off == 0 and e + 1 < 16:
            wts[e + 1] = load_w(e + 1)
    down(pend)
    P.release(m0)


def moe_drams(nc, dr):
    dr["moe_wr"] = din(nc, "moe_wr", [1, D, 20])
    dr["moe_rb"] = din(nc, "moe_rb", [1, 128, 20])
    dr["sel"] = din(nc, "sel", [16, 16, 128])
    dr["moe_wg"] = din(nc, "moe_wg", [1, 16, D, 512])
    dr["moe_wu"] = din(nc, "moe_wu", [1, 16, D, 512])
    dr["moe_wd"] = din(nc, "moe_wd", [1, 16, 512, D])


def build_L2(debug=False):
    nc = bass.Bass("TRN2", target_bir_lowering=False)
    dr = {}
    dr["cf"] = din(nc, "cf", [128, 384])
    dr["cb"] = din(nc, "cb", [128, 256])
    dr["kT"] = din(nc, "kT", [4, 128, NKT * 128], BF16)
    dr["v"] = din(nc, "v", [4, 128, NKT, 128], BF16)
    dr["qT"] = din(nc, "qT", [4, 128, TOK], BF16)
    dr["qcT"] = din(nc, "qcT", [4, 128, CTXL], BF16)
    dr["oa"] = din(nc, "oa", [4, 128, TOK], BF16)
    dr["oac"] = din(nc, "oac", [4, 128, CTXL], BF16)
    dr["xo"] = din(nc, "xo", [D, TOK])
    dr["ctx"] = din(nc, "ctx", [D, CTXL])
    dr["mods"] = din(nc, "mods", [2, 128, 48, 2])
    dr["lamp"] = din(nc, "lamp", [64, 4])
    dr["subn"] = din(nc, "subn", [128, 1])
    dr["w_out"] = din(nc, "w_out", [D, D])
    dr["nffnT"] = din(nc, "nffnT", [128, 8])
    dr["nmix1T"] = din(nc, "nmix1T", [128, 8])
    dr["od_w_in"] = din(nc, "od_w_in", [D, 2048])
    moe_drams(nc, dr)
    o_x2 = dout(nc, "o_x2", [8, 128, TOK])
    o_yg = dout(nc, "o_yg", [8, 128, TOK], BF16)
    o_u = dout(nc, "o_u", [8, 128, TOK])
    o_uc = dout(nc, "o_uc", [8, 128, CTXL])
    if debug:
        o_x1 = dout(nc, "o_x1", [8, 128, TOK])
        o_c2 = dout(nc, "o_c2", [8, 128, CTXL])

    P = Prog(nc)
    C = Ctx(nc, P, dr)
    outb = T(None, [Buf()])
    LAM_INIT = 0.8 - 0.6 * math.exp(-0.3 * 0)

    mods = [P.sb([128, 48, 2], F32, f"mods{l}") for l in range(2)]
    for l in range(2):
        P.dma(mods[l], D_(dr["mods"][l]))
    nffn = P.sb([128, 8], F32, "nffn")
    P.dma(nffn, D_(dr["nffnT"]))
    nmix1 = P.sb([128, 8], F32, "nmix1")
    P.dma(nmix1, D_(dr["nmix1T"]))
    _, _, G1 = mod_scalars(C, mods[0], nffn, 0, 0, "m0l")
    _, _, G1c = mod_scalars(C, mods[0], nffn, 0, 1, "m0c")
    A2, Sh2, G2 = mod_scalars(C, mods[0], nffn, 1, 0, "f0l")
    A2c, Sh2c, G2c = mod_scalars(C, mods[0], nffn, 1, 1, "f0c")
    A1n, Sh1n, _ = mod_scalars(C, mods[1], nmix1, 0, 0, "m1l")
    A1nc, Sh1nc, _ = mod_scalars(C, mods[1], nmix1, 0, 1, "m1c")

    lamp = P.sb([64, 4], F32, "lamp")
    P.dma(lamp, D_(dr["lamp"]))
    lpr = P.sb([64, 2], F32, "lpr")
    P.tt(lpr[:, 0:1], lamp[:, 0:1], lamp[:, 1:2], ALU.mult)
    P.tt(lpr[:, 1:2], lamp[:, 2:3], lamp[:, 3:4], ALU.mult)
    psl = P.ps()
    P.mm(psl[:, 0:2], C.onesf[0:64, :], lpr)
    lex = P.sb([128, 2], F32, "lex")
    P.act(lex, psl[:, 0:2], AF.Exp)
    nlam = P.sb([128, 1], F32, "nlam")
    P.tt(nlam, lex[:, 1:2], lex[:, 0:1], ALU.subtract)
    P.ts(nlam, nlam, -LAM_INIT, None, ALU.add)
    subn = P.sb([128, 1], F32, "subn")
    P.dma(subn, D_(dr["subn"]))
    P.ts(subn, subn, 1.0 - LAM_INIT, None, ALU.mult)

    xres = P.sb([128, 8, TOK], F32, "xres")
    xc = P.sb([128, 8, CTXL], F32, "xc")
    P.dma(xres, D_(dr["xo"].rearrange("(c p) t -> p c t", p=128)))
    P.dma(xc, D_(dr["ctx"].rearrange("(c p) t -> p c t", p=128)))
    m_mix = P.mark()
    w_out = P.sb([128, 8, D], BF16, "w_out")
    for kc in range(8):
        P.dma(w_out[:, kc, :], D_(dr["w_out"][kc * 128:(kc + 1) * 128, :]), q="pool")
    mix = P.sb([128, 8, TOK], BF16, "mix")
    mixc = P.sb([128, 8, CTXL], BF16, "mixc")
    P.dma(mix[:, 0:4, :], D_(dr["oa"].rearrange("c p t -> p c t")))
    P.dma(mixc[:, 0:4, :], D_(dr["oac"].rearrange("c p t -> p c t")))
    m_att = P.mark()
    qT = P.sb([128, 4, TOK], BF16, "qT")
    P.dma(qT, D_(dr["qT"].rearrange("h p t -> p h t")))
    qcT = P.sb([128, 4, CTXL], BF16, "qcT")
    P.dma(qcT, D_(dr["qcT"].rearrange("h p t -> p h t")))

    SB = [P.psum_banks[i] for i in range(4)]
    ACC = [(P.psum_banks[4], P.psum_banks[5]), (P.psum_banks[6], P.psum_banks[7])]
    P.held.update([0, 1, 2, 3, 4, 5, 6, 7])
    sctr = [0]

    def attn_group(kTh, vh, qsrc, W, kts, m):
        accO, accL = ACC[m]
        lo, hi = m * 64, (m + 1) * 64
        n = len(kts)

        def S(i):
            sb_ = SB[sctr[0] % 4]
            sctr[0] += 1
            kt = kts[i]
            P.mm(sb_[:, 0:W], kTh[lo:hi, kt * 128:(kt + 1) * 128], qsrc[lo:hi, :])
            return sb_

        cur = S(0)
        for i in range(n):
            nxt = S(i + 1) if i + 1 < n else None
            pt = P.tmp("pt", [128, 512], BF16, n=3)[:, 0:W]
            P.act(pt, cur[:, 0:W], AF.Exp, scale=0.125)
            kt = kts[i]
            P.mm(accO[:, 0:W], vh[:, kt, :], pt, start=(i == 0), stop=(i == n - 1))
            P.mm(accL[:, 0:W], C.ones, pt, start=(i == 0), stop=(i == n - 1))
            cur = nxt
        return accO, accL

    def attn_tile(kTh, vh, qsrc_fn, W, kts, dst):
        om = []
        for m in range(2):
            accO, accL = attn_group(kTh, vh, qsrc_fn, W, kts, m)
            rl = P.tmp("rl", [128, 512], F32)[:, 0:W]
            P.recip(rl, accL[:, 0:W])
            o = P.tmp("om", [128, 512], F32, n=4)[:, 0:W]
            P.tt(o, accO[:, 0:W], rl, ALU.mult)
            om.append(o)
        o = P.tmp("od", [128, 512], F32)[:, 0:W]
        P.stt(o, om[1], nlam, om[0], ALU.mult, ALU.add)
        sq = P.tmp("asq", [128, 512], BF16)[:, 0:W]
        P.act(sq, o, AF.Square)
        ps = SB[sctr[0] % 4]
        sctr[0] += 1
        P.mm(ps[:, 0:W], C.ones, sq)
        rstd = P.tmp("arstd", [128, 512], F32)[:, 0:W]
        P.act(rstd, ps[:, 0:W], AF.Sqrt, bias=C.epsb, scale=1.0 / 128.0)
        P.recip(rstd, rstd)
        P.stt(dst, o, subn, rstd, ALU.mult, ALU.mult)

    for hh in range(4):
        kTh = P.tmp("kTh", [128, NKT * 128], BF16, n=1)
        vh = P.tmp("vh", [128, NKT, 128], BF16, n=1)
        P.dma(kTh, D_(dr["kT"][hh]))
        P.dma(vh, D_(dr["v"][hh]))
        for qt in range(TOK // 512):
            attn_tile(kTh, vh, qT[:, hh, qt * 512:(qt + 1) * 512], 512, list(range(NKT)),
                      mix[:, 4 + hh, qt * 512:(qt + 1) * 512])
        attn_tile(kTh, vh, qcT[:, hh, :], CTXL, [NKT - 2, NKT - 1], mixc[:, 4 + hh, :])
    P.held.clear()
    P.ps_i = 0
    P.release(m_att)

    tiles = [(lambda c, t=t: xres[:, c, t * 512:(t + 1) * 512], 512, False) for t in range(TOK // 512)]
    tiles.append((lambda c: xc[:, c, :], CTXL, True))
    for ti, (xf, W, is_ctx) in enumerate(tiles):
        src = mixc if is_ctx else mix[:, :, ti * 512:(ti + 1) * 512]
        G = G1c if is_ctx else G1
        for dc in range(8):
            ps = P.ps()
            for kc in range(8):
                P.mm(ps[:, 0:W], w_out[:, kc, dc * 128:(dc + 1) * 128], src[:, kc, :], start=(kc == 0), stop=(kc == 7))
            P.stt(xf(dc), ps[:, 0:W], G[:, dc:dc + 1], xf(dc), ALU.mult, ALU.add)
    if debug:
        P.add("sp", lambda e: e.dma_start(out=o_x1.rearrange("c p t -> p c t"), in_=xres.ap), reads=[xres], writes=[outb], dma=True)
    P.release(m_mix)
    moe_layer(C, dr, 0, tiles, A2, Sh2, G2, A2c, Sh2c, G2c)
    P.add("sp", lambda e: e.dma_start(out=o_x2.rearrange("c p t -> p c t"), in_=xres.ap), reads=[xres], writes=[outb], dma=True)
    if debug:
        P.add("sp", lambda e: e.dma_start(out=o_c2.rearrange("c p t -> p c t"), in_=xc.ap), reads=[xc], writes=[outb], dma=True)

    w1 = P.sb([128, 8, 2048], BF16, "w1in")
    for kc in range(8):
        P.dma(w1[:, kc, :], D_(dr["od_w_in"][kc * 128:(kc + 1) * 128, :]), q="pool")
    for ti, (xf, W, is_ctx) in enumerate(tiles):
        A, Sh = (A1nc, Sh1nc) if is_ctx else (A1n, Sh1n)
        h1 = P.tmp(f"h1_{W}", [128, 8, W], BF16)
        ps = P.ps()
        for c in range(8):
            s_ = P.tmp(f"sq{W}", [128, W], BF16)
            P.act(s_, xf(c), AF.Square)
            P.mm(ps[:, 0:W], C.ones, s_, start=(c == 0), stop=(c == 7))
        rstd = C.rstd_from_ps(ps[:, 0:W], W, 1.0 / D)
        for c in range(8):
            t = P.tmp(f"nt{W}", [128, W], F32)
            P.tt(t, xf(c), rstd, ALU.mult)
            P.act(h1[:, c, :], t, AF.Identity, bias=Sh[:, c:c + 1], scale=A[:, c:c + 1])
        for f in range(16):
            if is_ctx and f < 8:
                continue
            ps = P.ps()
            for kc in range(8):
                P.mm(ps[:, 0:W], w1[:, kc, f * 128:(f + 1) * 128], h1[:, kc, :], start=(kc == 0), stop=(kc == 7))
            if f < 8:
                yg = P.tmp("yg", [128, W], BF16, n=3)
                P.act(yg, ps[:, 0:W], AF.Gelu_apprx_tanh)
                P.add("sp", lambda e, yg=yg, f=f, ti=ti: e.dma_start(out=o_yg[f, :, ti * 512:(ti + 1) * 512], in_=yg.ap),
                      reads=[yg], writes=[outb], dma=True)
            else:
                uu = P.tmp(f"uu{W}", [128, W], F32, n=3)
                P.copy(uu, ps[:, 0:W], eng="act")
                if is_ctx:
                    P.add("sp", lambda e, uu=uu, f=f: e.dma_start(out=o_uc[f - 8], in_=uu.ap), reads=[uu], writes=[outb], dma=True)
                else:
                    P.add("sp", lambda e, uu=uu, f=f, ti=ti: e.dma_start(out=o_u[f - 8, :, ti * 512:(ti + 1) * 512], in_=uu.ap),
                          reads=[uu], writes=[outb], dma=True)
    P.emit(final_reads=[outb])
    return nc, P


_CACHE = {}


def _get(name, fn):
    if name not in _CACHE:
        _CACHE[name] = fn()
    return _CACHE[name]


def host_sel():
    sel = np.zeros((16, 16, 128), np.float32)
    for e in range(16):
        sel[e, e, :] = 1.0
    return sel


def l1_inputs(r, inp, cf, cb, cosT, sinT):
    b, j = r // 4, r % 4
    s0 = j * TOK
    xT = np.ascontiguousarray(inp["x"][b].T)
    xh = np.zeros((D, 2), np.float32)
    hm = np.zeros((128, 2), np.float32)
    if s0 > 0:
        xh[:, 0] = xT[:, s0 - 1]
        hm[:, 0] = 1
    if s0 + TOK < SEQ:
        xh[:, 1] = xT[:, s0 + TOK]
        hm[:, 1] = 1
    cvec = np.stack([chunkT(inp["c"][b]), chunkT(inp["c_ctx"])], axis=-1)
    return dict(cf=cf, cb=cb, xo=np.ascontiguousarray(xT[:, s0:s0 + TOK]), xh=xh, hmask=hm,
                ctx=np.ascontiguousarray(inp["ctx"][b].T), cvec=np.ascontiguousarray(cvec),
                ada_w=inp["ada_w"], ada_bT=np.ascontiguousarray(inp["ada_b"].reshape(2, 48, 128).transpose(0, 2, 1)),
                nmixT=np.stack([chunkT(inp["norm_mix"][l]) for l in range(2)]),
                w_in=inp["ev_w_in"][0],
                convw=np.ascontiguousarray(inp["ev_conv_w"][0].reshape(3, 4, 128).transpose(2, 1, 0)),
                convb=np.ascontiguousarray(chunkT(inp["ev_conv_b"][0])),
                qkn=np.ascontiguousarray(np.stack([np.tile(inp["ev_q_norm"][0], 2), np.tile(inp["ev_k_norm"][0], 2)], axis=-1)),
                cos=np.ascontiguousarray(cosT[:, s0:s0 + TOK]), sin=np.ascontiguousarray(sinT[:, s0:s0 + TOK]))


def moe_inputs(inp, l):
    wr = np.concatenate([inp["moe_w_grp"][l], inp["moe_w_rt"][l].reshape(D, 16)], axis=1)[None]
    rb = np.concatenate([inp["moe_b_grp"][l], inp["moe_b_rt"][l].reshape(16)])
    rb = np.ascontiguousarray(np.broadcast_to(rb[None, None, :], (1, 128, 20)))
    return dict(moe_wr=np.ascontiguousarray(wr), moe_rb=rb, sel=host_sel(),
                moe_wg=inp["moe_w_gate"][l:l + 1], moe_wu=inp["moe_w_up"][l:l + 1], moe_wd=inp["moe_w_down"][l:l + 1])


def l2_inputs(r, inp, o1, cf, cb, xo_list):
    b = r // 4
    grp = [o1[4 * b + j] for j in range(4)]
    kT = np.concatenate([g["o_kT"] for g in grp] + [grp[0]["o_kcT"]], axis=2)
    v = np.concatenate([g["o_v"] for g in grp] + [grp[0]["o_vc"]], axis=0)
    v = np.ascontiguousarray(v.reshape(NKT, 128, 4, 128).transpose(2, 1, 0, 3))
    d = dict(cf=cf, cb=cb, kT=np.ascontiguousarray(kT), v=v, qT=o1[r]["o_qT"], qcT=o1[r]["o_qcT"],
             oa=o1[r]["o_oa"], oac=o1[r]["o_oac"], xo=xo_list[r], ctx=np.ascontiguousarray(inp["ctx"][b].T),
             mods=o1[r]["o_mods"],
             lamp=np.ascontiguousarray(np.stack([inp["ev_lam_q1"][0], inp["ev_lam_k1"][0], inp["ev_lam_q2"][0], inp["ev_lam_k2"][0]], axis=-1)),
             subn=np.ascontiguousarray(inp["ev_sub_norm"][0].reshape(128, 1)),
             w_out=inp["ev_w_out"][0], nffnT=chunkT(inp["norm_ffn"][0]), nmix1T=chunkT(inp["norm_mix"][1]),
             od_w_in=inp["od_w_in"][0])
    d.update(moe_inputs(inp, 0))
    return d


LRU_PW = [2048]


def lru_params(C, dr):
    P = C.P
    prm = {}
    prm["cw"] = P.sb([128, 8, 2, 4], F32, "lcw")
    P.dma(prm["cw"], D_(dr["l_cw"]))
    for k in ("l_cb", "l_ba", "l_bx", "l_lam"):
        prm[k] = P.sb([128, 8, 2], F32, k)
        P.dma(prm[k], D_(dr[k]))
    prm["wa"] = P.sb([128, 2, 8, 128], BF16, "lwa")
    prm["wx"] = P.sb([128, 2, 8, 128], BF16, "lwx")
    P.dma(prm["wa"], D_(dr["l_wa"]), q="pool")
    P.dma(prm["wx"], D_(dr["l_wx"]), q="pool")
    e = P.sb([128, 8, 2], F32, "l_e")
    P.act(e, prm["l_lam"], AF.Exp, scale=-1.0)
    P.act(e, e, AF.Ln, bias=C.onesf[:, 0:1])
    prm["s1"] = P.sb([128, 8, 2], F32, "l_s1")
    prm["s2"] = P.sb([128, 8, 2], F32, "l_s2")
    P.ts(prm["s1"], e, -8.0, None, ALU.mult)
    P.ts(prm["s2"], e, -16.0, None, ALU.mult)
    return prm


def lru_drams(nc, dr):
    dr["l_cw"] = din(nc, "l_cw", [128, 8, 2, 4])
    for k in ("l_cb", "l_ba", "l_bx", "l_lam"):
        dr[k] = din(nc, k, [128, 8, 2])
    dr["l_wa"] = din(nc, "l_wa", [128, 2, 8, 128])
    dr["l_wx"] = din(nc, "l_wx", [128, 2, 8, 128])


def lru_coeffs(C, prm, ub, W, c, d, nb=2):
    P = C.P
    cw = prm["cw"]
    o0 = 0 if d == 0 else 3
    uc = P.tmp("l_uc", [128, LRU_PW[0]], F32, n=nb)[:, 0:W]
    P.ts(uc, ub[:, o0:o0 + W], cw[:, c, d, 0:1], prm["l_cb"][:, c, d:d + 1], ALU.mult, ALU.add)
    for k in range(1, 4):
        P.stt(uc, ub[:, o0 + k:o0 + k + W], cw[:, c, d, k:k + 1], uc, ALU.mult, ALU.add)
    ucb = P.tmp("l_ucb", [128, LRU_PW[0]], BF16, n=nb)[:, 0:W]
    P.copy(ucb, uc, eng="pool")
    r = P.tmp("l_r", [128, LRU_PW[0]], F32, n=nb)[:, 0:W]
    ig = P.tmp("l_ig", [128, LRU_PW[0]], F32, n=nb)[:, 0:W]
    for t0 in range(0, W, 512):
        w = min(512, W - t0)
        ps = P.ps()
        P.mm(ps[:, 0:w], prm["wa"][:, d, c, :], ucb[:, t0:t0 + w])
        P.act(r[:, t0:t0 + w], ps[:, 0:w], AF.Sigmoid, bias=prm["l_ba"][:, c, d:d + 1])
        ps2 = P.ps()
        P.mm(ps2[:, 0:w], prm["wx"][:, d, c, :], ucb[:, t0:t0 + w])
        P.act(ig[:, t0:t0 + w], ps2[:, 0:w], AF.Sigmoid, bias=prm["l_bx"][:, c, d:d + 1])
    a = P.tmp("l_a", [128, LRU_PW[0]], F32, n=nb)[:, 0:W]
    P.act(a, r, AF.Exp, scale=prm["s1"][:, c, d:d + 1])
    t = P.tmp("l_t", [128, LRU_PW[0]], F32, n=nb)[:, 0:W]
    P.act(t, r, AF.Exp, scale=prm["s2"][:, c, d:d + 1])
    P.act(t, t, AF.Sqrt, bias=C.onesf[:, 0:1], scale=-1.0)
    P.tt(ig, ig, uc, ALU.mult)
    P.tt(ig, ig, t, ALU.mult)
    return a, ig


def lru_scan(C, a, bb, W, d, init, out):
    P = C.P
    if d == 0:
        P.scan(out, a, bb, init)
    else:
        P.scan(out[:, ::-1], a[:, ::-1], bb[:, ::-1], init)


def load_ub(C, dr_u, dr_hp, dr_hn, c, W, key, nb=2):
    P = C.P
    ub = P.tmp(key, [128, W + 6], F32, n=nb)
    if dr_hp is None:
        P.memset(ub[:, 0:3], 0.0)
        P.memset(ub[:, W + 3:W + 6], 0.0)
    else:
        P.dma(ub[:, 0:3], D_(dr_hp[c]))
        P.dma(ub[:, W + 3:W + 6], D_(dr_hn[c]))
    P.dma(ub[:, 3:3 + W], D_(dr_u[c]))
    return ub


def build_L3():
    nc = bass.Bass("TRN2", target_bir_lowering=False)
    dr = {}
    dr["cf"] = din(nc, "cf", [128, 384])
    dr["cb"] = din(nc, "cb", [128, 256])
    dr["u"] = din(nc, "u", [8, 128, TOK])
    dr["uhp"] = din(nc, "uhp", [8, 128, 3])
    dr["uhn"] = din(nc, "uhn", [8, 128, 3])
    dr["uc"] = din(nc, "uc", [8, 128, CTXL])
    lru_drams(nc, dr)
    o_sum = dout(nc, "o_sum", [128, 8, 2, 3])
    P = Prog(nc)
    C = Ctx(nc, P, dr)
    outb = T(None, [Buf()])
    prm = lru_params(C, dr)
    summ = P.sb([128, 8, 2, 3], F32, "summ")
    for c in range(8):
        ubc = load_ub(C, dr["uc"], None, None, c, CTXL, "ubc")
        ub = load_ub(C, dr["u"], dr["uhp"], dr["uhn"], c, TOK, "ub")
        for d in range(2):
            a, bb = lru_coeffs(C, prm, ubc, CTXL, c, d)
            h = P.tmp("l_h", [128, 2048], F32)[:, 0:CTXL]
            lru_scan(C, a, bb, CTXL, d, 0.0, h)
            P.copy(summ[:, c, d, 2:3], h[:, CTXL - 1:CTXL] if d == 0 else h[:, 0:1])
            a, bb = lru_coeffs(C, prm, ub, TOK, c, d)
            h = P.tmp("l_h", [128, 2048], F32)[:, 0:TOK]
            lru_scan(C, a, bb, TOK, d, 0.0, h)
            P.copy(summ[:, c, d, 1:2], h[:, TOK - 1:TOK] if d == 0 else h[:, 0:1])
            P.reduce(summ[:, c, d, 0:1], a, ALU.mult)
    P.add("sp", lambda e: e.dma_start(out=o_sum, in_=summ.ap), reads=[summ], writes=[outb], dma=True)
    P.emit(final_reads=[outb])
    return nc, P


def build_L4():
    nc = bass.Bass("TRN2", target_bir_lowering=False)
    dr = {}
    dr["cf"] = din(nc, "cf", [128, 384])
    dr["cb"] = din(nc, "cb", [128, 256])
    dr["u"] = din(nc, "u", [8, 128, TOK])
    dr["uhp"] = din(nc, "uhp", [8, 128, 3])
    dr["uhn"] = din(nc, "uhn", [8, 128, 3])
    dr["yg"] = din(nc, "yg", [8, 128, TOK], BF16)
    dr["x2"] = din(nc, "x2", [8, 128, TOK])
    dr["summ"] = din(nc, "summ", [4, 128, 8, 2, 3])
    dr["jmask"] = din(nc, "jmask", [128, 2, 4])
    dr["mods"] = din(nc, "mods", [2, 128, 48, 2])
    dr["nffnT"] = din(nc, "nffnT", [128, 8])
    dr["w_out"] = din(nc, "w_out", [D, D])
    lru_drams(nc, dr)
    moe_drams(nc, dr)
    o_out = dout(nc, "o_out", [8, 128, TOK])
    P = Prog(nc)
    C = Ctx(nc, P, dr)
    outb = T(None, [Buf()])
    prm = lru_params(C, dr)
    mods1 = P.sb([128, 48, 2], F32, "mods1")
    P.dma(mods1, D_(dr["mods"][1]))
    nffn = P.sb([128, 8], F32, "nffn")
    P.dma(nffn, D_(dr["nffnT"]))
    _, _, G1 = mod_scalars(C, mods1, nffn, 0, 0, "m1l")
    A2, Sh2, G2 = mod_scalars(C, mods1, nffn, 1, 0, "f1l")
    sm = P.sb([128, 4, 8, 2, 3], F32, "sm")
    for j in range(4):
        P.dma(sm[:, j], D_(dr["summ"][j]))
    jm = P.sb([128, 2, 4], F32, "jm")
    P.dma(jm, D_(dr["jmask"]))
    h0 = P.sb([128, 8, 2], F32, "h0")
    cand = P.sb([128, 8], F32, "cand")
    for d in range(2):
        P.copy(h0[:, :, d], sm[:, 0, :, d, 2])
        order = range(4) if d == 0 else range(3, -1, -1)
        for j in order:
            P.tt(cand, sm[:, j, :, d, 0], h0[:, :, d], ALU.mult)
            P.tt(cand, cand, sm[:, j, :, d, 1], ALU.add)
            P.tt(cand, cand, h0[:, :, d], ALU.subtract)
            P.stt(h0[:, :, d], cand, jm[:, d, j:j + 1], h0[:, :, d], ALU.mult, ALU.add)
    xres = P.sb([128, 8, TOK], F32, "xres")
    P.dma(xres, D_(dr["x2"].rearrange("c p t -> p c t")))
    m_mix = P.mark()
    w_out = P.sb([128, 8, D], BF16, "w_out")
    for kc in range(8):
        P.dma(w_out[:, kc, :], D_(dr["w_out"][kc * 128:(kc + 1) * 128, :]), q="pool")
    mix = P.sb([128, 8, TOK], BF16, "mix")
    m_sc = P.mark()
    for c in range(8):
        ub = load_ub(C, dr["u"], dr["uhp"], dr["uhn"], c, TOK, "ub", nb=1)
        yg = P.tmp("ygl", [128, TOK], BF16, n=1)
        P.dma(yg, D_(dr["yg"][c]))
        hs = []
        for d in range(2):
            a, bb = lru_coeffs(C, prm, ub, TOK, c, d, nb=1)
            h = P.tmp("l_h", [128, 2048], F32)
            lru_scan(C, a, bb, TOK, d, h0[:, c, d:d + 1], h)
            hs.append(h)
        P.tt(hs[0], hs[0], hs[1], ALU.add)
        P.tt(mix[:, c, :], hs[0], yg, ALU.mult)
    P.release(m_sc)
    tiles = [(lambda c, t=t: xres[:, c, t * 512:(t + 1) * 512], 512, False) for t in range(TOK // 512)]
    for ti, (xf, W, _) in enumerate(tiles):
        for dc in range(8):
            ps = P.ps()
            for kc in range(8):
                P.mm(ps[:, 0:W], w_out[:, kc, dc * 128:(dc + 1) * 128], mix[:, kc, ti * 512:(ti + 1) * 512],
                     start=(kc == 0), stop=(kc == 7))
            P.stt(xf(dc), ps[:, 0:W], G1[:, dc:dc + 1], xf(dc), ALU.mult, ALU.add)
    P.release(m_mix)
    moe_layer(C, dr, 0, tiles, A2, Sh2, G2)
    P.add("sp", lambda e: e.dma_start(out=o_out.rearrange("c p t -> p c t"), in_=xres.ap), reads=[xres], writes=[outb], dma=True)
    P.emit(final_reads=[outb])
    return nc, P


def lru_inputs(inp):
    cw = np.ascontiguousarray(inp["od_conv_w"][0].reshape(2, 4, 8, 128).transpose(3, 2, 0, 1))

    def v(a):
        return np.ascontiguousarray(a.reshape(2, 8, 128).transpose(2, 1, 0))

    def w(a):
        return np.ascontiguousarray(a.transpose(2, 0, 1, 3))
    return dict(l_cw=cw, l_cb=v(inp["od_conv_b"][0]), l_ba=v(inp["od_b_a"][0]), l_bx=v(inp["od_b_x"][0]),
                l_lam=v(inp["od_lam"][0]), l_wa=w(inp["od_w_a"][0]), l_wx=w(inp["od_w_x"][0]))


def halo_inputs(r, o2):
    b, j = r // 4, r % 4
    z = np.zeros((8, 128, 3), np.float32)
    hp = np.ascontiguousarray(o2[r - 1]["o_u"][:, :, TOK - 3:TOK]) if j > 0 else z
    hn = np.ascontiguousarray(o2[r + 1]["o_u"][:, :, 0:3]) if j < 3 else z
    return hp, hn


def kernel_unfused(**inp):
    inp = {k: np.asarray(v) for k, v in inp.items()}
    cf, cb = host_consts()
    cosT, sinT = rope_tables()
    cores = list(range(NCORE))
    nc1, _ = _get("L1", build_L1)
    ims1 = [l1_inputs(r, inp, cf, cb, cosT, sinT) for r in cores]
    o1 = run_bass_kernel_spmd(nc1, ims1, core_ids=cores).results
    xo_list = [im["xo"] for im in ims1]
    nc2, _ = _get("L2", build_L2)
    ims2 = [l2_inputs(r, inp, o1, cf, cb, xo_list) for r in cores]
    o2 = run_bass_kernel_spmd(nc2, ims2, core_ids=cores).results
    del ims1, ims2
    lin = lru_inputs(inp)
    nc3, _ = _get("L3", build_L3)
    ims3 = []
    for r in cores:
        hp, hn = halo_inputs(r, o2)
        d = dict(cf=cf, cb=cb, u=o2[r]["o_u"], uhp=hp, uhn=hn, uc=o2[r]["o_uc"])
        d.update(lin)
        ims3.append(d)
    o3 = run_bass_kernel_spmd(nc3, ims3, core_ids=cores).results
    nc4, _ = _get("L4", build_L4)
    ims4 = []
    for r in cores:
        b, j = r // 4, r % 4
        hp, hn = halo_inputs(r, o2)
        summ = np.ascontiguousarray(np.stack([o3[4 * b + jj]["o_sum"] for jj in range(4)], axis=0))
        jm = np.zeros((128, 2, 4), np.float32)
        for jj in range(4):
            jm[:, 0, jj] = 1.0 if jj < j else 0.0
            jm[:, 1, jj] = 1.0 if jj > j else 0.0
        d = dict(cf=cf, cb=cb, u=o2[r]["o_u"], uhp=hp, uhn=hn, yg=o2[r]["o_yg"], x2=o2[r]["o_x2"], summ=summ, jmask=jm,
                 mods=o1[r]["o_mods"], nffnT=chunkT(inp["norm_ffn"][1]), w_out=inp["od_w_out"][0])
        d.update(lin)
        d.update(moe_inputs(inp, 1))
        ims4.append(d)
    o4 = run_bass_kernel_spmd(nc4, ims4, core_ids=cores).results
    out = np.empty((2, SEQ, D), np.float32)
    for r in cores:
        b, j = r // 4, r % 4
        out[b, j * TOK:(j + 1) * TOK, :] = o4[r]["o_out"].reshape(D, TOK).T
    return out


GROUPS = [[0, 1, 2, 3], [4, 5, 6, 7]]
HALF = 512


def qk_stage_a(C, ps_in, W, gain, rope, out):
    P = C.P
    k_sb = P.tmp(f"qk_k{W}", [128, W], F32, n=2)
    P.copy(k_sb, ps_in, eng="act")
    sq = P.tmp(f"qk_sq{W}", [128, W], BF16)
    P.act(sq, ps_in, AF.Square)
    ps2 = P.ps()
    P.mm(ps2[:, 0:W], C.bones, sq)
    rs = C.rstd_from_ps(ps2[:, 0:W], W, 1.0 / 64.0, name="qk_rs", n=2)
    if not rope:
        P.stt(out, k_sb, gain, rs, ALU.mult, ALU.mult)
        return None
    kh = P.tmp(f"qk_kh{W}", [128, W], F32, n=2)
    P.stt(kh, k_sb, gain, rs, ALU.mult, ALU.mult)
    ps3 = P.ps()
    P.mm(ps3[:, 0:W], C.perm, kh)
    return dict(kh=kh, ps3=ps3, out=out)


def qk_stage_b(C, st, W, cos, sin):
    P = C.P
    t1 = P.tmp(f"qk_t1{W}", [128, W], F32, n=2)
    P.tt(t1, st["kh"], cos, ALU.mult)
    t2 = P.tmp(f"qk_t2{W}", [128, W], F32, n=2)
    P.tt(t2, st["ps3"][:, 0:W], sin, ALU.mult)
    P.tt(st["out"], t1, t2, ALU.add)


def build_fused():
    nc = bass.Bass("TRN2", target_bir_lowering=False)
    dr = {}
    dr["cf"] = din(nc, "cf", [128, 384])
    dr["cb"] = din(nc, "cb", [128, 256])
    dr["xo"] = din(nc, "xo", [D, TOK])
    dr["xh"] = din(nc, "xh", [D, 2])
    dr["hmask"] = din(nc, "hmask", [128, 2])
    dr["ctx"] = din(nc, "ctx", [D, CTXL])
    dr["cvec"] = din(nc, "cvec", [128, 8, 2])
    dr["ada_wq"] = din(nc, "ada_wq", [2, D, 1536])
    dr["ada_bq"] = din(nc, "ada_bq", [128, 2, 12])
    dr["nmixT"] = din(nc, "nmixT", [2, 128, 8])
    dr["nffnT"] = din(nc, "nffnT", [2, 128, 8])
    dr["w_in"] = din(nc, "w_in", [D, 3072])
    dr["convw"] = din(nc, "convw", [128, 4, 3])
    dr["convb"] = din(nc, "convb", [128, 4])
    dr["qkn"] = din(nc, "qkn", [128, 2])
    dr["cos"] = din(nc, "cos", [128, TOK])
    dr["sin"] = din(nc, "sin", [128, TOK])
    dr["lamp"] = din(nc, "lamp", [64, 4])
    dr["subn"] = din(nc, "subn", [128, 1])
    dr["w_out"] = din(nc, "w_out", [D, D])
    dr["od_w_in"] = din(nc, "od_w_in", [D, 2048])
    dr["od_w_out"] = din(nc, "od_w_out", [D, D])
    dr["jmask"] = din(nc, "jmask", [128, 2, 4])
    dr["hsel"] = din(nc, "hsel", [128, 2, 4])
    lru_drams(nc, dr)
    dr["moe_wr"] = din(nc, "moe_wr", [2, D, 20])
    dr["moe_rb"] = din(nc, "moe_rb", [2, 128, 20])
    dr["sel"] = din(nc, "sel", [16, 16, 128])
    dr["moe_wg"] = din(nc, "moe_wg", [2, 16, D, 512])
    dr["moe_wu"] = din(nc, "moe_wu", [2, 16, D, 512])
    dr["moe_wd"] = din(nc, "moe_wd", [2, 16, 512, D])
    o_out = dout(nc, "o_out", [8, 128, TOK])
    kv_in = [nc.dram_tensor(f"kv_in{i}", [1024, 512], BF16).ap() for i in range(4)]
    kv_all = [nc.dram_tensor(f"kv_all{i}", [4 * 1024, 512], BF16).ap() for i in range(4)]
    hx_in = nc.dram_tensor("hx_in", [128, 48], F32).ap()
    hx_all = nc.dram_tensor("hx_all", [4 * 128, 48], F32).ap()
    sm_in = nc.dram_tensor("sm_in", [128, 48], F32).ap()
    sm_all = nc.dram_tensor("sm_all", [4 * 128, 48], F32).ap()
    kv_in_t = [T(kv_in[i], [Buf()]) for i in range(4)]
    kv_all_t = [T(kv_all[i], [Buf()]) for i in range(4)]
    hx_in_t, hx_all_t = T(hx_in, [Buf()]), T(hx_all, [Buf()])
    sm_in_t, sm_all_t = T(sm_in, [Buf()]), T(sm_all, [Buf()])
    mq_in = nc.dram_tensor("mq_in", [128, 48], F32).ap()
    mq_all = nc.dram_tensor("mq_all", [4 * 128, 48], F32).ap()
    mq_in_t, mq_all_t = T(mq_in, [Buf()]), T(mq_all, [Buf()])

    P = Prog(nc)
    C = Ctx(nc, P, dr)
    outb = T(None, [Buf()])
    LAM_INIT = 0.8 - 0.6 * math.exp(-0.3 * 0)

    def allgather(src_t, dst_t):
        P.add("pool", lambda e: e.collective_compute("AllGather", ALU.bypass, replica_groups=GROUPS,
                                                     ins=[src_t.ap.opt()], outs=[dst_t.ap.opt()]),
              reads=[src_t], writes=[dst_t], cc=True)

    cvec = P.sb([128, 8, 2], F32, "cvec")
    P.dma(cvec, D_(dr["cvec"]))
    mods = [P.sb([128, 48, 2], F32, f"mods{l}") for l in range(2)]
    m_md = P.mark()
    scv = P.sb([128, 8, 2], F32, "silu_c")
    P.act(scv, cvec, AF.Silu)
    bq = P.sb([128, 2, 12], F32, "bq")
    P.dma(bq, D_(dr["ada_bq"]))
    psm = P.ps(hold=True)
    for l in range(2):
        for hf_ in range(2):
            wq = P.tmp("adawq", [128, 8, 768], F32, n=2)
            P.dma(wq, D_(dr["ada_wq"][l].rearrange("(kc p) f -> p kc f", p=128)[:, :, hf_ * 768:(hf_ + 1) * 768]))
            for fc in range(6):
                g = l * 12 + hf_ * 6 + fc
                for kc in range(8):
                    P.mm(psm[:, 2 * g:2 * g + 2], wq[:, kc, fc * 128:(fc + 1) * 128], scv[:, kc, :],
                         start=(kc == 0), stop=(kc == 7))
    mq = P.sb([128, 2, 12, 2], F32, "mq")
    P.tt(mq, psm[:, 0:48].re("p (l g c) -> p l g c", l=2, c=2),
         T(bq.ap.unsqueeze(3).to_broadcast([128, 2, 12, 2]), bq.bufs), ALU.add)
    P.ps_free(psm)
    P.dma(mq_in_t, mq.re("p l g c -> p (l g c)"))
    allgather(mq_in_t, mq_all_t)
    mqa = P.sb([128, 4, 48], F32, "mqa")
    P.dma(mqa, T(mq_all.rearrange("(r p) f -> p r f", p=128), mq_all_t.bufs))
    for l in range(2):
        P.copy(mods[l].re("p (r i) c -> p r i c", r=4), mqa[:, :, l * 24:(l + 1) * 24].re("p r (i c) -> p r i c", c=2))
    P.release(m_md)
    nmix = [P.sb([128, 8], F32, f"nmix{l}") for l in range(2)]
    nffn = [P.sb([128, 8], F32, f"nffn{l}") for l in range(2)]
    for l in range(2):
        P.dma(nmix[l], D_(dr["nmixT"][l]))
        P.dma(nffn[l], D_(dr["nffnT"][l]))
    A_lat, Sh_lat, G1 = mod_scalars(C, mods[0], nmix[0], 0, 0, "a0l")
    A_ctx, Sh_ctx, G1c = mod_scalars(C, mods[0], nmix[0], 0, 1, "a0c")
    A2, Sh2, G2 = mod_scalars(C, mods[0], nffn[0], 1, 0, "f0l")
    A2c, Sh2c, G2c = mod_scalars(C, mods[0], nffn[0], 1, 1, "f0c")
    A1n, Sh1n, G1n = mod_scalars(C, mods[1], nmix[1], 0, 0, "a1l")
    A1nc, Sh1nc, _ = mod_scalars(C, mods[1], nmix[1], 0, 1, "a1c")
    A2n, Sh2n, G2n = mod_scalars(C, mods[1], nffn[1], 1, 0, "f1l")
    convw = P.sb([128, 4, 3], F32, "convw")
    P.dma(convw, D_(dr["convw"]))
    convb = P.sb([128, 4], F32, "convb")
    P.dma(convb, D_(dr["convb"]))
    qkn = P.sb([128, 2], F32, "qkn")
    P.dma(qkn, D_(dr["qkn"]))
    hmask = P.sb([128, 2], F32, "hmask")
    P.dma(hmask, D_(dr["hmask"]))
    jm = P.sb([128, 2, 4], F32, "jm")
    P.dma(jm, D_(dr["jmask"]))
    hsel = P.sb([128, 2, 4], F32, "hsel")
    P.dma(hsel, D_(dr["hsel"]))
    lamp = P.sb([64, 4], F32, "lamp")
    P.dma(lamp, D_(dr["lamp"]))
    lpr = P.sb([64, 2], F32, "lpr")
    P.tt(lpr[:, 0:1], lamp[:, 0:1], lamp[:, 1:2], ALU.mult)
    P.tt(lpr[:, 1:2], lamp[:, 2:3], lamp[:, 3:4], ALU.mult)
    psl = P.ps()
    P.mm(psl[:, 0:2], C.onesf[0:64, :], lpr)
    lex = P.sb([128, 2], F32, "lex")
    P.act(lex, psl[:, 0:2], AF.Exp)
    nlam = P.sb([128, 1], F32, "nlam")
    P.tt(nlam, lex[:, 1:2], lex[:, 0:1], ALU.subtract)
    P.ts(nlam, nlam, -LAM_INIT, None, ALU.add)
    subn = P.sb([128, 1], F32, "subn")
    P.dma(subn, D_(dr["subn"]))
    P.ts(subn, subn, 1.0 - LAM_INIT, None, ALU.mult)

    m_base = P.mark()
    mix = P.sb([128, 4, TOK], BF16, "mixa")
    mixc = P.sb([128, 4, CTXL], BF16, "mixca")
    qT = P.sb([128, 4, TOK], BF16, "qT")
    qcT = P.sb([128, 4, CTXL], BF16, "qcT")
    kcT = P.sb([128, 4, CTXL], BF16, "kcT")
    vc = P.sb([128, 2, 512], BF16, "vc")
    m_front = P.mark()
    cbs = P.sb([128, 4, TOK], BF16, "cbs")
    ub = P.sb([128, 4, TOK + 2], BF16, "ub")
    cbc = P.sb([128, 4, CTXL], BF16, "cbc")
    ubc = P.sb([128, 4, CTXL + 2], BF16, "ubc")
    P.memset(ubc[:, :, 0:1], 0.0)
    P.memset(ubc[:, :, CTXL + 1:CTXL + 2], 0.0)
    w_in = P.sb([128, 8, 3072], BF16, "w_in")
    for kc in range(8):
        P.dma(w_in[:, kc, :], D_(dr["w_in"][kc * 128:(kc + 1) * 128, :]), q="pool")

    def proj(h, W, f):
        ps = P.ps()
        for kc in range(8):
            P.mm(ps[:, 0:W], w_in[:, kc, f * 128:(f + 1) * 128], h[:, kc, 0:W], start=(kc == 0), stop=(kc == 7))
        return ps[:, 0:W]

    vin_v = [kv_in[t][512:1024, :].rearrange("(h p) (k v) -> p k h v", p=128, v=128) for t in range(4)]

    def front_norm(xsrc, W, A, Sh):
        xt = P.tmp(f"xt{W}", [128, 8, W], F32, n=1)
        P.dma(xt, D_(xsrc.rearrange("(c p) t -> p c t", p=128)))
        h = P.tmp(f"h{W}", [128, 8, W], BF16, n=2)
        norm_mod(C, xt, W, A, Sh, h)
        return h

    def front_tile(xsrc, W, A, Sh, rope, dst, pre_h=None, mid_hook=None):
        t0 = dst["t0"]
        if rope:
            cs_c = P.tmp("cos_t", [128, W], F32, n=1)
            cs_s = P.tmp("sin_t", [128, W], F32, n=1)
            P.dma(cs_c, D_(dr["cos"][:, t0:t0 + W]))
            P.dma(cs_s, D_(dr["sin"][:, t0:t0 + W]))
        h = pre_h if pre_h is not None else front_norm(xsrc, W, A, Sh)
        if dst.get("cb") is not None:
            for c in range(4):
                ps = proj(h, W, c)
                P.copy(dst["cb"][:, c, t0:t0 + W], ps, eng="act")
        for c in range(4):
            ps_cc = proj(h, W, 4 + c)
            cc = P.tmp(f"cc{W}", [128, W], F32)
            P.copy(cc, ps_cc, eng="act")
            ps_cx = proj(h, W, 8 + c)
            P.tt(dst["u"](c), cc, ps_cx, ALU.mult)
        if mid_hook is not None:
            mid_hook()
        if dst.get("q") is None:
            return
        hu = [(12 + hh, 0, hh) for hh in range(4)] + [(16 + hh, 1, hh) for hh in range(4)]

        def unit_out(kind, hh):
            if kind == 0:
                return dst["q"][:, hh, t0:t0 + W], None
            if dst.get("k") is not None:
                return dst["k"][:, hh, t0:t0 + W], None
            kt_ = P.tmp("kt_", [128, W], BF16, n=3)
            return kt_, hh

        def finish(o, hh_store):
            if hh_store is not None:
                nb_ = Buf()
                kv_in_t[t0 // 512].bufs.append(nb_)
                P.add("sp", lambda e, o=o, hh=hh_store, t0=t0: e.dma_start(out=kv_in[t0 // 512][hh * 128:(hh + 1) * 128, :], in_=o.ap),
                      reads=[o], writes=[T(None, [nb_])], dma=True)

        ps_next = proj(h, W, hu[0][0])
        prev = None
        for i, (f_, kind, hh) in enumerate(hu):
            ps_cur = ps_next
            if i + 1 < len(hu):
                ps_next = proj(h, W, hu[i + 1][0])
            o, hs_ = unit_out(kind, hh)
            st = qk_stage_a(C, ps_cur, W, qkn[:, kind:kind + 1], rope, o)
            if prev is not None:
                qk_stage_b(C, prev[0], W, cs_c, cs_s)
                finish(prev[1], prev[2])
            if st is None:
                finish(o, hs_)
                prev = None
            else:
                prev = (st, o, hs_)
        if prev is not None:
            qk_stage_b(C, prev[0], W, cs_c, cs_s)
            finish(prev[1], prev[2])
        for sub in range(W // 128):
            ps = P.ps()
            for kc in range(8):
                P.mm(ps, h[:, kc, sub * 128:(sub + 1) * 128], w_in[:, kc, 2560:3072], start=(kc == 0), stop=(kc == 7))
            if dst.get("v") is not None:
                P.copy(dst["v"][:, sub, :], ps, eng="act")
            else:
                vs = P.tmp("vs", [128, 512], BF16, n=2)
                P.copy(vs, ps, eng="act")
                nb_ = Buf()
                kv_in_t[t0 // 512].bufs.append(nb_)
                P.add("sp", lambda e, vs=vs, sub=sub, t0=t0: e.dma_start(
                    out=vin_v[t0 // 512][:, sub], in_=vs.ap.rearrange("p (h v) -> p h v", v=128)),
                    reads=[vs], writes=[T(None, [nb_])], dma=True)

    def conv_piece(ubuf, cbt, s0, w, dstm):
        for c in range(4):
            acc = P.tmp("cacc", [128, 512], F32, n=1)[:, 0:w]
            P.ts(acc, ubuf[:, c, 1 + s0:1 + s0 + w], convw[:, c, 1:2], convb[:, c:c + 1], ALU.mult, ALU.add)
            P.stt(acc, ubuf[:, c, s0:s0 + w], convw[:, c, 0:1], acc, ALU.mult, ALU.add)
            P.stt(acc, ubuf[:, c, 2 + s0:2 + s0 + w], convw[:, c, 2:3], acc, ALU.mult, ALU.add)
            P.tt(dstm[:, c, s0:s0 + w], acc, cbt[:, c, s0:s0 + w], ALU.mult)

    uh = P.sb([128, 4, 2], F32, "uh")
    m_own = P.mark()
    front_tile(dr["ctx"], CTXL, A_ctx, Sh_ctx, False,
               dict(cb=cbc, u=lambda c: ubc[:, c, 1:1 + CTXL], q=qcT, k=kcT, v=vc, t0=0))
    P.release(m_own)
    nxt_h = [front_norm(dr["xo"][:, 0:512], 512, A_lat, Sh_lat)]
    for t in range(TOK // 512):
        cur_h = nxt_h[0]

        def hook(t=t):
            if t + 1 < TOK // 512:
                nxt_h[0] = front_norm(dr["xo"][:, (t + 1) * 512:(t + 2) * 512], 512, A_lat, Sh_lat)
        front_tile(dr["xo"][:, t * 512:(t + 1) * 512], 512, A_lat, Sh_lat, True,
                   dict(cb=cbs, u=lambda c, t=t: ub[:, c, 1 + t * 512:1 + (t + 1) * 512], q=qT, k=None, v=None, t0=t * 512),
                   pre_h=cur_h, mid_hook=hook)
        allgather(kv_in_t[t], kv_all_t[t])
        if t == 1:
            front_tile(dr["xh"], 2, A_lat, Sh_lat, False, dict(u=lambda c: uh[:, c, :], t0=0))
            for c in range(4):
                P.tt(ub[:, c, 0:1], uh[:, c, 0:1], hmask[:, 0:1], ALU.mult)
                P.tt(ub[:, c, TOK + 1:TOK + 2], uh[:, c, 1:2], hmask[:, 1:2], ALU.mult)
        if t >= 1:
            conv_piece(ub, cbs, (t - 1) * 512, 512, mix)

    conv_piece(ub, cbs, TOK - 512, 512, mix)
    conv_piece(ubc, cbc, 0, CTXL, mixc)
    P.release(m_front)

    top0 = P.top
    xres = P.sb_top([128, 8, TOK], F32, "xres")
    top_x = P.top
    xc = P.sb_top([128, 8, CTXL], F32, "xc")
    xc_off = P.top
    w_out = P.sb([128, 8, D], BF16, "w_out")
    for kc in range(8):
        P.dma(w_out[:, kc, :], D_(dr["w_out"][kc * 128:(kc + 1) * 128, :]), q="pool")
    mixb = P.sb([128, 4, TOK], BF16, "mixb")
    mixcb = P.sb([128, 4, CTXL], BF16, "mixcb")
    m_att = P.mark()
    SB = [P.psum_banks[i] for i in range(4)]
    ACC = [(P.psum_banks[4], P.psum_banks[6]), (P.psum_banks[5], P.psum_banks[7])]
    P.held.update(range(8))
    sctr = [0]

    def attn_group(ksrc, vsrc, qsrc, W, kts, m):
        accO, accL = ACC[m]
        lo, hi = m * 64, (m + 1) * 64
        n = len(kts)

        qz = P.tmp("qz", [128, 512], BF16, n=1)[:, 0:W]
        P.memset(qz, 0.0, eng="pool")
        P.copy(qz[lo:hi, :], qsrc[lo:hi, :], eng="pool")

        def S_(i):
            sb_ = SB[sctr[0] % 4]
            sctr[0] += 1
            P.mm(sb_[:, 0:W], ksrc(kts[i]), qz)
            return sb_

        LOOK = 2
        pend_pt = []
        first_acc = [True]
        sq_ = [S_(i) for i in range(min(LOOK, n))]
        for i in range(n):
            if i + LOOK < n:
                sq_.append(S_(i + LOOK))
            cur = sq_[i]
            pt = P.tmp("pt", [128, 512], BF16, n=6)[:, 0:W]
            P.act(pt, cur[:, 0:W], AF.Exp, scale=0.125)
            P.mm(accO[:, 0:W], vsrc(kts[i]), pt, start=(i == 0), stop=(i == n - 1))
            pend_pt.append(pt)
            if len(pend_pt) == 4 or i == n - 1:
                lvl = list(pend_pt)
                pend_pt = []
                while len(lvl) > 2:
                    nl = []
                    for j2 in range(0, len(lvl) - 1, 2):
                        pp = P.tmp("pp", [128, 512], BF16, n=4)[:, 0:W]
                        P.tt(pp, lvl[j2], lvl[j2 + 1], ALU.add)
                        nl.append(pp)
                    if len(lvl) % 2 == 1:
                        nl.append(lvl[-1])
                    lvl = nl
                if first_acc[0]:
                    lacc = P.tmp("lacc", [128, 512], F32, n=1)[:, 0:W]
                    if len(lvl) == 2:
                        P.tt(lacc, lvl[0], lvl[1], ALU.add)
                    else:
                        P.copy(lacc, lvl[0])
                    first_acc[0] = False
                else:
                    if len(lvl) == 2:
                        pp = P.tmp("pp", [128, 512], BF16, n=4)[:, 0:W]
                        P.tt(pp, lvl[0], lvl[1], ALU.add)
                        lvl = [pp]
                    P.tt(lacc, lacc, lvl[0], ALU.add)
        P.mm(accL[:, 0:W], C.onesf, lacc)
        return accO, accL

    def attn_tile(ksrc, vsrc, qsrc, W, kts, dst):
        om = []
        for m in range(2):
            accO, accL = attn_group(ksrc, vsrc, qsrc, W, kts, m)
            rl = P.tmp("rl", [128, 512], F32, n=1)[:, 0:W]
            P.act(rl, accL[:, 0:W], AF.Ln)
            P.act(rl, rl, AF.Exp, scale=-1.0)
            o = P.tmp("om", [128, 512], F32, n=2)[:, 0:W]
            P.tt(o, accO[:, 0:W], rl, ALU.mult)
            om.append(o)
        o = P.tmp("od", [128, 512], F32, n=1)[:, 0:W]
        P.stt(o, om[1], nlam, om[0], ALU.mult, ALU.add)
        sq = P.tmp("asq", [128, 512], BF16, n=1)[:, 0:W]
        P.act(sq, o, AF.Square)
        ps = SB[sctr[0] % 4]
        sctr[0] += 1
        P.mm(ps[:, 0:W], C.ones, sq)
        rstd = P.tmp("arstd", [128, 512], F32, n=1)[:, 0:W]
        P.act(rstd, ps[:, 0:W], AF.Ln, bias=C.epsb, scale=1.0 / 128.0)
        P.act(rstd, rstd, AF.Exp, scale=-0.5)
        P.stt(dst, o, subn, rstd, ALU.mult, ALU.mult)

    kall_v = [kv_all[t].rearrange("(r s p) c -> s p r c", r=4, p=128) for t in range(4)]
    vall_v = [kv_all[t].rearrange("(r s p) (k v) -> s p r k v", r=4, p=128, v=128) for t in range(4)]
    NK0 = SEQ // 128
    NK0 = SEQ // 128
    KORDER = [r * 16 + t * 4 + kk for t in range(4) for r in range(4) for kk in range(4)] + [NK0, NK0 + 1]
    for hh in range(4):
        kTp = [P.tmp(f"kTh{t}", [128, 4, 512], BF16, n=1) for t in range(4)]
        vhp = [P.tmp(f"vh{t}", [128, 4, 4, 128], BF16, n=1) for t in range(4)]
        for t in range(4):
            P.dma(kTp[t], T(kall_v[t][hh], kv_all_t[t].bufs))
            P.dma(vhp[t], T(vall_v[t][4 + hh], kv_all_t[t].bufs))
        if hh == 0:
            P.dma(xres, D_(dr["xo"].rearrange("(c p) t -> p c t", p=128)))
            P.dma(xc, D_(dr["ctx"].rearrange("(c p) t -> p c t", p=128)))

        def ksrc(kt, hh=hh, kTp=kTp):
            if kt >= NK0:
                return kcT[:, hh, (kt - NK0) * 128:(kt - NK0 + 1) * 128]
            r, t, kk = kt // 16, (kt % 16) // 4, kt % 4
            return kTp[t][:, r, kk * 128:(kk + 1) * 128]

        def vsrc(kt, hh=hh, vhp=vhp):
            if kt >= NK0:
                return vc[:, kt - NK0, hh * 128:(hh + 1) * 128]
            r, t, kk = kt // 16, (kt % 16) // 4, kt % 4
            return vhp[t][:, r, kk, :]

        for qt in range(TOK // 512):
            attn_tile(ksrc, vsrc, qT[:, hh, qt * 512:(qt + 1) * 512], 512, KORDER,
                      mixb[:, hh, qt * 512:(qt + 1) * 512])
        attn_tile(ksrc, vsrc, qcT[:, hh, :], CTXL, [NKT - 2, NKT - 1], mixcb[:, hh, :])
    P.held.clear()
    P.ps_i = 0
    P.release(m_att)

    tiles = [(lambda c, t=t: xres[:, c, t * 512:(t + 1) * 512], 512, False) for t in range(TOK // 512)]
    tiles_c = tiles + [(lambda c: xc[:, c, :], CTXL, True)]
    for ti, (xf, W, is_ctx) in enumerate(tiles_c):
        srcs = (mixc, mixcb) if is_ctx else (mix[:, :, ti * 512:(ti + 1) * 512], mixb[:, :, ti * 512:(ti + 1) * 512])
        G = G1c if is_ctx else G1
        for dc in range(8):
            ps = P.ps()
            for kc in range(8):
                P.mm(ps[:, 0:W], w_out[:, kc, dc * 128:(dc + 1) * 128], srcs[kc // 4][:, kc % 4, :], start=(kc == 0), stop=(kc == 7))
            P.stt(xf(dc), ps[:, 0:W], G[:, dc:dc + 1], xf(dc), ALU.mult, ALU.add)
    P.release(m_base)
    moe_layer(C, dr, 0, tiles_c, A2, Sh2, G2, A2c, Sh2c, G2c)

    yg = P.sb_top([128, 8, TOK], BF16, "yg")
    uu = P.sb_top([128, 8, TOK + 6], BF16, "uu")
    ucx = T(nc.alloc_sbuf_tensor_at("ucx_alias", [128, 8, CTXL + 6], BF16, offset=xc_off).ap(), xc.bufs)
    m_l1 = P.mark()
    w1 = P.sb([128, 8, 2048], BF16, "w1in")
    for kc in range(8):
        P.dma(w1[:, kc, :], D_(dr["od_w_in"][kc * 128:(kc + 1) * 128, :]), q="pool")
    def l1_norm(ti):
        xf, W, is_ctx = tiles_c[ti]
        A, Sh = (A1nc, Sh1nc) if is_ctx else (A1n, Sh1n)
        h1 = P.tmp(f"h1_{W}", [128, 8, W], BF16, n=(1 if is_ctx else 2))
        ps = P.ps()
        for c in range(8):
            s_ = P.tmp(f"sq{W}", [128, W], BF16)
            P.act(s_, xf(c), AF.Square)
            P.mm(ps[:, 0:W], C.ones, s_, start=(c == 0), stop=(c == 7))
        rstd = C.rstd_from_ps(ps[:, 0:W], W, 1.0 / D)
        for c in range(8):
            t = P.tmp(f"nt{W}", [128, W], F32)
            P.tt(t, xf(c), rstd, ALU.mult)
            P.act(h1[:, c, :], t, AF.Identity, bias=Sh[:, c:c + 1], scale=A[:, c:c + 1])
        return h1

    def l1_proj(ti, h1):
        xf, W, is_ctx = tiles_c[ti]
        for f in range(16):
            if is_ctx and f < 8:
                continue
            ps = P.ps()
            for kc in range(8):
                P.mm(ps[:, 0:W], w1[:, kc, f * 128:(f + 1) * 128], h1[:, kc, :], start=(kc == 0), stop=(kc == 7))
            if f < 8:
                P.act(yg[:, f, ti * 512:(ti + 1) * 512], ps[:, 0:W], AF.Gelu_apprx_tanh)
            elif is_ctx:
                P.copy(ucx[:, f - 8, 3:3 + CTXL], ps[:, 0:W], eng="act")
            else:
                P.copy(uu[:, f - 8, 3 + ti * 512:3 + (ti + 1) * 512], ps[:, 0:W], eng="act")

    h1n = l1_norm(0)
    for ti in range(len(tiles_c)):
        h1c = h1n
        if ti + 1 < len(tiles_c):
            h1n = l1_norm(ti + 1)
        l1_proj(ti, h1c)
    P.memset(ucx[:, :, 0:3], 0.0)
    P.memset(ucx[:, :, CTXL + 3:CTXL + 6], 0.0)
    P.release(m_l1)

    prm = lru_params(C, dr)
    hs1 = P.sb([128, 8, 2], F32, "hs1")
    P.ts(hs1, prm["s1"], 0.5, None, ALU.mult)
    hba = P.sb([128, 8, 2], F32, "hba")
    P.ts(hba, prm["l_ba"], 0.5, None, ALU.mult)
    hbx = P.sb([128, 8, 2], F32, "hbx")
    P.ts(hbx, prm["l_bx"], 0.5, None, ALU.mult)
    qtr = P.sb([128, 1], F32, "qtr")
    P.memset(qtr, 0.25)
    hx = P.sb([128, 8, 6], F32, "hx")
    P.copy(hx[:, :, 0:3], uu[:, :, 3:6])
    P.copy(hx[:, :, 3:6], uu[:, :, TOK:TOK + 3])
    P.dma(hx_in_t, hx.re("p c k -> p (c k)"))
    allgather(hx_in_t, hx_all_t)
    hxa = P.sb([128, 4, 8, 6], F32, "hxa")
    P.dma(hxa.re("p r c k -> p r (c k)"), T(hx_all.rearrange("(r p) f -> p r f", p=128), hx_all_t.bufs))
    halo = P.sb([128, 8, 6], F32, "halo")
    P.memset(halo, 0.0)
    for j in range(4):
        P.stt(halo[:, :, 0:3], hxa[:, j, :, 3:6], hsel[:, 0, j:j + 1], halo[:, :, 0:3], ALU.mult, ALU.add)
        P.stt(halo[:, :, 3:6], hxa[:, j, :, 0:3], hsel[:, 1, j:j + 1], halo[:, :, 3:6], ALU.mult, ALU.add)
    P.copy(uu[:, :, 0:3], halo[:, :, 0:3])
    P.copy(uu[:, :, TOK + 3:TOK + 6], halo[:, :, 3:6])

    PW = HALF

    cur_dg = [None, None]

    def get_diag(c):
        if cur_dg[0] != c:
            dg = P.tmp("l_dg", [128, 2, 4, 128], BF16, n=2)
            for d_ in range(2):
                for k in range(4):
                    P.ts(dg[:, d_, k, :], C.ident, prm["cw"][:, c, d_, k:k + 1], None, ALU.mult)
            cur_dg[0], cur_dg[1] = c, dg
        return cur_dg[1]

    def stageA1a(win, W, c, d):
        dg = get_diag(c)
        o0 = 0 if d == 0 else 3
        psc = P.ps()
        for k in range(4):
            P.mm(psc[:, 0:W], dg[:, d, k, :], win[:, o0 + k:o0 + k + W], start=(k == 0), stop=(k == 3))
        uc = P.tmp("l_uc", [128, PW], F32, n=4)[:, 0:W]
        P.ts(uc, psc[:, 0:W], prm["l_cb"][:, c, d:d + 1], None, ALU.add)
        ucb = P.tmp("l_ucb", [128, PW], BF16, n=2)[:, 0:W]
        P.copy(ucb, uc, eng="dve")
        return dict(uc=uc, ucb=ucb)

    def stageA1b(st, W, c, d):
        ps = P.ps()
        P.mm(ps[:, 0:W], prm["wa"][:, d, c, :], st["ucb"])
        ps2 = P.ps()
        P.mm(ps2[:, 0:W], prm["wx"][:, d, c, :], st["ucb"])
        st["ps"], st["ps2"] = ps, ps2

    def stageA2(st, W, c, d):
        tr = P.tmp("l_tr", [128, PW], F32, n=2)[:, 0:W]
        ti = P.tmp("l_ti", [128, PW], F32, n=4)[:, 0:W]
        P.act(tr, st["ps"][:, 0:W], AF.Tanh, bias=hba[:, c, d:d + 1], scale=0.5)
        P.act(ti, st["ps2"][:, 0:W], AF.Tanh, bias=hbx[:, c, d:d + 1], scale=0.5)
        st["tr"], st["ti"] = tr, ti

    def stageEX(st, W, c, d):
        a = P.tmp("l_a", [128, PW], F32, n=3)[:, 0:W]
        P.act(a, st["tr"], AF.Exp, bias=hs1[:, c, d:d + 1], scale=hs1[:, c, d:d + 1])
        t = P.tmp("l_t", [128, PW], F32, n=3)[:, 0:W]
        P.act(t, st["tr"], AF.Exp, bias=prm["s1"][:, c, d:d + 1], scale=prm["s1"][:, c, d:d + 1])
        st["a"], st["t"] = a, t

    def stageSQ(st, W):
        P.act(st["t"], st["t"], AF.Sqrt, bias=qtr, scale=-0.25)

    def stageB2(st, W, c, d, init, out, want_A=None):
        uc, ti, a, t = st["uc"], st["ti"], st["a"], st["t"]
        P.stt(ti, ti, 1.0, uc, ALU.add, ALU.mult)
        P.tt(ti, ti, t, ALU.mult)
        if d == 0:
            P.scan(out, a, ti, init)
        else:
            P.scan(out[:, ::-1], a[:, ::-1], ti[:, ::-1], init)
        if want_A is not None:
            P.reduce(want_A, a, ALU.mult)

    def run_units(units):
        n = len(units)
        if n == 0:
            return
        sts = [None] * n

        def A1(i):
            u = units[i]
            sts[i] = stageA1a(u["win"], u["W"], u["c"], u["d"])

        def A1b(i):
            u = units[i]
            stageA1b(sts[i], u["W"], u["c"], u["d"])

        def A2(i):
            u = units[i]
            stageA2(sts[i], u["W"], u["c"], u["d"])

        def EX(i):
            u = units[i]
            stageEX(sts[i], u["W"], u["c"], u["d"])

        def B2(i):
            u = units[i]
            stageB2(sts[i], u["W"], u["c"], u["d"], u["init"](), u["out"], u.get("want_A"))
            if u.get("after"):
                u["after"]()
            sts[i] = None

        A1(0)
        if n > 1:
            A1(1)
        A1b(0)
        if n > 1:
            A1b(1)
        A2(0)
        if n > 1:
            A2(1)
        i = 0
        while i < n:
            two = i + 1 < n
            for j in (i + 2, i + 3):
                if j < n:
                    A1(j)
            for j in (i + 2, i + 3):
                if j < n:
                    A1b(j)
            EX(i)
            if two:
                EX(i + 1)
            stageSQ(sts[i], units[i]["W"])
            if two:
                stageSQ(sts[i + 1], units[i + 1]["W"])
            for j in (i + 2, i + 3):
                if j < n:
                    A2(j)
            B2(i)
            if two:
                B2(i + 1)
            i += 2

    NQ = TOK // HALF
    summ = P.sb([128, 8, 2, 3], F32, "summ")
    apq = P.sb([128, 8, 2, NQ], F32, "apq")
    carry = {}
    units = []
    for c in range(8):
        for d in range(2):
            hcx = P.tmp("l_hx", [128, PW], F32, n=2)[:, 0:CTXL]

            def after_ctx(c=c, d=d, hcx=hcx):
                P.copy(summ[:, c, d, 2:3], hcx[:, CTXL - 1:CTXL] if d == 0 else hcx[:, 0:1])
            units.append(dict(win=ucx[:, c, :], W=CTXL, c=c, d=d, init=lambda: 0.0, out=hcx, after=after_ctx))
            order = list(range(NQ)) if d == 0 else list(range(NQ - 1, -1, -1))
            for n_, k in enumerate(order):
                ho = P.tmp("l_hx", [128, PW], F32, n=2)[:, 0:HALF]
                key = (c, d)

                def init_fn(key=key, n_=n_):
                    return 0.0 if n_ == 0 else carry[key]

                def after_q(key=key, ho=ho, d=d, n_=n_, c=c):
                    if n_ == NQ - 1:
                        P.copy(summ[:, c, d, 1:2], ho[:, HALF - 1:HALF] if d == 0 else ho[:, 0:1])
                    else:
                        cr = P.tmp("l_cr", [128, 1], F32, n=4)
                        P.copy(cr, ho[:, HALF - 1:HALF] if d == 0 else ho[:, 0:1])
                        carry[key] = cr
                units.append(dict(win=uu[:, c, k * HALF:k * HALF + HALF + 6], W=HALF, c=c, d=d, init=init_fn, out=ho,
                                  after=after_q, want_A=apq[:, c, d, k:k + 1]))
    run_units(units)
    P.tt(summ[:, :, :, 0], apq[:, :, :, 0], apq[:, :, :, 1], ALU.mult)
    for k in range(2, NQ):
        P.tt(summ[:, :, :, 0], summ[:, :, :, 0], apq[:, :, :, k], ALU.mult)
    P.dma(sm_in_t, summ.re("p c d k -> p (c d k)"))
    allgather(sm_in_t, sm_all_t)
    sm = P.sb([128, 4, 8, 2, 3], F32, "sm")
    P.dma(sm.re("p r c d k -> p r (c d k)"), T(sm_all.rearrange("(r p) f -> p r f", p=128), sm_all_t.bufs))
    h0 = P.sb([128, 8, 2], F32, "h0")
    cand = P.sb([128, 8], F32, "cand")
    for d in range(2):
        P.copy(h0[:, :, d], summ[:, :, d, 2])
        order = range(4) if d == 0 else range(3, -1, -1)
        for j in order:
            P.tt(cand, sm[:, j, :, d, 0], h0[:, :, d], ALU.mult)
            P.tt(cand, cand, sm[:, j, :, d, 1], ALU.add)
            P.tt(cand, cand, h0[:, :, d], ALU.subtract)
            P.stt(h0[:, :, d], cand, jm[:, d, j:j + 1], h0[:, :, d], ALU.mult, ALU.add)
    hst = [T(None, None)] * NQ
    hfull = [P.sb([128, HALF], F32, f"hfull{k}") for k in range(NQ)]
    units = []
    carry2 = {}
    for c in range(8):
        dirs = [0, 1] if c % 2 == 0 else [1, 0]
        for di, d in enumerate(dirs):
            order = list(range(NQ)) if d == 0 else list(range(NQ - 1, -1, -1))
            for n_, k in enumerate(order):
                out = hfull[k] if di == 0 else P.tmp("l_hx", [128, PW], F32, n=2)[:, 0:HALF]
                key = (c, d)

                def init_fn(key=key, n_=n_, c=c, d=d):
                    return h0[:, c, d:d + 1] if n_ == 0 else carry2[key]

                def after_q(key=key, out=out, d=d, di=di, k=k, c=c):
                    cr = P.tmp("l_cr", [128, 1], F32, n=4)
                    P.copy(cr, out[:, HALF - 1:HALF] if d == 0 else out[:, 0:1])
                    carry2[key] = cr
                    if di == 1:
                        P.tt(out, out, hfull[k], ALU.add)
                        P.tt(yg[:, c, k * HALF:(k + 1) * HALF], out, yg[:, c, k * HALF:(k + 1) * HALF], ALU.mult)
                units.append(dict(win=uu[:, c, k * HALF:k * HALF + HALF + 6], W=HALF, c=c, d=d, init=init_fn, out=out, after=after_q))
    run_units(units)
    P.release(m_base)
    w_out1 = P.sb([128, 8, D], BF16, "w_out1")
    for kc in range(8):
        P.dma(w_out1[:, kc, :], D_(dr["od_w_out"][kc * 128:(kc + 1) * 128, :]), q="pool")
    for ti, (xf, W, _) in enumerate(tiles):
        for dc in range(8):
            ps = P.ps()
            for kc in range(8):
                P.mm(ps[:, 0:W], w_out1[:, kc, dc * 128:(dc + 1) * 128], yg[:, kc, ti * 512:(ti + 1) * 512],
                     start=(kc == 0), stop=(kc == 7))
            P.stt(xf(dc), ps[:, 0:W], G1n[:, dc:dc + 1], xf(dc), ALU.mult, ALU.add)
    P.release(m_base)
    P.top_release(top_x)
    moe_layer(C, dr, 1, tiles, A2n, Sh2n, G2n)
    P.add("sp", lambda e: e.dma_start(out=o_out.rearrange("c p t -> p c t"), in_=xres.ap), reads=[xres], writes=[outb], dma=True)
    LRU_PW[0] = 2048
    P.emit(final_reads=[outb])
    return nc, P


def fused_inputs(r, inp, cf, cb, cosT, sinT, lin):
    b, j = r // 4, r % 4
    d = l1_inputs(r, inp, cf, cb, cosT, sinT)
    del d["ada_w"], d["ada_bT"]
    d["ada_wq"] = np.ascontiguousarray(inp["ada_w"][:, :, 1536 * j:1536 * (j + 1)])
    d["ada_bq"] = np.ascontiguousarray(inp["ada_b"].reshape(2, 48, 128)[:, 12 * j:12 * (j + 1), :].transpose(2, 0, 1))
    jm = np.zeros((128, 2, 4), np.float32)
    hs = np.zeros((128, 2, 4), np.float32)
    for jj in range(4):
        jm[:, 0, jj] = 1.0 if jj < j else 0.0
        jm[:, 1, jj] = 1.0 if jj > j else 0.0
        hs[:, 0, jj] = 1.0 if jj == j - 1 else 0.0
        hs[:, 1, jj] = 1.0 if jj == j + 1 else 0.0
    d.update(dict(
        nffnT=np.stack([chunkT(inp["norm_ffn"][l]) for l in range(2)]),
        lamp=np.ascontiguousarray(np.stack([inp["ev_lam_q1"][0], inp["ev_lam_k1"][0], inp["ev_lam_q2"][0], inp["ev_lam_k2"][0]], axis=-1)),
        subn=np.ascontiguousarray(inp["ev_sub_norm"][0].reshape(128, 1)),
        w_out=inp["ev_w_out"][0], od_w_in=inp["od_w_in"][0], od_w_out=inp["od_w_out"][0], jmask=jm, hsel=hs))
    d.update(lin)
    m0, m1 = moe_inputs(inp, 0), moe_inputs(inp, 1)
    d.update(dict(moe_wr=np.concatenate([m0["moe_wr"], m1["moe_wr"]], 0), moe_rb=np.concatenate([m0["moe_rb"], m1["moe_rb"]], 0),
                  sel=m0["sel"], moe_wg=inp["moe_w_gate"], moe_wu=inp["moe_w_up"], moe_wd=inp["moe_w_down"]))
    return d


def kernel(**inp):
    inp = {k: np.asarray(v) for k, v in inp.items()}
    cf, cb = host_consts()
    cosT, sinT = rope_tables()
    cores = list(range(NCORE))
    lin = lru_inputs(inp)
    ncf, _ = _get("F", build_fused)
    ims = [fused_inputs(r, inp, cf, cb, cosT, sinT, lin) for r in cores]
    res = run_bass_kernel_spmd(ncf, ims, core_ids=cores).results
    out = np.empty((2, SEQ, D), np.float32)
    for r in cores:
        b, j = r // 4, r % 4
        out[b, j * TOK:(j + 1) * TOK, :] = res[r]["o_out"].reshape(D, TOK).T
    return out
```

```python
import math
import numpy as np
import ml_dtypes
from contextlib import ExitStack
import concourse.bass as bass
import concourse.mybir as mybir
from concourse.bass_utils import run_bass_kernel_spmd

F32 = mybir.dt.float32
BF16 = mybir.dt.bfloat16
AF = mybir.ActivationFunctionType
ALU = mybir.AluOpType
AX = mybir.AxisListType
NPBF = ml_dtypes.bfloat16

NDSEM = 8
EPS = 1e-6
NCORE = 8
TOK = 2048
CTXL = 256
SEQ = 8192
D = 1024
NKT = (SEQ + CTXL) // 128


class Buf:
    __slots__ = ("lw", "rd")

    def __init__(self):
        self.lw = None
        self.rd = []


class T:
    __slots__ = ("ap", "bufs")

    def __init__(self, ap, bufs):
        self.ap = ap
        self.bufs = bufs

    def __getitem__(self, key):
        return T(self.ap[key], self.bufs)

    def re(self, s, **kw):
        return T(self.ap.rearrange(s, **kw), self.bufs)

    def bc(self, shape):
        return T(self.ap.to_broadcast(shape), self.bufs)


def D_(ap):
    return T(ap, [])


class Prog:
    ENG = ["pe", "act", "dve", "pool", "sp"]

    def __init__(self, nc):
        self.nc = nc
        self.ins = {e: [] for e in self.ENG}
        self.ndma = {e: 0 for e in self.ENG}
        self.sb_off = 16512
        self.sb_max = 0
        self.uid = 0
        self.psum_banks = []
        self.ps_i = 0
        self.barrier_deps = {}
        self.all_dmas_since_barrier = []
        self.pools = {}
        self.held = set()
        self.top = 229312
        self.ncc = 0
        self.init_psum()

    def sb(self, shape, dtype, name=None):
        self.uid += 1
        name = f"{name or 't'}_{self.uid}"
        esz = 4 if dtype == F32 else 2
        free = 1
        for s in shape[1:]:
            free *= s
        nbytes = (free * esz + 31) // 32 * 32
        h = self.nc.alloc_sbuf_tensor_at(name, list(shape), dtype, offset=self.sb_off)
        self.sb_off += nbytes
        self.sb_max = max(self.sb_max, self.sb_off)
        assert self.sb_off <= self.top, f"SBUF overflow {self.sb_off} > {self.top} at {name}"
        return T(h.ap(), [Buf()])

    def sb_top(self, shape, dtype, name=None):
        self.uid += 1
        name = f"{name or 't'}_{self.uid}"
        esz = 4 if dtype == F32 else 2
        free = 1
        for s in shape[1:]:
            free *= s
        nbytes = (free * esz + 31) // 32 * 32
        self.top -= nbytes
        assert self.sb_off <= self.top, f"SBUF overflow (top) {self.sb_off} > {self.top} at {name}"
        h = self.nc.alloc_sbuf_tensor_at(name, list(shape), dtype, offset=self.top)
        return T(h.ap(), [Buf()])

    def top_release(self, t):
        self.barrier()
        self.top = t

    def mark(self):
        return self.sb_off

    def release(self, m):
        self.barrier()
        self.sb_off = m
        for k in [k for k, v in self.pools.items() if v[0] >= m]:
            del self.pools[k]

    def tmp(self, key, shape, dtype, n=2):
        if key not in self.pools:
            off = self.sb_off
            self.pools[key] = [off, [self.sb(shape, dtype, key) for _ in range(n)], 0]
        p = self.pools[key]
        t = p[1][p[2] % len(p[1])]
        p[2] += 1
        return t

    def init_psum(self):
        for i in range(8):
            h = self.nc.alloc_psum_tensor(f"psb{i}", [128, 512], F32)
            self.psum_banks.append(T(h.ap(), [Buf()]))

    def ps(self, hold=False):
        while (self.ps_i % 8) in self.held:
            self.ps_i += 1
        i = self.ps_i % 8
        self.ps_i += 1
        if hold:
            self.held.add(i)
        return self.psum_banks[i]

    def ps_free(self, t):
        for i, b in enumerate(self.psum_banks):
            if b.bufs is t.bufs:
                self.held.discard(i)

    def add(self, eng, fn, reads=(), writes=(), dma=False, cc=False):
        lst = self.ins[eng]
        idx = len(lst)
        if cc:
            j = self.ncc
            self.ncc += 1
            me = ("x", eng, j)
        elif dma:
            j = self.ndma[eng]
            self.ndma[eng] += 1
            me = ("d", eng, j)
        else:
            j = None
            me = ("c", eng, idx)
        deps = set()
        for t in reads:
            for b in t.bufs:
                if b.lw is not None:
                    deps.add(b.lw)
        for t in writes:
            for b in t.bufs:
                if b.lw is not None:
                    deps.add(b.lw)
                deps.update(b.rd)
        deps.discard(me)
        if eng in self.barrier_deps:
            deps |= self.barrier_deps.pop(eng)
        if eng == "pe":
            deps = {d for d in deps if not (d[0] == "c" and d[1] == "pe")}
        for t in reads:
            for b in t.bufs:
                b.rd.append(me)
        for t in writes:
            for b in t.bufs:
                b.lw = me
                b.rd = []
        lst.append(dict(fn=fn, deps=deps, dma=dma, j=j, cc=cc))
        if dma or cc:
            self.all_dmas_since_barrier.append(me)
        return me

    def barrier(self):
        deps = set()
        for e in self.ENG:
            for k in range(len(self.ins[e]) - 1, -1, -1):
                if not self.ins[e][k]["dma"] and not self.ins[e][k]["cc"]:
                    deps.add(("c", e, k))
                    break
        deps |= set(self.all_dmas_since_barrier)
        self.all_dmas_since_barrier = []
        for e in self.ENG:
            self.barrier_deps[e] = set(deps) | self.barrier_deps.get(e, set())

    def mm(self, out, lhsT, rhs, start=True, stop=True, **kw):
        return self.add("pe", lambda e: e.matmul(out.ap, lhsT.ap, rhs.ap, start=start, stop=stop, **kw),
                        reads=[lhsT, rhs], writes=[out])

    def tr(self, out, in_, ident):
        return self.add("pe", lambda e: e.transpose(out.ap, in_.ap, ident.ap), reads=[in_, ident], writes=[out])

    def act(self, out, in_, func, bias=None, scale=None, accum=None):
        reads = [in_]
        kw = {}
        if bias is not None:
            if isinstance(bias, T):
                reads.append(bias)
                kw["bias"] = bias.ap
            else:
                kw["bias"] = bias
        if scale is not None:
            if isinstance(scale, T):
                reads.append(scale)
                kw["scale"] = scale.ap
            else:
                kw["scale"] = scale
        writes = [out]
        if accum is not None:
            writes.append(accum)
            kw["accum_out"] = accum.ap
        return self.add("act", lambda e: e.activation(out.ap, in_.ap, func, **kw), reads=reads, writes=writes)

    def tt(self, out, a, b, op, eng="dve"):
        return self.add(eng, lambda e: e.tensor_tensor(out.ap, a.ap, b.ap, op), reads=[a, b], writes=[out])

    def ts(self, out, a, s1, s2, op0, op1=None, eng="dve"):
        reads = [a]
        v1 = s1.ap if isinstance(s1, T) else s1
        v2 = s2.ap if isinstance(s2, T) else s2
        if isinstance(s1, T):
            reads.append(s1)
        if isinstance(s2, T):
            reads.append(s2)
        if op1 is None:
            return self.add(eng, lambda e: e.tensor_scalar(out.ap, a.ap, v1, None, op0), reads=reads, writes=[out])
        return self.add(eng, lambda e: e.tensor_scalar(out.ap, a.ap, v1, v2, op0, op1), reads=reads, writes=[out])

    def stt(self, out, in0, scalar, in1, op0, op1):
        reads = [in0, in1]
        sv = scalar.ap if isinstance(scalar, T) else scalar
        if isinstance(scalar, T):
            reads.append(scalar)
        return self.add("dve", lambda e: e.scalar_tensor_tensor(out.ap, in0.ap, sv, in1.ap, op0, op1),
                        reads=reads, writes=[out])

    def scan(self, out, d0, d1, init, op0=ALU.mult, op1=ALU.add):
        reads = [d0, d1]
        iv = init.ap if isinstance(init, T) else init
        if isinstance(init, T):
            reads.append(init)
        return self.add("dve", lambda e: e.tensor_tensor_scan(out.ap, d0.ap, d1.ap, iv, op0, op1),
                        reads=reads, writes=[out])

    def copy(self, out, in_, eng="dve"):
        if eng == "act":
            return self.add("act", lambda e: e.copy(out.ap, in_.ap), reads=[in_], writes=[out])
        return self.add(eng, lambda e: e.tensor_copy(out.ap, in_.ap), reads=[in_], writes=[out])

    def recip(self, out, in_):
        return self.add("dve", lambda e: e.reciprocal(out.ap, in_.ap), reads=[in_], writes=[out])

    def reduce(self, out, in_, op, axis=AX.X):
        return self.add("dve", lambda e: e.tensor_reduce(out.ap, in_.ap, axis, op), reads=[in_], writes=[out])

    def memset(self, t, val, eng="dve"):
        return self.add(eng, lambda e: e.memset(t.ap, val), reads=[], writes=[t])

    def dma(self, out, in_, q="sp"):
        return self.add(q, lambda e: e.dma_start(out=out.ap, in_=in_.ap), reads=[in_], writes=[out], dma=True)

    def emit(self, final_reads=()):
        nc = self.nc
        if final_reads:
            self.add("sp", lambda e: e.nop(), reads=list(final_reads), writes=[])
        waits = {e: [] for e in self.ENG}
        marked = {e: set() for e in self.ENG}
        for e in self.ENG:
            seen_c = {}
            seen_d = set()
            for ins in self.ins[e]:
                w = []
                if ins["dma"] and ins["j"] >= NDSEM:
                    pj = ins["j"] - NDSEM
                    if ("d", e, pj) not in seen_d:
                        w.append(("d", e, pj))
                        seen_d.add(("d", e, pj))
                best = {}
                for d in ins["deps"]:
                    if d[0] == "c":
                        if d[2] > best.get(d[1], -1):
                            best[d[1]] = d[2]
                    else:
                        if (d[0], d[1], d[2]) not in seen_d:
                            seen_d.add((d[0], d[1], d[2]))
                            w.append(d)
                for f, i in best.items():
                    if i > seen_c.get(f, -1):
                        seen_c[f] = i
                        marked[f].add(i)
                        w.append(("c", f, i))
                waits[e].append(w)
        val = {e: {} for e in self.ENG}
        for e in self.ENG:
            c = 0
            for i in sorted(marked[e]):
                c += 1
                val[e][i] = c
        self.stats = {e: (len(self.ins[e]), len(marked[e])) for e in self.ENG}
        with ExitStack() as st:
            csem = {e: st.enter_context(nc.semaphore(f"cs_{e}")) for e in self.ENG}
            xsem = [st.enter_context(nc.semaphore(f"xs_{i}")) for i in range(self.ncc)]
            dsem = {e: [st.enter_context(nc.semaphore(f"ds_{e}{i}")) for i in range(NDSEM)]
                    for e in self.ENG if self.ndma[e] > 0}
            block = st.enter_context(nc.Block())

            def run(e, eng):
                for k, ins in enumerate(self.ins[e]):
                    for d in waits[e][k]:
                        if d[0] == "c":
                            eng.wait_ge(csem[d[1]], val[d[1]][d[2]])
                        elif d[0] == "x":
                            eng.wait_ge(xsem[d[2]], 1)
                        else:
                            eng.wait_ge(dsem[d[1]][d[2] % NDSEM], 16 * (d[2] // NDSEM + 1))
                    r = ins["fn"](eng)
                    if ins["cc"]:
                        r.then_inc(xsem[ins["j"]])
                    elif ins["dma"]:
                        r.then_inc(dsem[e][ins["j"] % NDSEM], 16)
                    elif k in marked[e]:
                        r.then_inc(csem[e], 1)

            @block.tensor
            def _(eng):
                run("pe", eng)

            @block.scalar
            def _(eng):
                run("act", eng)

            @block.vector
            def _(eng):
                run("dve", eng)

            @block.gpsimd
            def _(eng):
                run("pool", eng)

            @block.sync
            def _(eng):
                run("sp", eng)


class Ctx:
    def __init__(self, nc, P, dram):
        self.nc, self.P, self.dram = nc, P, dram
        cf = P.sb([128, 384], F32, "cf")
        P.dma(cf, D_(dram["cf"]))
        self.ident = cf[:, 0:128]
        self.perm = cf[:, 128:256]
        self.onesf = cf[:, 256:384]
        cb = P.sb([128, 256], BF16, "cb")
        P.dma(cb, D_(dram["cb"]), q="pool")
        self.ones = cb[:, 0:128]
        self.bones = cb[:, 128:256]
        self.epsb = P.sb([128, 1], F32, "eps")
        P.memset(self.epsb, EPS)

    def rstd_from_ps(self, ps_ss, W, inv_n, name="rstd", n=2):
        P = self.P
        r = P.tmp(f"{name}{W}", [128, W], F32, n=n)
        P.act(r, ps_ss, AF.Ln, bias=self.epsb, scale=inv_n)
        P.act(r, r, AF.Exp, scale=-0.5)
        return r


def norm_mod(C, xt, W, A, Bsh, out_h, out_f32=None):
    P = C.P
    ps = P.ps()
    for c in range(8):
        s = P.tmp(f"sq{W}", [128, W], BF16)
        P.act(s, xt[:, c, :], AF.Square)
        P.mm(ps[:, 0:W], C.ones, s, start=(c == 0), stop=(c == 7))
    rstd = C.rstd_from_ps(ps[:, 0:W], W, 1.0 / D)
    for c in range(8):
        t = P.tmp(f"nt{W}", [128, W], F32)
        P.tt(t, xt[:, c, :], rstd, ALU.mult)
        if out_f32 is not None:
            P.act(out_f32[:, c, :], t, AF.Identity, bias=Bsh[:, c:c + 1], scale=A[:, c:c + 1])
            P.copy(out_h[:, c, :], out_f32[:, c, :], eng="pool")
        else:
            P.act(out_h[:, c, :], t, AF.Identity, bias=Bsh[:, c:c + 1], scale=A[:, c:c + 1])
    return


def compute_mods(C, dram, l_list, cvec, out_mods):
    P = C.P
    m = P.mark()
    sc = P.sb([128, 8, 2], F32, "silu_c")
    P.act(sc, cvec, AF.Silu)
    wbuf = [P.sb([128, 8, 1024], F32, "adaw") for _ in range(2)]
    k = 0
    for li, l in enumerate(l_list):
        ps = P.ps(hold=True)
        bT = P.sb([128, 48], F32, "adab")
        P.dma(bT, D_(dram["ada_bT"][l]))
        for j in range(6):
            w = wbuf[k % 2]
            k += 1
            P.dma(w, D_(dram["ada_w"][l].rearrange("(kc p) f -> p kc f", p=128)[:, :, j * 1024:(j + 1) * 1024]))
            for fc in range(8):
                g = j * 8 + fc
                for kc in range(8):
                    P.mm(ps[:, 2 * g:2 * g + 2], w[:, kc, fc * 128:(fc + 1) * 128], sc[:, kc, :],
                         start=(kc == 0), stop=(kc == 7))
        P.tt(out_mods[li], ps[:, 0:96].re("p (g c) -> p g c", c=2),
             T(bT.ap.unsqueeze(2).to_broadcast([128, 48, 2]), bT.bufs), ALU.add)
        P.ps_free(ps)
    P.release(m)


def din(nc, name, shape, dt=F32):
    return nc.dram_tensor(name, list(shape), dt, kind="ExternalInput").ap()


def dout(nc, name, shape, dt=F32):
    return nc.dram_tensor(name, list(shape), dt, kind="ExternalOutput").ap()


def mod_scalars(C, mods_l, gainT, which, col, name):
    P = C.P
    base = 24 * which
    A = P.sb([128, 8], F32, name)
    P.ts(A, mods_l[:, base + 8:base + 16, col], 1.0, None, ALU.add)
    P.tt(A, A, gainT, ALU.mult)
    Sh = P.sb([128, 8], F32, name + "s")
    P.copy(Sh, mods_l[:, base:base + 8, col])
    G = P.sb([128, 8], F32, name + "g")
    P.copy(G, mods_l[:, base + 16:base + 24, col])
    return A, Sh, G


def qk_norm_rope(C, ps_in, W, gain, cos, sin, out):
    P = C.P
    k_sb = P.tmp(f"qk_k{W}", [128, W], F32, n=2)
    P.copy(k_sb, ps_in, eng="act")
    sq = P.tmp(f"qk_sq{W}", [128, W], BF16)
    P.act(sq, ps_in, AF.Square)
    ps2 = P.ps()
    P.mm(ps2[:, 0:W], C.bones, sq)
    rs = C.rstd_from_ps(ps2[:, 0:W], W, 1.0 / 64.0, name="qk_rs", n=2)
    if cos is None:
        P.stt(out, k_sb, gain, rs, ALU.mult, ALU.mult)
        return
    kh = P.tmp(f"qk_kh{W}", [128, W], F32, n=2)
    P.stt(kh, k_sb, gain, rs, ALU.mult, ALU.mult)
    ps3 = P.ps()
    P.mm(ps3[:, 0:W], C.perm, kh)
    t1 = P.tmp(f"qk_t1{W}", [128, W], F32, n=2)
    P.tt(t1, kh, cos, ALU.mult)
    t2 = P.tmp(f"qk_t2{W}", [128, W], F32, n=2)
    P.tt(t2, ps3[:, 0:W], sin, ALU.mult)
    P.tt(out, t1, t2, ALU.add)


def build_L1():
    nc = bass.Bass("TRN2", target_bir_lowering=False)
    dr = {}
    dr["cf"] = din(nc, "cf", [128, 384])
    dr["cb"] = din(nc, "cb", [128, 256])
    dr["xo"] = din(nc, "xo", [D, TOK])
    dr["xh"] = din(nc, "xh", [D, 2])
    dr["hmask"] = din(nc, "hmask", [128, 2])
    dr["ctx"] = din(nc, "ctx", [D, CTXL])
    dr["cvec"] = din(nc, "cvec", [128, 8, 2])
    dr["ada_w"] = din(nc, "ada_w", [2, D, 6 * D])
    dr["ada_bT"] = din(nc, "ada_bT", [2, 128, 48])
    dr["nmixT"] = din(nc, "nmixT", [2, 128, 8])
    dr["w_in"] = din(nc, "w_in", [D, 3072])
    dr["convw"] = din(nc, "convw", [128, 4, 3])
    dr["convb"] = din(nc, "convb", [128, 4])
    dr["qkn"] = din(nc, "qkn", [128, 2])
    dr["cos"] = din(nc, "cos", [128, TOK])
    dr["sin"] = din(nc, "sin", [128, TOK])
    o_kT = dout(nc, "o_kT", [4, 128, TOK], BF16)
    o_v = dout(nc, "o_v", [TOK, 512], BF16)
    o_kcT = dout(nc, "o_kcT", [4, 128, CTXL], BF16)
    o_vc = dout(nc, "o_vc", [CTXL, 512], BF16)
    o_qT = dout(nc, "o_qT", [4, 128, TOK], BF16)
    o_qcT = dout(nc, "o_qcT", [4, 128, CTXL], BF16)
    o_oa = dout(nc, "o_oa", [4, 128, TOK], BF16)
    o_oac = dout(nc, "o_oac", [4, 128, CTXL], BF16)
    o_mods = dout(nc, "o_mods", [2, 128, 48, 2])

    P = Prog(nc)
    C = Ctx(nc, P, dr)
    outb = T(None, [Buf()])

    cvec = P.sb([128, 8, 2], F32, "cvec")
    P.dma(cvec, D_(dr["cvec"]))
    mods = [P.sb([128, 48, 2], F32, f"mods{l}") for l in range(2)]
    compute_mods(C, dr, [0, 1], cvec, mods)
    for l in range(2):
        P.add("sp", lambda e, l=l: e.dma_start(out=o_mods[l], in_=mods[l].ap), reads=[mods[l]], writes=[outb], dma=True)
    nmix = P.sb([128, 8], F32, "nmix")
    P.dma(nmix, D_(dr["nmixT"][0]))
    A_lat, Sh_lat, _ = mod_scalars(C, mods[0], nmix, 0, 0, "Alat")
    A_ctx, Sh_ctx, _ = mod_scalars(C, mods[0], nmix, 0, 1, "Actx")

    w_in = P.sb([128, 8, 3072], BF16, "w_in")
    for kc in range(8):
        P.dma(w_in[:, kc, :], D_(dr["w_in"][kc * 128:(kc + 1) * 128, :]), q="pool")
    convw = P.sb([128, 4, 3], F32, "convw")
    P.dma(convw, D_(dr["convw"]))
    convb = P.sb([128, 4], F32, "convb")
    P.dma(convb, D_(dr["convb"]))
    qkn = P.sb([128, 2], F32, "qkn")
    P.dma(qkn, D_(dr["qkn"]))
    hmask = P.sb([128, 2], F32, "hmask")
    P.dma(hmask, D_(dr["hmask"]))

    qT = P.sb([128, 4, TOK], BF16, "qT")
    kT = P.sb([128, 4, TOK], BF16, "kT")
    cbs = P.sb([128, 4, TOK], BF16, "cbs")
    ub = P.sb([128, 4, TOK + 2], BF16, "ub")
    qcT = P.sb([128, 4, CTXL], BF16, "qcT")
    kcT = P.sb([128, 4, CTXL], BF16, "kcT")
    cbc = P.sb([128, 4, CTXL], BF16, "cbc")
    ubc = P.sb([128, 4, CTXL + 2], BF16, "ubc")
    P.memset(ubc[:, :, 0:1], 0.0)
    P.memset(ubc[:, :, CTXL + 1:CTXL + 2], 0.0)

    def proj(h, W, f):
        ps = P.ps()
        for kc in range(8):
            P.mm(ps[:, 0:W], w_in[:, kc, f * 128:(f + 1) * 128], h[:, kc, 0:W], start=(kc == 0), stop=(kc == 7))
        return ps[:, 0:W]

    def front_tile(xsrc, W, A, Sh, rope, dst):
        xt = P.tmp(f"xt{W}", [128, 8, W], F32, n=1)
        P.dma(xt, D_(xsrc.rearrange("(c p) t -> p c t", p=128)))
        if rope:
            cs_c = P.tmp("cos_t", [128, W], F32)
            cs_s = P.tmp("sin_t", [128, W], F32)
            P.dma(cs_c, D_(rope[0][:, dst["t0"]:dst["t0"] + W]))
            P.dma(cs_s, D_(rope[1][:, dst["t0"]:dst["t0"] + W]))
        h = P.tmp(f"h{W}", [128, 8, W], BF16)
        norm_mod(C, xt, W, A, Sh, h)
        t0 = dst["t0"]
        if dst.get("cb") is not None:
            for c in range(4):
                ps = proj(h, W, c)
                P.copy(dst["cb"][:, c, t0:t0 + W], ps, eng="act")
        for c in range(4):
            ps_cc = proj(h, W, 4 + c)
            cc = P.tmp(f"cc{W}", [128, W], F32)
            P.copy(cc, ps_cc, eng="act")
            ps_cx = proj(h, W, 8 + c)
            P.tt(dst["u"](c), cc, ps_cx, ALU.mult)
        if dst.get("q") is None:
            return
        for hh in range(4):
            ps = proj(h, W, 12 + hh)
            cs = (cs_c, cs_s) if rope else (None, None)
            qk_norm_rope(C, ps, W, qkn[:, 0:1], cs[0], cs[1], dst["q"][:, hh, t0:t0 + W])
        for hh in range(4):
            ps = proj(h, W, 16 + hh)
            cs = (cs_c, cs_s) if rope else (None, None)
            qk_norm_rope(C, ps, W, qkn[:, 1:2], cs[0], cs[1], dst["k"][:, hh, t0:t0 + W])
        for sub in range(W // 128):
            ps = P.ps()
            for kc in range(8):
                P.mm(ps, h[:, kc, sub * 128:(sub + 1) * 128], w_in[:, kc, 2560:3072], start=(kc == 0), stop=(kc == 7))
            vs = P.tmp("vs", [128, 512], BF16, n=3)
            P.copy(vs, ps, eng="act")
            r0 = t0 + sub * 128
            P.add("sp", lambda e, vs=vs, r0=r0, dv=dst["v_out"]: e.dma_start(out=dv[r0:r0 + 128, :], in_=vs.ap),
                  reads=[vs], writes=[outb], dma=True)

    cos, sin = dr["cos"], dr["sin"]

    m_own = P.mark()
    for t in range(TOK // 512):
        front_tile(dr["xo"][:, t * 512:(t + 1) * 512], 512, A_lat, Sh_lat, (cos, sin),
                   dict(cb=cbs, u=lambda c, t=t: ub[:, c, 1 + t * 512:1 + (t + 1) * 512], q=qT, k=kT, v_out=o_v, t0=t * 512))
    P.release(m_own)
    uh = P.sb([128, 4, 2], F32, "uh")
    front_tile(dr["xh"], 2, A_lat, Sh_lat, None, dict(u=lambda c: uh[:, c, :], t0=0))
    for c in range(4):
        P.tt(ub[:, c, 0:1], uh[:, c, 0:1], hmask[:, 0:1], ALU.mult)
        P.tt(ub[:, c, TOK + 1:TOK + 2], uh[:, c, 1:2], hmask[:, 1:2], ALU.mult)
    front_tile(dr["ctx"], CTXL, A_ctx, Sh_ctx, None,
               dict(cb=cbc, u=lambda c: ubc[:, c, 1:1 + CTXL], q=qcT, k=kcT, v_out=o_vc, t0=0))

    def conv(ubuf, cbt, W, o_dst):
        for c in range(4):
            acc = P.tmp(f"cacc{W}", [128, W], F32)
            P.ts(acc, ubuf[:, c, 1:1 + W], convw[:, c, 1:2], convb[:, c:c + 1], ALU.mult, ALU.add)
            P.stt(acc, ubuf[:, c, 0:W], convw[:, c, 0:1], acc, ALU.mult, ALU.add)
            P.stt(acc, ubuf[:, c, 2:2 + W], convw[:, c, 2:3], acc, ALU.mult, ALU.add)
            oa = P.tmp(f"oa{W}", [128, W], BF16)
            P.tt(oa, acc, cbt[:, c, :], ALU.mult)
            P.add("sp", lambda e, oa=oa, c=c: e.dma_start(out=o_dst[c], in_=oa.ap), reads=[oa], writes=[outb], dma=True)

    conv(ub, cbs, TOK, o_oa)
    conv(ubc, cbc, CTXL, o_oac)
    for (src, dst) in ((qT, o_qT), (kT, o_kT), (qcT, o_qcT), (kcT, o_kcT)):
        P.add("sp", lambda e, src=src, dst=dst: e.dma_start(out=dst.rearrange("h p t -> p h t"), in_=src.ap),
              reads=[src], writes=[outb], dma=True)
    P.emit(final_reads=[outb])
    return nc, P


def host_consts():
    cf = np.zeros((128, 384), np.float32)
    cf[:, 256:384] = 1.0
    cf[np.arange(128), np.arange(128)] = 1.0
    for m in range(128):
        partner = m + 32 if (m % 64) < 32 else m - 32
        cf[partner, 128 + m] = 1.0
    cb = np.zeros((128, 256), np.float32)
    cb[:, 0:128] = 1.0
    for k in range(128):
        for m in range(128):
            if k // 64 == m // 64:
                cb[k, 128 + m] = 1.0
    return cf, cb


def rope_tables():
    n_rows = SEQ // 64
    rows = np.repeat(np.arange(n_rows), 64).astype(np.float32)
    cols = np.tile(np.arange(64), n_rows).astype(np.float32)
    n_freq = 16
    inv = (np.float32(10000.0) ** (-np.arange(n_freq, dtype=np.float32) / np.float32(n_freq))).astype(np.float32)
    ang = np.concatenate([rows[:, None] * inv, cols[:, None] * inv], axis=-1).astype(np.float32)
    cos, sin = np.cos(ang).astype(np.float32), np.sin(ang).astype(np.float32)
    idx = np.arange(128) % 32
    cosT = np.ascontiguousarray(cos[:, idx].T)
    sgn = np.where((np.arange(128) % 64) < 32, -1.0, 1.0).astype(np.float32)
    sinT = np.ascontiguousarray(sin[:, idx].T * sgn[:, None])
    return cosT, sinT


def chunkT(v):
    return np.ascontiguousarray(v.reshape(-1, 128).T)


def moe_layer(C, dr, l, tiles, A2, Sh2, G2, A2c=None, Sh2c=None, G2c=None):
    P = C.P
    m0 = P.mark()
    NT = sum(W // 128 for (_, W, _) in tiles)
    TT = sum(W for (_, W, _) in tiles)
    hf = P.sb([128, 8, TT], BF16, "hf")
    g_hi = P.sb([16, TT], BF16, "g_hi")
    g_lo = P.sb([16, TT], BF16, "g_lo")
    wr = P.sb([128, 8, 20], F32, "wr")
    P.dma(wr, D_(dr["moe_wr"][l].rearrange("(kc p) n -> p kc n", p=128)))
    rb = P.sb([128, 20], F32, "rb")
    P.dma(rb, D_(dr["moe_rb"][l]))
    sel = P.sb([16, 16, 128], BF16, "sel")
    P.dma(sel, D_(dr["sel"]), q="pool")
    def load_w(e):
        wg = P.tmp("wg", [128, 8, 512], BF16)
        wu = P.tmp("wu", [128, 8, 512], BF16)
        wd = P.tmp("wd", [128, 4, 1024], BF16)
        P.dma(wg, D_(dr["moe_wg"][l, e].rearrange("(kc p) f -> p kc f", p=128)), q="pool")
        P.dma(wu, D_(dr["moe_wu"][l, e].rearrange("(kc p) f -> p kc f", p=128)), q="pool")
        P.dma(wd, D_(dr["moe_wd"][l, e].rearrange("(fc p) d -> p fc d", p=128)), q="pool")
        return wg, wu, wd

    wts = {0: load_w(0)}
    m1 = P.mark()
    ps_l = P.ps(hold=True)
    off = 0
    nt = 0
    wg1, wu1 = P.pools["wg"][1][1], P.pools["wu"][1][1]
    al_a = P.nc.alloc_sbuf_tensor_at(f"hf32a_{l}_{P.uid}", [128, 4, 512], F32, offset=P.pools["wg"][0] + 8192).ap()
    al_b = P.nc.alloc_sbuf_tensor_at(f"hf32b_{l}_{P.uid}", [128, 4, 512], F32, offset=P.pools["wu"][0] + 8192).ap()
    hf32_main = P.tmp("hf32", [128, 8, 512], F32, n=1)
    stage = [[], []]
    for c in range(8):
        stage[0].append(T(hf32_main.ap[:, c, :], [Buf()]))
        b_ = Buf()
        (wg1 if c < 4 else wu1).bufs.append(b_)
        stage[1].append(T((al_a if c < 4 else al_b)[:, c % 4, :], [b_]))
    for ti_, (xf, W, is_ctx) in enumerate(tiles):
        A, Sh = (A2c, Sh2c) if is_ctx else (A2, Sh2)

        def hf32c(c, ti_=ti_, W=W):
            return stage[ti_ % 2][c][:, 0:W]
        ps = P.ps()
        for c in range(8):
            s_ = P.tmp("msq", [128, 512], BF16, n=1)[:, 0:W]
            P.act(s_, xf(c), AF.Square)
            P.mm(ps[:, 0:W], C.ones, s_, start=(c == 0), stop=(c == 7))
        rstd = P.tmp("mrstd", [128, 512], F32, n=1)[:, 0:W]
        P.act(rstd, ps[:, 0:W], AF.Ln, bias=C.epsb, scale=1.0 / D)
        P.act(rstd, rstd, AF.Exp, scale=-0.5)
        ps_t = P.ps()
        for c in range(8):
            t = P.tmp("mnt", [128, 512], F32)[:, 0:W]
            P.tt(t, xf(c), rstd, ALU.mult)
            P.act(hf32c(c), t, AF.Identity, bias=Sh[:, c:c + 1], scale=A[:, c:c + 1])
            P.copy(hf[:, c, off:off + W], hf32c(c), eng="dve")
            P.mm(ps_t[0:20, 0:W], wr[:, c, :], hf32c(c), start=(c == 0), stop=(c == 7))
        lT = P.tmp("mlT", [20, 512], F32, n=1)[:, 0:W]
        P.copy(lT, ps_t[0:20, 0:W], eng="act")
        for sub in range(W // 128):
            P.tr(ps_l[:, nt * 20:nt * 20 + 20], lT[:, sub * 128:(sub + 1) * 128], C.ident[0:20, 0:20])
            nt += 1
        off += W
    Lg = P.sb([128, NT, 20], F32, "Lg")
    P.tt(Lg, ps_l[:, 0:NT * 20].re("p (t k) -> p t k", k=20), T(rb.ap.unsqueeze(1).to_broadcast([128, NT, 20]), rb.bufs), ALU.add)
    gl = Lg[:, :, 0:4]
    el = Lg[:, :, 4:20].re("p t (g e) -> p t g e", e=4)

    def bc3(t, n):
        return T(t.ap.unsqueeze(2).to_broadcast([128, NT, n]), t.bufs)

    gmax = P.sb([128, NT], F32, "gmax")
    P.reduce(gmax, gl, ALU.max)
    oh = P.sb([128, NT, 4], F32, "oh")
    P.tt(oh, gl, bc3(gmax, 4), ALU.is_equal)
    gsh = P.sb([128, NT, 4], F32, "gsh")
    P.tt(gsh, gl, bc3(gmax, 4), ALU.subtract)
    P.act(gsh, gsh, AF.Exp)
    psel = P.sb([128, NT], F32, "psel")
    P.reduce(psel, gsh, ALU.add)
    P.recip(psel, psel)
    e4 = P.sb([128, NT, 4, 4], F32, "e4")
    P.tt(e4, el, T(oh.ap.unsqueeze(3).to_broadcast([128, NT, 4, 4]), oh.bufs), ALU.mult)
    esel = P.sb([128, NT, 4], F32, "esel")
    P.reduce(esel, e4.re("p t g e -> p t e g"), ALU.add)
    mx1 = P.sb([128, NT], F32, "mx1")
    P.reduce(mx1, esel, ALU.max)
    mk1 = P.sb([128, NT, 4], F32, "mk1")
    P.tt(mk1, esel, bc3(mx1, 4), ALU.is_equal)
    es2 = P.sb([128, NT, 4], F32, "es2")
    P.stt(es2, mk1, -1e30, esel, ALU.mult, ALU.add)
    mx2 = P.sb([128, NT], F32, "mx2")
    P.reduce(mx2, es2, ALU.max)
    mk2 = P.sb([128, NT, 4], F32, "mk2")
    P.tt(mk2, es2, bc3(mx2, 4), ALU.is_equal)
    w1 = P.sb([128, NT], F32, "w1")
    P.tt(w1, mx1, mx2, ALU.subtract)
    P.act(w1, w1, AF.Sigmoid)
    P.tt(w1, w1, psel, ALU.mult)
    w2 = P.sb([128, NT], F32, "w2")
    P.tt(w2, psel, w1, ALU.subtract)
    P.tt(mk1, mk1, bc3(w1, 4), ALU.mult)
    P.tt(mk2, mk2, bc3(w2, 4), ALU.mult)
    P.tt(mk1, mk1, mk2, ALU.add)
    gate = e4
    P.tt(gate, T(oh.ap.unsqueeze(3).to_broadcast([128, NT, 4, 4]), oh.bufs),
         T(mk1.ap.unsqueeze(2).to_broadcast([128, NT, 4, 4]), mk1.bufs), ALU.mult)
    gflat = gate.re("p t g e -> p t (g e)")
    for t4 in range(0, NT, 4):
        ps = P.ps()
        n = min(4, NT - t4)
        for i in range(n):
            P.tr(ps[0:16, i * 128:(i + 1) * 128], gflat[:, t4 + i, :], C.ident)
        P.copy(g_hi[:, t4 * 128:(t4 + n) * 128], ps[0:16, 0:n * 128])
        P.tt(g_lo[:, t4 * 128:(t4 + n) * 128], ps[0:16, 0:n * 128], g_hi[:, t4 * 128:(t4 + n) * 128], ALU.subtract)
    P.ps_free(ps_l)
    P.release(m1)

    steps = []
    for e in range(16):
        off = 0
        for (xf, W, is_ctx) in tiles:
            steps.append((e, xf, W, is_ctx, off))
            off += W

    pend = None

    def down(st):
        (e, xf, W, is_ctx, off, actb, wd) = st
        G = G2c if is_ctx else G2
        for dc in range(8):
            ps = P.ps()
            for fc in range(4):
                P.mm(ps[:, 0:W], wd[:, fc, dc * 128:(dc + 1) * 128], actb[:, fc, :], start=(fc == 0), stop=(fc == 3))
            P.stt(xf(dc), ps[:, 0:W], G[:, dc:dc + 1], xf(dc), ALU.mult, ALU.add)

    for si, (e, xf, W, is_ctx, off) in enumerate(steps):
        wg, wu, wd = wts[e]
        psb = P.ps()
        P.mm(psb[:, 0:W], sel[:, e, :], g_hi[:, off:off + W], start=True, stop=False)
        P.mm(psb[:, 0:W], sel[:, e, :], g_lo[:, off:off + W], start=False, stop=True)
        gbc = P.tmp(f"gbc{W}", [128, W], F32)
        P.copy(gbc, psb[:, 0:W], eng="act")
        actb = P.tmp(f"actb{W}", [128, 4, W], BF16)
        for fc in range(4):
            ps_g = P.ps()
            for kc in range(8):
                P.mm(ps_g[:, 0:W], wg[:, kc, fc * 128:(fc + 1) * 128], hf[:, kc, off:off + W], start=(kc == 0), stop=(kc == 7))
            ps_u = P.ps()
            for kc in range(8):
                P.mm(ps_u[:, 0:W], wu[:, kc, fc * 128:(fc + 1) * 128], hf[:, kc, off:off + W], start=(kc == 0), stop=(kc == 7))
            sg = P.tmp(f"sg{W}", [128, W], F32)
            P.act(sg, ps_g[:, 0:W], AF.Silu)
            t2 = P.tmp(f"t2{W}", [128, W], F32)
            P.tt(t2, sg, ps_u[:, 0:W], ALU.mult)
            P.tt(actb[:, fc, :], t2, gbc, ALU.mult)
        if pend is not None:
            down(pend)
        pend = (e, xf, W, is_ctx, off, actb, wd)
        if off == 0 and e + 1 < 16:
            wts[e + 1] = load_w(e + 1)
    down(pend)
    P.release(m0)


def moe_drams(nc, dr):
    dr["moe_wr"] = din(nc, "moe_wr", [1, D, 20])
    dr["moe_rb"] = din(nc, "moe_rb", [1, 128, 20])
    dr["sel"] = din(nc, "sel", [16, 16, 128])
    dr["moe_wg"] = din(nc, "moe_wg", [1, 16, D, 512])
    dr["moe_wu"] = din(nc, "moe_wu", [1, 16, D, 512])
    dr["moe_wd"] = din(nc, "moe_wd", [1, 16, 512, D])


def build_L2(debug=False):
    nc = bass.Bass("TRN2", target_bir_lowering=False)
    dr = {}
    dr["cf"] = din(nc, "cf", [128, 384])
    dr["cb"] = din(nc, "cb", [128, 256])
    dr["kT"] = din(nc, "kT", [4, 128, NKT * 128], BF16)
    dr["v"] = din(nc, "v", [4, 128, NKT, 128], BF16)
    dr["qT"] = din(nc, "qT", [4, 128, TOK], BF16)
    dr["qcT"] = din(nc, "qcT", [4, 128, CTXL], BF16)
    dr["oa"] = din(nc, "oa", [4, 128, TOK], BF16)
    dr["oac"] = din(nc, "oac", [4, 128, CTXL], BF16)
    dr["xo"] = din(nc, "xo", [D, TOK])
    dr["ctx"] = din(nc, "ctx", [D, CTXL])
    dr["mods"] = din(nc, "mods", [2, 128, 48, 2])
    dr["lamp"] = din(nc, "lamp", [64, 4])
    dr["subn"] = din(nc, "subn", [128, 1])
    dr["w_out"] = din(nc, "w_out", [D, D])
    dr["nffnT"] = din(nc, "nffnT", [128, 8])
    dr["nmix1T"] = din(nc, "nmix1T", [128, 8])
    dr["od_w_in"] = din(nc, "od_w_in", [D, 2048])
    moe_drams(nc, dr)
    o_x2 = dout(nc, "o_x2", [8, 128, TOK])
    o_yg = dout(nc, "o_yg", [8, 128, TOK], BF16)
    o_u = dout(nc, "o_u", [8, 128, TOK])
    o_uc = dout(nc, "o_uc", [8, 128, CTXL])
    if debug:
        o_x1 = dout(nc, "o_x1", [8, 128, TOK])
        o_c2 = dout(nc, "o_c2", [8, 128, CTXL])

    P = Prog(nc)
    C = Ctx(nc, P, dr)
    outb = T(None, [Buf()])
    LAM_INIT = 0.8 - 0.6 * math.exp(-0.3 * 0)

    mods = [P.sb([128, 48, 2], F32, f"mods{l}") for l in range(2)]
    for l in range(2):
        P.dma(mods[l], D_(dr["mods"][l]))
    nffn = P.sb([128, 8], F32, "nffn")
    P.dma(nffn, D_(dr["nffnT"]))
    nmix1 = P.sb([128, 8], F32, "nmix1")
    P.dma(nmix1, D_(dr["nmix1T"]))
    _, _, G1 = mod_scalars(C, mods[0], nffn, 0, 0, "m0l")
    _, _, G1c = mod_scalars(C, mods[0], nffn, 0, 1, "m0c")
    A2, Sh2, G2 = mod_scalars(C, mods[0], nffn, 1, 0, "f0l")
    A2c, Sh2c, G2c = mod_scalars(C, mods[0], nffn, 1, 1, "f0c")
    A1n, Sh1n, _ = mod_scalars(C, mods[1], nmix1, 0, 0, "m1l")
    A1nc, Sh1nc, _ = mod_scalars(C, mods[1], nmix1, 0, 1, "m1c")

    lamp = P.sb([64, 4], F32, "lamp")
    P.dma(lamp, D_(dr["lamp"]))
    lpr = P.sb([64, 2], F32, "lpr")
    P.tt(lpr[:, 0:1], lamp[:, 0:1], lamp[:, 1:2], ALU.mult)
    P.tt(lpr[:, 1:2], lamp[:, 2:3], lamp[:, 3:4], ALU.mult)
    psl = P.ps()
    P.mm(psl[:, 0:2], C.onesf[0:64, :], lpr)
    lex = P.sb([128, 2], F32, "lex")
    P.act(lex, psl[:, 0:2], AF.Exp)
    nlam = P.sb([128, 1], F32, "nlam")
    P.tt(nlam, lex[:, 1:2], lex[:, 0:1], ALU.subtract)
    P.ts(nlam, nlam, -LAM_INIT, None, ALU.add)
    subn = P.sb([128, 1], F32, "subn")
    P.dma(subn, D_(dr["subn"]))
    P.ts(subn, subn, 1.0 - LAM_INIT, None, ALU.mult)

    xres = P.sb([128, 8, TOK], F32, "xres")
    xc = P.sb([128, 8, CTXL], F32, "xc")
    P.dma(xres, D_(dr["xo"].rearrange("(c p) t -> p c t", p=128)))
    P.dma(xc, D_(dr["ctx"].rearrange("(c p) t -> p c t", p=128)))
    m_mix = P.mark()
    w_out = P.sb([128, 8, D], BF16, "w_out")
    for kc in range(8):
        P.dma(w_out[:, kc, :], D_(dr["w_out"][kc * 128:(kc + 1) * 128, :]), q="pool")
    mix = P.sb([128, 8, TOK], BF16, "mix")
    mixc = P.sb([128, 8, CTXL], BF16, "mixc")
    P.dma(mix[:, 0:4, :], D_(dr["oa"].rearrange("c p t -> p c t")))
    P.dma(mixc[:, 0:4, :], D_(dr["oac"].rearrange("c p t -> p c t")))
    m_att = P.mark()
    qT = P.sb([128, 4, TOK], BF16, "qT")
    P.dma(qT, D_(dr["qT"].rearrange("h p t -> p h t")))
    qcT = P.sb([128, 4, CTXL], BF16, "qcT")
    P.dma(qcT, D_(dr["qcT"].rearrange("h p t -> p h t")))

    SB = [P.psum_banks[i] for i in range(4)]
    ACC = [(P.psum_banks[4], P.psum_banks[5]), (P.psum_banks[6], P.psum_banks[7])]
    P.held.update([0, 1, 2, 3, 4, 5, 6, 7])
    sctr = [0]

    def attn_group(kTh, vh, qsrc, W, kts, m):
        accO, accL = ACC[m]
        lo, hi = m * 64, (m + 1) * 64
        n = len(kts)

        def S(i):
            sb_ = SB[sctr[0] % 4]
            sctr[0] += 1
            kt = kts[i]
            P.mm(sb_[:, 0:W], kTh[lo:hi, kt * 128:(kt + 1) * 128], qsrc[lo:hi, :])
            return sb_

        cur = S(0)
        for i in range(n):
            nxt = S(i + 1) if i + 1 < n else None
            pt = P.tmp("pt", [128, 512], BF16, n=3)[:, 0:W]
            P.act(pt, cur[:, 0:W], AF.Exp, scale=0.125)
            kt = kts[i]
            P.mm(accO[:, 0:W], vh[:, kt, :], pt, start=(i == 0), stop=(i == n - 1))
            P.mm(accL[:, 0:W], C.ones, pt, start=(i == 0), stop=(i == n - 1))
            cur = nxt
        return accO, accL

    def attn_tile(kTh, vh, qsrc_fn, W, kts, dst):
        om = []
        for m in range(2):
            accO, accL = attn_group(kTh, vh, qsrc_fn, W, kts, m)
            rl = P.tmp("rl", [128, 512], F32)[:, 0:W]
            P.recip(rl, accL[:, 0:W])
            o = P.tmp("om", [128, 512], F32, n=4)[:, 0:W]
            P.tt(o, accO[:, 0:W], rl, ALU.mult)
            om.append(o)
        o = P.tmp("od", [128, 512], F32)[:, 0:W]
        P.stt(o, om[1], nlam, om[0], ALU.mult, ALU.add)
        sq = P.tmp("asq", [128, 512], BF16)[:, 0:W]
        P.act(sq, o, AF.Square)
        ps = SB[sctr[0] % 4]
        sctr[0] += 1
        P.mm(ps[:, 0:W], C.ones, sq)
        rstd = P.tmp("arstd", [128, 512], F32)[:, 0:W]
        P.act(rstd, ps[:, 0:W], AF.Sqrt, bias=C.epsb, scale=1.0 / 128.0)
        P.recip(rstd, rstd)
        P.stt(dst, o, subn, rstd, ALU.mult, ALU.mult)

    for hh in range(4):
        kTh = P.tmp("kTh", [128, NKT * 128], BF16, n=1)
        vh = P.tmp("vh", [128, NKT, 128], BF16, n=1)
        P.dma(kTh, D_(dr["kT"][hh]))
        P.dma(vh, D_(dr["v"][hh]))
        for qt in range(TOK // 512):
            attn_tile(kTh, vh, qT[:, hh, qt * 512:(qt + 1) * 512], 512, list(range(NKT)),
                      mix[:, 4 + hh, qt * 512:(qt + 1) * 512])
        attn_tile(kTh, vh, qcT[:, hh, :], CTXL, [NKT - 2, NKT - 1], mixc[:, 4 + hh, :])
    P.held.clear()
    P.ps_i = 0
    P.release(m_att)

    tiles = [(lambda c, t=t: xres[:, c, t * 512:(t + 1) * 512], 512, False) for t in range(TOK // 512)]
    tiles.append((lambda c: xc[:, c, :], CTXL, True))
    for ti, (xf, W, is_ctx) in enumerate(tiles):
        src = mixc if is_ctx else mix[:, :, ti * 512:(ti + 1) * 512]
        G = G1c if is_ctx else G1
        for dc in range(8):
            ps = P.ps()
            for kc in range(8):
                P.mm(ps[:, 0:W], w_out[:, kc, dc * 128:(dc + 1) * 128], src[:, kc, :], start=(kc == 0), stop=(kc == 7))
            P.stt(xf(dc), ps[:, 0:W], G[:, dc:dc + 1], xf(dc), ALU.mult, ALU.add)
    if debug:
        P.add("sp", lambda e: e.dma_start(out=o_x1.rearrange("c p t -> p c t"), in_=xres.ap), reads=[xres], writes=[outb], dma=True)
    P.release(m_mix)
    moe_layer(C, dr, 0, tiles, A2, Sh2, G2, A2c, Sh2c, G2c)
    P.add("sp", lambda e: e.dma_start(out=o_x2.rearrange("c p t -> p c t"), in_=xres.ap), reads=[xres], writes=[outb], dma=True)
    if debug:
        P.add("sp", lambda e: e.dma_start(out=o_c2.rearrange("c p t -> p c t"), in_=xc.ap), reads=[xc], writes=[outb], dma=True)

    w1 = P.sb([128, 8, 2048], BF16, "w1in")
    for kc in range(8):
        P.dma(w1[:, kc, :], D_(dr["od_w_in"][kc * 128:(kc + 1) * 128, :]), q="pool")
    for ti, (xf, W, is_ctx) in enumerate(tiles):
        A, Sh = (A1nc, Sh1nc) if is_ctx else (A1n, Sh1n)
        h1 = P.tmp(f"h1_{W}", [128, 8, W], BF16)
        ps = P.ps()
        for c in range(8):
            s_ = P.tmp(f"sq{W}", [128, W], BF16)
            P.act(s_, xf(c), AF.Square)
            P.mm(ps[:, 0:W], C.ones, s_, start=(c == 0), stop=(c == 7))
        rstd = C.rstd_from_ps(ps[:, 0:W], W, 1.0 / D)
        for c in range(8):
            t = P.tmp(f"nt{W}", [128, W], F32)
            P.tt(t, xf(c), rstd, ALU.mult)
            P.act(h1[:, c, :], t, AF.Identity, bias=Sh[:, c:c + 1], scale=A[:, c:c + 1])
        for f in range(16):
            if is_ctx and f < 8:
                continue
            ps = P.ps()
            for kc in range(8):
                P.mm(ps[:, 0:W], w1[:, kc, f * 128:(f + 1) * 128], h1[:, kc, :], start=(kc == 0), stop=(kc == 7))
            if f < 8:
                yg = P.tmp("yg", [128, W], BF16, n=3)
                P.act(yg, ps[:, 0:W], AF.Gelu_apprx_tanh)
                P.add("sp", lambda e, yg=yg, f=f, ti=ti: e.dma_start(out=o_yg[f, :, ti * 512:(ti + 1) * 512], in_=yg.ap),
                      reads=[yg], writes=[outb], dma=True)
            else:
                uu = P.tmp(f"uu{W}", [128, W], F32, n=3)
                P.copy(uu, ps[:, 0:W], eng="act")
                if is_ctx:
                    P.add("sp", lambda e, uu=uu, f=f: e.dma_start(out=o_uc[f - 8], in_=uu.ap), reads=[uu], writes=[outb], dma=True)
                else:
                    P.add("sp", lambda e, uu=uu, f=f, ti=ti: e.dma_start(out=o_u[f - 8, :, ti * 512:(ti + 1) * 512], in_=uu.ap),
                          reads=[uu], writes=[outb], dma=True)
    P.emit(final_reads=[outb])
    return nc, P


_CACHE = {}


def _get(name, fn):
    if name not in _CACHE:
        _CACHE[name] = fn()
    return _CACHE[name]


def host_sel():
    sel = np.zeros((16, 16, 128), np.float32)
    for e in range(16):
        sel[e, e, :] = 1.0
    return sel


def l1_inputs(r, inp, cf, cb, cosT, sinT):
    b, j = r // 4, r % 4
    s0 = j * TOK
    xT = np.ascontiguousarray(inp["x"][b].T)
    xh = np.zeros((D, 2), np.float32)
    hm = np.zeros((128, 2), np.float32)
    if s0 > 0:
        xh[:, 0] = xT[:, s0 - 1]
        hm[:, 0] = 1
    if s0 + TOK < SEQ:
        xh[:, 1] = xT[:, s0 + TOK]
        hm[:, 1] = 1
    cvec = np.stack([chunkT(inp["c"][b]), chunkT(inp["c_ctx"])], axis=-1)
    return dict(cf=cf, cb=cb, xo=np.ascontiguousarray(xT[:, s0:s0 + TOK]), xh=xh, hmask=hm,
                ctx=np.ascontiguousarray(inp["ctx"][b].T), cvec=np.ascontiguousarray(cvec),
                ada_w=inp["ada_w"], ada_bT=np.ascontiguousarray(inp["ada_b"].reshape(2, 48, 128).transpose(0, 2, 1)),
                nmixT=np.stack([chunkT(inp["norm_mix"][l]) for l in range(2)]),
                w_in=inp["ev_w_in"][0],
                convw=np.ascontiguousarray(inp["ev_conv_w"][0].reshape(3, 4, 128).transpose(2, 1, 0)),
                convb=np.ascontiguousarray(chunkT(inp["ev_conv_b"][0])),
                qkn=np.ascontiguousarray(np.stack([np.tile(inp["ev_q_norm"][0], 2), np.tile(inp["ev_k_norm"][0], 2)], axis=-1)),
                cos=np.ascontiguousarray(cosT[:, s0:s0 + TOK]), sin=np.ascontiguousarray(sinT[:, s0:s0 + TOK]))


def moe_inputs(inp, l):
    wr = np.concatenate([inp["moe_w_grp"][l], inp["moe_w_rt"][l].reshape(D, 16)], axis=1)[None]
    rb = np.concatenate([inp["moe_b_grp"][l], inp["moe_b_rt"][l].reshape(16)])
    rb = np.ascontiguousarray(np.broadcast_to(rb[None, None, :], (1, 128, 20)))
    return dict(moe_wr=np.ascontiguousarray(wr), moe_rb=rb, sel=host_sel(),
                moe_wg=inp["moe_w_gate"][l:l + 1], moe_wu=inp["moe_w_up"][l:l + 1], moe_wd=inp["moe_w_down"][l:l + 1])


def l2_inputs(r, inp, o1, cf, cb, xo_list):
    b = r // 4
    grp = [o1[4 * b + j] for j in range(4)]
    kT = np.concatenate([g["o_kT"] for g in grp] + [grp[0]["o_kcT"]], axis=2)
    v = np.concatenate([g["o_v"] for g in grp] + [grp[0]["o_vc"]], axis=0)
    v = np.ascontiguousarray(v.reshape(NKT, 128, 4, 128).transpose(2, 1, 0, 3))
    d = dict(cf=cf, cb=cb, kT=np.ascontiguousarray(kT), v=v, qT=o1[r]["o_qT"], qcT=o1[r]["o_qcT"],
             oa=o1[r]["o_oa"], oac=o1[r]["o_oac"], xo=xo_list[r], ctx=np.ascontiguousarray(inp["ctx"][b].T),
             mods=o1[r]["o_mods"],
             lamp=np.ascontiguousarray(np.stack([inp["ev_lam_q1"][0], inp["ev_lam_k1"][0], inp["ev_lam_q2"][0], inp["ev_lam_k2"][0]], axis=-1)),
             subn=np.ascontiguousarray(inp["ev_sub_norm"][0].reshape(128, 1)),
             w_out=inp["ev_w_out"][0], nffnT=chunkT(inp["norm_ffn"][0]), nmix1T=chunkT(inp["norm_mix"][1]),
             od_w_in=inp["od_w_in"][0])
    d.update(moe_inputs(inp, 0))
    return d


LRU_PW = [2048]


def lru_params(C, dr):
    P = C.P
    prm = {}
    prm["cw"] = P.sb([128, 8, 2, 4], F32, "lcw")
    P.dma(prm["cw"], D_(dr["l_cw"]))
    for k in ("l_cb", "l_ba", "l_bx", "l_lam"):
        prm[k] = P.sb([128, 8, 2], F32, k)
        P.dma(prm[k], D_(dr[k]))
    prm["wa"] = P.sb([128, 2, 8, 128], BF16, "lwa")
    prm["wx"] = P.sb([128, 2, 8, 128], BF16, "lwx")
    P.dma(prm["wa"], D_(dr["l_wa"]), q="pool")
    P.dma(prm["wx"], D_(dr["l_wx"]), q="pool")
    e = P.sb([128, 8, 2], F32, "l_e")
    P.act(e, prm["l_lam"], AF.Exp, scale=-1.0)
    P.act(e, e, AF.Ln, bias=C.onesf[:, 0:1])
    prm["s1"] = P.sb([128, 8, 2], F32, "l_s1")
    prm["s2"] = P.sb([128, 8, 2], F32, "l_s2")
    P.ts(prm["s1"], e, -8.0, None, ALU.mult)
    P.ts(prm["s2"], e, -16.0, None, ALU.mult)
    return prm


def lru_drams(nc, dr):
    dr["l_cw"] = din(nc, "l_cw", [128, 8, 2, 4])
    for k in ("l_cb", "l_ba", "l_bx", "l_lam"):
        dr[k] = din(nc, k, [128, 8, 2])
    dr["l_wa"] = din(nc, "l_wa", [128, 2, 8, 128])
    dr["l_wx"] = din(nc, "l_wx", [128, 2, 8, 128])


def lru_coeffs(C, prm, ub, W, c, d, nb=2):
    P = C.P
    cw = prm["cw"]
    o0 = 0 if d == 0 else 3
    uc = P.tmp("l_uc", [128, LRU_PW[0]], F32, n=nb)[:, 0:W]
    P.ts(uc, ub[:, o0:o0 + W], cw[:, c, d, 0:1], prm["l_cb"][:, c, d:d + 1], ALU.mult, ALU.add)
    for k in range(1, 4):
        P.stt(uc, ub[:, o0 + k:o0 + k + W], cw[:, c, d, k:k + 1], uc, ALU.mult, ALU.add)
    ucb = P.tmp("l_ucb", [128, LRU_PW[0]], BF16, n=nb)[:, 0:W]
    P.copy(ucb, uc, eng="pool")
    r = P.tmp("l_r", [128, LRU_PW[0]], F32, n=nb)[:, 0:W]
    ig = P.tmp("l_ig", [128, LRU_PW[0]], F32, n=nb)[:, 0:W]
    for t0 in range(0, W, 512):
        w = min(512, W - t0)
        ps = P.ps()
        P.mm(ps[:, 0:w], prm["wa"][:, d, c, :], ucb[:, t0:t0 + w])
        P.act(r[:, t0:t0 + w], ps[:, 0:w], AF.Sigmoid, bias=prm["l_ba"][:, c, d:d + 1])
        ps2 = P.ps()
        P.mm(ps2[:, 0:w], prm["wx"][:, d, c, :], ucb[:, t0:t0 + w])
        P.act(ig[:, t0:t0 + w], ps2[:, 0:w], AF.Sigmoid, bias=prm["l_bx"][:, c, d:d + 1])
    a = P.tmp("l_a", [128, LRU_PW[0]], F32, n=nb)[:, 0:W]
    P.act(a, r, AF.Exp, scale=prm["s1"][:, c, d:d + 1])
    t = P.tmp("l_t", [128, LRU_PW[0]], F32, n=nb)[:, 0:W]
    P.act(t, r, AF.Exp, scale=prm["s2"][:, c, d:d + 1])
    P.act(t, t, AF.Sqrt, bias=C.onesf[:, 0:1], scale=-1.0)
    P.tt(ig, ig, uc, ALU.mult)
    P.tt(ig, ig, t, ALU.mult)
    return a, ig


def lru_scan(C, a, bb, W, d, init, out):
    P = C.P
    if d == 0:
        P.scan(out, a, bb, init)
    else:
        P.scan(out[:, ::-1], a[:, ::-1], bb[:, ::-1], init)


def load_ub(C, dr_u, dr_hp, dr_hn, c, W, key, nb=2):
    P = C.P
    ub = P.tmp(key, [128, W + 6], F32, n=nb)
    if dr_hp is None:
        P.memset(ub[:, 0:3], 0.0)
        P.memset(ub[:, W + 3:W + 6], 0.0)
    else:
        P.dma(ub[:, 0:3], D_(dr_hp[c]))
        P.dma(ub[:, W + 3:W + 6], D_(dr_hn[c]))
    P.dma(ub[:, 3:3 + W], D_(dr_u[c]))
    return ub


def build_L3():
    nc = bass.Bass("TRN2", target_bir_lowering=False)
    dr = {}
    dr["cf"] = din(nc, "cf", [128, 384])
    dr["cb"] = din(nc, "cb", [128, 256])
    dr["u"] = din(nc, "u", [8, 128, TOK])
    dr["uhp"] = din(nc, "uhp", [8, 128, 3])
    dr["uhn"] = din(nc, "uhn", [8, 128, 3])
    dr["uc"] = din(nc, "uc", [8, 128, CTXL])
    lru_drams(nc, dr)
    o_sum = dout(nc, "o_sum", [128, 8, 2, 3])
    P = Prog(nc)
    C = Ctx(nc, P, dr)
    outb = T(None, [Buf()])
    prm = lru_params(C, dr)
    summ = P.sb([128, 8, 2, 3], F32, "summ")
    for c in range(8):
        ubc = load_ub(C, dr["uc"], None, None, c, CTXL, "ubc")
        ub = load_ub(C, dr["u"], dr["uhp"], dr["uhn"], c, TOK, "ub")
        for d in range(2):
            a, bb = lru_coeffs(C, prm, ubc, CTXL, c, d)
            h = P.tmp("l_h", [128, 2048], F32)[:, 0:CTXL]
            lru_scan(C, a, bb, CTXL, d, 0.0, h)
            P.copy(summ[:, c, d, 2:3], h[:, CTXL - 1:CTXL] if d == 0 else h[:, 0:1])
            a, bb = lru_coeffs(C, prm, ub, TOK, c, d)
            h = P.tmp("l_h", [128, 2048], F32)[:, 0:TOK]
            lru_scan(C, a, bb, TOK, d, 0.0, h)
            P.copy(summ[:, c, d, 1:2], h[:, TOK - 1:TOK] if d == 0 else h[:, 0:1])
            P.reduce(summ[:, c, d, 0:1], a, ALU.mult)
    P.add("sp", lambda e: e.dma_start(out=o_sum, in_=summ.ap), reads=[summ], writes=[outb], dma=True)
    P.emit(final_reads=[outb])
    return nc, P


def build_L4():
    nc = bass.Bass("TRN2", target_bir_lowering=False)
    dr = {}
    dr["cf"] = din(nc, "cf", [128, 384])
    dr["cb"] = din(nc, "cb", [128, 256])
    dr["u"] = din(nc, "u", [8, 128, TOK])
    dr["uhp"] = din(nc, "uhp", [8, 128, 3])
    dr["uhn"] = din(nc, "uhn", [8, 128, 3])
    dr["yg"] = din(nc, "yg", [8, 128, TOK], BF16)
    dr["x2"] = din(nc, "x2", [8, 128, TOK])
    dr["summ"] = din(nc, "summ", [4, 128, 8, 2, 3])
    dr["jmask"] = din(nc, "jmask", [128, 2, 4])
    dr["mods"] = din(nc, "mods", [2, 128, 48, 2])
    dr["nffnT"] = din(nc, "nffnT", [128, 8])
    dr["w_out"] = din(nc, "w_out", [D, D])
    lru_drams(nc, dr)
    moe_drams(nc, dr)
    o_out = dout(nc, "o_out", [8, 128, TOK])
    P = Prog(nc)
    C = Ctx(nc, P, dr)
    outb = T(None, [Buf()])
    prm = lru_params(C, dr)
    mods1 = P.sb([128, 48, 2], F32, "mods1")
    P.dma(mods1, D_(dr["mods"][1]))
    nffn = P.sb([128, 8], F32, "nffn")
    P.dma(nffn, D_(dr["nffnT"]))
    _, _, G1 = mod_scalars(C, mods1, nffn, 0, 0, "m1l")
    A2, Sh2, G2 = mod_scalars(C, mods1, nffn, 1, 0, "f1l")
    sm = P.sb([128, 4, 8, 2, 3], F32, "sm")
    for j in range(4):
        P.dma(sm[:, j], D_(dr["summ"][j]))
    jm = P.sb([128, 2, 4], F32, "jm")
    P.dma(jm, D_(dr["jmask"]))
    h0 = P.sb([128, 8, 2], F32, "h0")
    cand = P.sb([128, 8], F32, "cand")
    for d in range(2):
        P.copy(h0[:, :, d], sm[:, 0, :, d, 2])
        order = range(4) if d == 0 else range(3, -1, -1)
        for j in order:
            P.tt(cand, sm[:, j, :, d, 0], h0[:, :, d], ALU.mult)
            P.tt(cand, cand, sm[:, j, :, d, 1], ALU.add)
            P.tt(cand, cand, h0[:, :, d], ALU.subtract)
            P.stt(h0[:, :, d], cand, jm[:, d, j:j + 1], h0[:, :, d], ALU.mult, ALU.add)
    xres = P.sb([128, 8, TOK], F32, "xres")
    P.dma(xres, D_(dr["x2"].rearrange("c p t -> p c t")))
    m_mix = P.mark()
    w_out = P.sb([128, 8, D], BF16, "w_out")
    for kc in range(8):
        P.dma(w_out[:, kc, :], D_(dr["w_out"][kc * 128:(kc + 1) * 128, :]), q="pool")
    mix = P.sb([128, 8, TOK], BF16, "mix")
    m_sc = P.mark()
    for c in range(8):
        ub = load_ub(C, dr["u"], dr["uhp"], dr["uhn"], c, TOK, "ub", nb=1)
        yg = P.tmp("ygl", [128, TOK], BF16, n=1)
        P.dma(yg, D_(dr["yg"][c]))
        hs = []
        for d in range(2):
            a, bb = lru_coeffs(C, prm, ub, TOK, c, d, nb=1)
            h = P.tmp("l_h", [128, 2048], F32)
            lru_scan(C, a, bb, TOK, d, h0[:, c, d:d + 1], h)
            hs.append(h)
        P.tt(hs[0], hs[0], hs[1], ALU.add)
        P.tt(mix[:, c, :], hs[0], yg, ALU.mult)
    P.release(m_sc)
    tiles = [(lambda c, t=t: xres[:, c, t * 512:(t + 1) * 512], 512, False) for t in range(TOK // 512)]
    for ti, (xf, W, _) in enumerate(tiles):
        for dc in range(8):
            ps = P.ps()
            for kc in range(8):
                P.mm(ps[:, 0:W], w_out[:, kc, dc * 128:(dc + 1) * 128], mix[:, kc, ti * 512:(ti + 1) * 512],
                     start=(kc == 0), stop=(kc == 7))
            P.stt(xf(dc), ps[:, 0:W], G1[:, dc:dc + 1], xf(dc), ALU.mult, ALU.add)
    P.release(m_mix)
    moe_layer(C, dr, 0, tiles, A2, Sh2, G2)
    P.add("sp", lambda e: e.dma_start(out=o_out.rearrange("c p t -> p c t"), in_=xres.ap), reads=[xres], writes=[outb], dma=True)
    P.emit(final_reads=[outb])
    return nc, P


def lru_inputs(inp):
    cw = np.ascontiguousarray(inp["od_conv_w"][0].reshape(2, 4, 8, 128).transpose(3, 2, 0, 1))

    def v(a):
        return np.ascontiguousarray(a.reshape(2, 8, 128).transpose(2, 1, 0))

    def w(a):
        return np.ascontiguousarray(a.transpose(2, 0, 1, 3))
    return dict(l_cw=cw, l_cb=v(inp["od_conv_b"][0]), l_ba=v(inp["od_b_a"][0]), l_bx=v(inp["od_b_x"][0]),
                l_lam=v(inp["od_lam"][0]), l_wa=w(inp["od_w_a"][0]), l_wx=w(inp["od_w_x"][0]))


def halo_inputs(r, o2):
    b, j = r // 4, r % 4
    z = np.zeros((8, 128, 3), np.float32)
    hp = np.ascontiguousarray(o2[r - 1]["o_u"][:, :, TOK - 3:TOK]) if j > 0 else z
    hn = np.ascontiguousarray(o2[r + 1]["o_u"][:, :, 0:3]) if j < 3 else z
    return hp, hn


def kernel_unfused(**inp):
    inp = {k: np.asarray(v) for k, v in inp.items()}
    cf, cb = host_consts()
    cosT, sinT = rope_tables()
    cores = list(range(NCORE))
    nc1, _ = _get("L1", build_L1)
    ims1 = [l1_inputs(r, inp, cf, cb, cosT, sinT) for r in cores]
    o1 = run_bass_kernel_spmd(nc1, ims1, core_ids=cores).results
    xo_list = [im["xo"] for im in ims1]
    nc2, _ = _get("L2", build_L2)
    ims2 = [l2_inputs(r, inp, o1, cf, cb, xo_list) for r in cores]
    o2 = run_bass_kernel_spmd(nc2, ims2, core_ids=cores).results
    del ims1, ims2
    lin = lru_inputs(inp)
    nc3, _ = _get("L3", build_L3)
    ims3 = []
    for r in cores:
        hp, hn = halo_inputs(r, o2)
        d = dict(cf=cf, cb=cb, u=o2[r]["o_u"], uhp=hp, uhn=hn, uc=o2[r]["o_uc"])
        d.update(lin)
        ims3.append(d)
    o3 = run_bass_kernel_spmd(nc3, ims3, core_ids=cores).results
    nc4, _ = _get("L4", build_L4)
    ims4 = []
    for r in cores:
        b, j = r // 4, r % 4
        hp, hn = halo_inputs(r, o2)
        summ = np.ascontiguousarray(np.stack([o3[4 * b + jj]["o_sum"] for jj in range(4)], axis=0))
        jm = np.zeros((128, 2, 4), np.float32)
        for jj in range(4):
            jm[:, 0, jj] = 1.0 if jj < j else 0.0
            jm[:, 1, jj] = 1.0 if jj > j else 0.0
        d = dict(cf=cf, cb=cb, u=o2[r]["o_u"], uhp=hp, uhn=hn, yg=o2[r]["o_yg"], x2=o2[r]["o_x2"], summ=summ, jmask=jm,
                 mods=o1[r]["o_mods"], nffnT=chunkT(inp["norm_ffn"][1]), w_out=inp["od_w_out"][0])
        d.update(lin)
        d.update(moe_inputs(inp, 1))
        ims4.append(d)
    o4 = run_bass_kernel_spmd(nc4, ims4, core_ids=cores).results
    out = np.empty((2, SEQ, D), np.float32)
    for r in cores:
        b, j = r // 4, r % 4
        out[b, j * TOK:(j + 1) * TOK, :] = o4[r]["o_out"].reshape(D, TOK).T
    return out


GROUPS = [[0, 1, 2, 3], [4, 5, 6, 7]]
HALF = 512


def qk_stage_a(C, ps_in, W, gain, rope, out):
    P = C.P
    k_sb = P.tmp(f"qk_k{W}", [128, W], F32, n=2)
    P.copy(k_sb, ps_in, eng="act")
    sq = P.tmp(f"qk_sq{W}", [128, W], BF16)
    P.act(sq, ps_in, AF.Square)
    ps2 = P.ps()
    P.mm(ps2[:, 0:W], C.bones, sq)
    rs = C.rstd_from_ps(ps2[:, 0:W], W, 1.0 / 64.0, name="qk_rs", n=2)
    if not rope:
        P.stt(out, k_sb, gain, rs, ALU.mult, ALU.mult)
        return None
    kh = P.tmp(f"qk_kh{W}", [128, W], F32, n=2)
    P.stt(kh, k_sb, gain, rs, ALU.mult, ALU.mult)
    ps3 = P.ps()
    P.mm(ps3[:, 0:W], C.perm, kh)
    return dict(kh=kh, ps3=ps3, out=out)


def qk_stage_b(C, st, W, cos, sin):
    P = C.P
    t1 = P.tmp(f"qk_t1{W}", [128, W], F32, n=2)
    P.tt(t1, st["kh"], cos, ALU.mult)
    t2 = P.tmp(f"qk_t2{W}", [128, W], F32, n=2)
    P.tt(t2, st["ps3"][:, 0:W], sin, ALU.mult)
    P.tt(st["out"], t1, t2, ALU.add)


def build_fused():
    nc = bass.Bass("TRN2", target_bir_lowering=False)
    dr = {}
    dr["cf"] = din(nc, "cf", [128, 384])
    dr["cb"] = din(nc, "cb", [128, 256])
    dr["xo"] = din(nc, "xo", [D, TOK])
    dr["xh"] = din(nc, "xh", [D, 2])
    dr["hmask"] = din(nc, "hmask", [128, 2])
    dr["ctx"] = din(nc, "ctx", [D, CTXL])
    dr["cvec"] = din(nc, "cvec", [128, 8, 2])
    dr["ada_wq"] = din(nc, "ada_wq", [2, D, 1536])
    dr["ada_bq"] = din(nc, "ada_bq", [128, 2, 12])
    dr["nmixT"] = din(nc, "nmixT", [2, 128, 8])
    dr["nffnT"] = din(nc, "nffnT", [2, 128, 8])
    dr["w_in"] = din(nc, "w_in", [D, 3072])
    dr["convw"] = din(nc, "convw", [128, 4, 3])
    dr["convb"] = din(nc, "convb", [128, 4])
    dr["qkn"] = din(nc, "qkn", [128, 2])
    dr["cos"] = din(nc, "cos", [128, TOK])
    dr["sin"] = din(nc, "sin", [128, TOK])
    dr["lamp"] = din(nc, "lamp", [64, 4])
    dr["subn"] = din(nc, "subn", [128, 1])
    dr["w_out"] = din(nc, "w_out", [D, D])
    dr["od_w_in"] = din(nc, "od_w_in", [D, 2048])
    dr["od_w_out"] = din(nc, "od_w_out", [D, D])
    dr["jmask"] = din(nc, "jmask", [128, 2, 4])
    dr["hsel"] = din(nc, "hsel", [128, 2, 4])
    lru_drams(nc, dr)
    dr["moe_wr"] = din(nc, "moe_wr", [2, D, 20])
    dr["moe_rb"] = din(nc, "moe_rb", [2, 128, 20])
    dr["sel"] = din(nc, "sel", [16, 16, 128])
    dr["moe_wg"] = din(nc, "moe_wg", [2, 16, D, 512])
    dr["moe_wu"] = din(nc, "moe_wu", [2, 16, D, 512])
    dr["moe_wd"] = din(nc, "moe_wd", [2, 16, 512, D])
    o_out = dout(nc, "o_out", [8, 128, TOK])
    kv_in = [nc.dram_tensor(f"kv_in{i}", [1024, 512], BF16).ap() for i in range(4)]
    kv_all = [nc.dram_tensor(f"kv_all{i}", [4 * 1024, 512], BF16).ap() for i in range(4)]
    hx_in = nc.dram_tensor("hx_in", [128, 48], F32).ap()
    hx_all = nc.dram_tensor("hx_all", [4 * 128, 48], F32).ap()
    sm_in = nc.dram_tensor("sm_in", [128, 48], F32).ap()
    sm_all = nc.dram_tensor("sm_all", [4 * 128, 48], F32).ap()
    kv_in_t = [T(kv_in[i], [Buf()]) for i in range(4)]
    kv_all_t = [T(kv_all[i], [Buf()]) for i in range(4)]
    hx_in_t, hx_all_t = T(hx_in, [Buf()]), T(hx_all, [Buf()])
    sm_in_t, sm_all_t = T(sm_in, [Buf()]), T(sm_all, [Buf()])
    mq_in = nc.dram_tensor("mq_in", [128, 48], F32).ap()
    mq_all = nc.dram_tensor("mq_all", [4 * 128, 48], F32).ap()
    mq_in_t, mq_all_t = T(mq_in, [Buf()]), T(mq_all, [Buf()])

    P = Prog(nc)
    C = Ctx(nc, P, dr)
    outb = T(None, [Buf()])
    LAM_INIT = 0.8 - 0.6 * math.exp(-0.3 * 0)

    def allgather(src_t, dst_t):
        P.add("pool", lambda e: e.collective_compute("AllGather", ALU.bypass, replica_groups=GROUPS,
                                                     ins=[src_t.ap.opt()], outs=[dst_t.ap.opt()]),
              reads=[src_t], writes=[dst_t], cc=True)

    cvec = P.sb([128, 8, 2], F32, "cvec")
    P.dma(cvec, D_(dr["cvec"]))
    mods = [P.sb([128, 48, 2], F32, f"mods{l}") for l in range(2)]
    m_md = P.mark()
    scv = P.sb([128, 8, 2], F32, "silu_c")
    P.act(scv, cvec, AF.Silu)
    bq = P.sb([128, 2, 12], F32, "bq")
    P.dma(bq, D_(dr["ada_bq"]))
    psm = P.ps(hold=True)
    for l in range(2):
        for hf_ in range(2):
            wq = P.tmp("adawq", [128, 8, 768], F32, n=2)
            P.dma(wq, D_(dr["ada_wq"][l].rearrange("(kc p) f -> p kc f", p=128)[:, :, hf_ * 768:(hf_ + 1) * 768]))
            for fc in range(6):
                g = l * 12 + hf_ * 6 + fc
                for kc in range(8):
                    P.mm(psm[:, 2 * g:2 * g + 2], wq[:, kc, fc * 128:(fc + 1) * 128], scv[:, kc, :],
                         start=(kc == 0), stop=(kc == 7))
    mq = P.sb([128, 2, 12, 2], F32, "mq")
    P.tt(mq, psm[:, 0:48].re("p (l g c) -> p l g c", l=2, c=2),
         T(bq.ap.unsqueeze(3).to_broadcast([128, 2, 12, 2]), bq.bufs), ALU.add)
    P.ps_free(psm)
    P.dma(mq_in_t, mq.re("p l g c -> p (l g c)"))
    allgather(mq_in_t, mq_all_t)
    mqa = P.sb([128, 4, 48], F32, "mqa")
    P.dma(mqa, T(mq_all.rearrange("(r p) f -> p r f", p=128), mq_all_t.bufs))
    for l in range(2):
        P.copy(mods[l].re("p (r i) c -> p r i c", r=4), mqa[:, :, l * 24:(l + 1) * 24].re("p r (i c) -> p r i c", c=2))
    P.release(m_md)
    nmix = [P.sb([128, 8], F32, f"nmix{l}") for l in range(2)]
    nffn = [P.sb([128, 8], F32, f"nffn{l}") for l in range(2)]
    for l in range(2):
        P.dma(nmix[l], D_(dr["nmixT"][l]))
        P.dma(nffn[l], D_(dr["nffnT"][l]))
    A_lat, Sh_lat, G1 = mod_scalars(C, mods[0], nmix[0], 0, 0, "a0l")
    A_ctx, Sh_ctx, G1c = mod_scalars(C, mods[0], nmix[0], 0, 1, "a0c")
    A2, Sh2, G2 = mod_scalars(C, mods[0], nffn[0], 1, 0, "f0l")
    A2c, Sh2c, G2c = mod_scalars(C, mods[0], nffn[0], 1, 1, "f0c")
    A1n, Sh1n, G1n = mod_scalars(C, mods[1], nmix[1], 0, 0, "a1l")
    A1nc, Sh1nc, _ = mod_scalars(C, mods[1], nmix[1], 0, 1, "a1c")
    A2n, Sh2n, G2n = mod_scalars(C, mods[1], nffn[1], 1, 0, "f1l")
    convw = P.sb([128, 4, 3], F32, "convw")
    P.dma(convw, D_(dr["convw"]))
    convb = P.sb([128, 4], F32, "convb")
    P.dma(convb, D_(dr["convb"]))
    qkn = P.sb([128, 2], F32, "qkn")
    P.dma(qkn, D_(dr["qkn"]))
    hmask = P.sb([128, 2], F32, "hmask")
    P.dma(hmask, D_(dr["hmask"]))
    jm = P.sb([128, 2, 4], F32, "jm")
    P.dma(jm, D_(dr["jmask"]))
    hsel = P.sb([128, 2, 4], F32, "hsel")
    P.dma(hsel, D_(dr["hsel"]))
    lamp = P.sb([64, 4], F32, "lamp")
    P.dma(lamp, D_(dr["lamp"]))
    lpr = P.sb([64, 2], F32, "lpr")
    P.tt(lpr[:, 0:1], lamp[:, 0:1], lamp[:, 1:2], ALU.mult)
    P.tt(lpr[:, 1:2], lamp[:, 2:3], lamp[:, 3:4], ALU.mult)
    psl = P.ps()
    P.mm(psl[:, 0:2], C.onesf[0:64, :], lpr)
    lex = P.sb([128, 2], F32, "lex")
    P.act(lex, psl[:, 0:2], AF.Exp)
    nlam = P.sb([128, 1], F32, "nlam")
    P.tt(nlam, lex[:, 1:2], lex[:, 0:1], ALU.subtract)
    P.ts(nlam, nlam, -LAM_INIT, None, ALU.add)
    subn = P.sb([128, 1], F32, "subn")
    P.dma(subn, D_(dr["subn"]))
    P.ts(subn, subn, 1.0 - LAM_INIT, None, ALU.mult)

    m_base = P.mark()
    mix = P.sb([128, 4, TOK], BF16, "mixa")
    mixc = P.sb([128, 4, CTXL], BF16, "mixca")
    qT = P.sb([128, 4, TOK], BF16, "qT")
    qcT = P.sb([128, 4, CTXL], BF16, "qcT")
    kcT = P.sb([128, 4, CTXL], BF16, "kcT")
    vc = P.sb([128, 2, 512], BF16, "vc")
    m_front = P.mark()
    cbs = P.sb([128, 4, TOK], BF16, "cbs")
    ub = P.sb([128, 4, TOK + 2], BF16, "ub")
    cbc = P.sb([128, 4, CTXL], BF16, "cbc")
    ubc = P.sb([128, 4, CTXL + 2], BF16, "ubc")
    P.memset(ubc[:, :, 0:1], 0.0)
    P.memset(ubc[:, :, CTXL + 1:CTXL + 2], 0.0)
    w_in = P.sb([128, 8, 3072], BF16, "w_in")
    for kc in range(8):
        P.dma(w_in[:, kc, :], D_(dr["w_in"][kc * 128:(kc + 1) * 128, :]), q="pool")

    def proj(h, W, f):
        ps = P.ps()
        for kc in range(8):
            P.mm(ps[:, 0:W], w_in[:, kc, f * 128:(f + 1) * 128], h[:, kc, 0:W], start=(kc == 0), stop=(kc == 7))
        return ps[:, 0:W]

    vin_v = [kv_in[t][512:1024, :].rearrange("(h p) (k v) -> p k h v", p=128, v=128) for t in range(4)]

    def front_norm(xsrc, W, A, Sh):
        xt = P.tmp(f"xt{W}", [128, 8, W], F32, n=1)
        P.dma(xt, D_(xsrc.rearrange("(c p) t -> p c t", p=128)))
        h = P.tmp(f"h{W}", [128, 8, W], BF16, n=2)
        norm_mod(C, xt, W, A, Sh, h)
        return h

    def front_tile(xsrc, W, A, Sh, rope, dst, pre_h=None, mid_hook=None):
        t0 = dst["t0"]
        if rope:
            cs_c = P.tmp("cos_t", [128, W], F32, n=1)
            cs_s = P.tmp("sin_t", [128, W], F32, n=1)
            P.dma(cs_c, D_(dr["cos"][:, t0:t0 + W]))
            P.dma(cs_s, D_(dr["sin"][:, t0:t0 + W]))
        h = pre_h if pre_h is not None else front_norm(xsrc, W, A, Sh)
        if dst.get("cb") is not None:
            for c in range(4):
                ps = proj(h, W, c)
                P.copy(dst["cb"][:, c, t0:t0 + W], ps, eng="act")
        for c in range(4):
            ps_cc = proj(h, W, 4 + c)
            cc = P.tmp(f"cc{W}", [128, W], F32)
            P.copy(cc, ps_cc, eng="act")
            ps_cx = proj(h, W, 8 + c)
            P.tt(dst["u"](c), cc, ps_cx, ALU.mult)
        if mid_hook is not None:
            mid_hook()
        if dst.get("q") is None:
            return
        hu = [(12 + hh, 0, hh) for hh in range(4)] + [(16 + hh, 1, hh) for hh in range(4)]

        def unit_out(kind, hh):
            if kind == 0:
                return dst["q"][:, hh, t0:t0 + W], None
            if dst.get("k") is not None:
                return dst["k"][:, hh, t0:t0 + W], None
            kt_ = P.tmp("kt_", [128, W], BF16, n=3)
            return kt_, hh

        def finish(o, hh_store):
            if hh_store is not None:
                nb_ = Buf()
                kv_in_t[t0 // 512].bufs.append(nb_)
                P.add("sp", lambda e, o=o, hh=hh_store, t0=t0: e.dma_start(out=kv_in[t0 // 512][hh * 128:(hh + 1) * 128, :], in_=o.ap),
                      reads=[o], writes=[T(None, [nb_])], dma=True)

        ps_next = proj(h, W, hu[0][0])
        prev = None
        for i, (f_, kind, hh) in enumerate(hu):
            ps_cur = ps_next
            if i + 1 < len(hu):
                ps_next = proj(h, W, hu[i + 1][0])
            o, hs_ = unit_out(kind, hh)
            st = qk_stage_a(C, ps_cur, W, qkn[:, kind:kind + 1], rope, o)
            if prev is not None:
                qk_stage_b(C, prev[0], W, cs_c, cs_s)
                finish(prev[1], prev[2])
            if st is None:
                finish(o, hs_)
                prev = None
            else:
                prev = (st, o, hs_)
        if prev is not None:
            qk_stage_b(C, prev[0], W, cs_c, cs_s)
            finish(prev[1], prev[2])
        for sub in range(W // 128):
            ps = P.ps()
            for kc in range(8):
                P.mm(ps, h[:, kc, sub * 128:(sub + 1) * 128], w_in[:, kc, 2560:3072], start=(kc == 0), stop=(kc == 7))
            if dst.get("v") is not None:
                P.copy(dst["v"][:, sub, :], ps, eng="act")
            else:
                vs = P.tmp("vs", [128, 512], BF16, n=2)
                P.copy(vs, ps, eng="act")
                nb_ = Buf()
                kv_in_t[t0 // 512].bufs.append(nb_)
                P.add("sp", lambda e, vs=vs, sub=sub, t0=t0: e.dma_start(
                    out=vin_v[t0 // 512][:, sub], in_=vs.ap.rearrange("p (h v) -> p h v", v=128)),
                    reads=[vs], writes=[T(None, [nb_])], dma=True)

    def conv_piece(ubuf, cbt, s0, w, dstm):
        for c in range(4):
            acc = P.tmp("cacc", [128, 512], F32, n=1)[:, 0:w]
            P.ts(acc, ubuf[:, c, 1 + s0:1 + s0 + w], convw[:, c, 1:2], convb[:, c:c + 1], ALU.mult, ALU.add)
            P.stt(acc, ubuf[:, c, s0:s0 + w], convw[:, c, 0:1], acc, ALU.mult, ALU.add)
            P.stt(acc, ubuf[:, c, 2 + s0:2 + s0 + w], convw[:, c, 2:3], acc, ALU.mult, ALU.add)
            P.tt(dstm[:, c, s0:s0 + w], acc, cbt[:, c, s0:s0 + w], ALU.mult)

    uh = P.sb([128, 4, 2], F32, "uh")
    m_own = P.mark()
    front_tile(dr["ctx"], CTXL, A_ctx, Sh_ctx, False,
               dict(cb=cbc, u=lambda c: ubc[:, c, 1:1 + CTXL], q=qcT, k=kcT, v=vc, t0=0))
    P.release(m_own)
    nxt_h = [front_norm(dr["xo"][:, 0:512], 512, A_lat, Sh_lat)]
    for t in range(TOK // 512):
        cur_h = nxt_h[0]

        def hook(t=t):
            if t + 1 < TOK // 512:
                nxt_h[0] = front_norm(dr["xo"][:, (t + 1) * 512:(t + 2) * 512], 512, A_lat, Sh_lat)
        front_tile(dr["xo"][:, t * 512:(t + 1) * 512], 512, A_lat, Sh_lat, True,
                   dict(cb=cbs, u=lambda c, t=t: ub[:, c, 1 + t * 512:1 + (t + 1) * 512], q=qT, k=None, v=None, t0=t * 512),
                   pre_h=cur_h, mid_hook=hook)
        allgather(kv_in_t[t], kv_all_t[t])
        if t == 1:
            front_tile(dr["xh"], 2, A_lat, Sh_lat, False, dict(u=lambda c: uh[:, c, :], t0=0))
            for c in range(4):
                P.tt(ub[:, c, 0:1], uh[:, c, 0:1], hmask[:, 0:1], ALU.mult)
                P.tt(ub[:, c, TOK + 1:TOK + 2], uh[:, c, 1:2], hmask[:, 1:2], ALU.mult)
        if t >= 1:
            conv_piece(ub, cbs, (t - 1) * 512, 512, mix)

    conv_piece(ub, cbs, TOK - 512, 512, mix)
    conv_piece(ubc, cbc, 0, CTXL, mixc)
    P.release(m_front)

    top0 = P.top
    xres = P.sb_top([128, 8, TOK], F32, "xres")
    top_x = P.top
    xc = P.sb_top([128, 8, CTXL], F32, "xc")
    xc_off = P.top
    w_out = P.sb([128, 8, D], BF16, "w_out")
    for kc in range(8):
        P.dma(w_out[:, kc, :], D_(dr["w_out"][kc * 128:(kc + 1) * 128, :]), q="pool")
    mixb = P.sb([128, 4, TOK], BF16, "mixb")
    mixcb = P.sb([128, 4, CTXL], BF16, "mixcb")
    m_att = P.mark()
    SB = [P.psum_banks[i] for i in range(4)]
    ACC = [(P.psum_banks[4], P.psum_banks[6]), (P.psum_banks[5], P.psum_banks[7])]
    P.held.update(range(8))
    sctr = [0]

    def attn_group(ksrc, vsrc, qsrc, W, kts, m):
        accO, accL = ACC[m]
        lo, hi = m * 64, (m + 1) * 64
        n = len(kts)

        qz = P.tmp("qz", [128, 512], BF16, n=1)[:, 0:W]
        P.memset(qz, 0.0, eng="pool")
        P.copy(qz[lo:hi, :], qsrc[lo:hi, :], eng="pool")

        def S_(i):
            sb_ = SB[sctr[0] % 4]
            sctr[0] += 1
            P.mm(sb_[:, 0:W], ksrc(kts[i]), qz)
            return sb_

        LOOK = 2
        pend_pt = []
        first_acc = [True]
        sq_ = [S_(i) for i in range(min(LOOK, n))]
        for i in range(n):
            if i + LOOK < n:
                sq_.append(S_(i + LOOK))
            cur = sq_[i]
            pt = P.tmp("pt", [128, 512], BF16, n=6)[:, 0:W]
            P.act(pt, cur[:, 0:W], AF.Exp, scale=0.125)
            P.mm(accO[:, 0:W], vsrc(kts[i]), pt, start=(i == 0), stop=(i == n - 1))
            pend_pt.append(pt)
            if len(pend_pt) == 4 or i == n - 1:
                lvl = list(pend_pt)
                pend_pt = []
                while len(lvl) > 2:
                    nl = []
                    for j2 in range(0, len(lvl) - 1, 2):
                        pp = P.tmp("pp", [128, 512], BF16, n=4)[:, 0:W]
                        P.tt(pp, lvl[j2], lvl[j2 + 1], ALU.add)
                        nl.append(pp)
                    if len(lvl) % 2 == 1:
                        nl.append(lvl[-1])
                    lvl = nl
                if first_acc[0]:
                    lacc = P.tmp("lacc", [128, 512], F32, n=1)[:, 0:W]
                    if len(lvl) == 2:
                        P.tt(lacc, lvl[0], lvl[1], ALU.add)
                    else:
                        P.copy(lacc, lvl[0])
                    first_acc[0] = False
                else:
                    if len(lvl) == 2:
                        pp = P.tmp("pp", [128, 512], BF16, n=4)[:, 0:W]
                        P.tt(pp, lvl[0], lvl[1], ALU.add)
                        lvl = [pp]
                    P.tt(lacc, lacc, lvl[0], ALU.add)
        P.mm(accL[:, 0:W], C.onesf, lacc)
        return accO, accL

    def attn_tile(ksrc, vsrc, qsrc, W, kts, dst):
        om = []
        for m in range(2):
            accO, accL = attn_group(ksrc, vsrc, qsrc, W, kts, m)
            rl = P.tmp("rl", [128, 512], F32, n=1)[:, 0:W]
            P.act(rl, accL[:, 0:W], AF.Ln)
            P.act(rl, rl, AF.Exp, scale=-1.0)
            o = P.tmp("om", [128, 512], F32, n=2)[:, 0:W]
            P.tt(o, accO[:, 0:W], rl, ALU.mult)
            om.append(o)
        o = P.tmp("od", [128, 512], F32, n=1)[:, 0:W]
        P.stt(o, om[1], nlam, om[0], ALU.mult, ALU.add)
        sq = P.tmp("asq", [128, 512], BF16, n=1)[:, 0:W]
        P.act(sq, o, AF.Square)
        ps = SB[sctr[0] % 4]
        sctr[0] += 1
        P.mm(ps[:, 0:W], C.ones, sq)
        rstd = P.tmp("arstd", [128, 512], F32, n=1)[:, 0:W]
        P.act(rstd, ps[:, 0:W], AF.Ln, bias=C.epsb, scale=1.0 / 128.0)
        P.act(rstd, rstd, AF.Exp, scale=-0.5)
        P.stt(dst, o, subn, rstd, ALU.mult, ALU.mult)

    kall_v = [kv_all[t].rearrange("(r s p) c -> s p r c", r=4, p=128) for t in range(4)]
    vall_v = [kv_all[t].rearrange("(r s p) (k v) -> s p r k v", r=4, p=128, v=128) for t in range(4)]
    NK0 = SEQ // 128
    NK0 = SEQ // 128
    KORDER = [r * 16 + t * 4 + kk for t in range(4) for r in range(4) for kk in range(4)] + [NK0, NK0 + 1]
    for hh in range(4):
        kTp = [P.tmp(f"kTh{t}", [128, 4, 512], BF16, n=1) for t in range(4)]
        vhp = [P.tmp(f"vh{t}", [128, 4, 4, 128], BF16, n=1) for t in range(4)]
        for t in range(4):
            P.dma(kTp[t], T(kall_v[t][hh], kv_all_t[t].bufs))
            P.dma(vhp[t], T(vall_v[t][4 + hh], kv_all_t[t].bufs))
        if hh == 0:
            P.dma(xres, D_(dr["xo"].rearrange("(c p) t -> p c t", p=128)))
            P.dma(xc, D_(dr["ctx"].rearrange("(c p) t -> p c t", p=128)))

        def ksrc(kt, hh=hh, kTp=kTp):
            if kt >= NK0:
                return kcT[:, hh, (kt - NK0) * 128:(kt - NK0 + 1) * 128]
            r, t, kk = kt // 16, (kt % 16) // 4, kt % 4
            return kTp[t][:, r, kk * 128:(kk + 1) * 128]

        def vsrc(kt, hh=hh, vhp=vhp):
            if kt >= NK0:
                return vc[:, kt - NK0, hh * 128:(hh + 1) * 128]
            r, t, kk = kt // 16, (kt % 16) // 4, kt % 4
            return vhp[t][:, r, kk, :]

        for qt in range(TOK // 512):
            attn_tile(ksrc, vsrc, qT[:, hh, qt * 512:(qt + 1) * 512], 512, KORDER,
                      mixb[:, hh, qt * 512:(qt + 1) * 512])
        attn_tile(ksrc, vsrc, qcT[:, hh, :], CTXL, [NKT - 2, NKT - 1], mixcb[:, hh, :])
    P.held.clear()
    P.ps_i = 0
    P.release(m_att)

    tiles = [(lambda c, t=t: xres[:, c, t * 512:(t + 1) * 512], 512, False) for t in range(TOK // 512)]
    tiles_c = tiles + [(lambda c: xc[:, c, :], CTXL, True)]
    for ti, (xf, W, is_ctx) in enumerate(tiles_c):
        srcs = (mixc, mixcb) if is_ctx else (mix[:, :, ti * 512:(ti + 1) * 512], mixb[:, :, ti * 512:(ti + 1) * 512])
        G = G1c if is_ctx else G1
        for dc in range(8):
            ps = P.ps()
            for kc in range(8):
                P.mm(ps[:, 0:W], w_out[:, kc, dc * 128:(dc + 1) * 128], srcs[kc // 4][:, kc % 4, :], start=(kc == 0), stop=(kc == 7))
            P.stt(xf(dc), ps[:, 0:W], G[:, dc:dc + 1], xf(dc), ALU.mult, ALU.add)
    P.release(m_base)
    moe_layer(C, dr, 0, tiles_c, A2, Sh2, G2, A2c, Sh2c, G2c)

    yg = P.sb_top([128, 8, TOK], BF16, "yg")
    uu = P.sb_top([128, 8, TOK + 6], BF16, "uu")
    ucx = T(nc.alloc_sbuf_tensor_at("ucx_alias", [128, 8, CTXL + 6], BF16, offset=xc_off).ap(), xc.bufs)
    m_l1 = P.mark()
    w1 = P.sb([128, 8, 2048], BF16, "w1in")
    for kc in range(8):
        P.dma(w1[:, kc, :], D_(dr["od_w_in"][kc * 128:(kc + 1) * 128, :]), q="pool")
    def l1_norm(ti):
        xf, W, is_ctx = tiles_c[ti]
        A, Sh = (A1nc, Sh1nc) if is_ctx else (A1n, Sh1n)
        h1 = P.tmp(f"h1_{W}", [128, 8, W], BF16, n=(1 if is_ctx else 2))
        ps = P.ps()
        for c in range(8):
            s_ = P.tmp(f"sq{W}", [128, W], BF16)
            P.act(s_, xf(c), AF.Square)
            P.mm(ps[:, 0:W], C.ones, s_, start=(c == 0), stop=(c == 7))
        rstd = C.rstd_from_ps(ps[:, 0:W], W, 1.0 / D)
        for c in range(8):
            t = P.tmp(f"nt{W}", [128, W], F32)
            P.tt(t, xf(c), rstd, ALU.mult)
            P.act(h1[:, c, :], t, AF.Identity, bias=Sh[:, c:c + 1], scale=A[:, c:c + 1])
        return h1

    def l1_proj(ti, h1):
        xf, W, is_ctx = tiles_c[ti]
        for f in range(16):
            if is_ctx and f < 8:
                continue
            ps = P.ps()
            for kc in range(8):
                P.mm(ps[:, 0:W], w1[:, kc, f * 128:(f + 1) * 128], h1[:, kc, :], start=(kc == 0), stop=(kc == 7))
            if f < 8:
                P.act(yg[:, f, ti * 512:(ti + 1) * 512], ps[:, 0:W], AF.Gelu_apprx_tanh)
            elif is_ctx:
                P.copy(ucx[:, f - 8, 3:3 + CTXL], ps[:, 0:W], eng="act")
            else:
                P.copy(uu[:, f - 8, 3 + ti * 512:3 + (ti + 1) * 512], ps[:, 0:W], eng="act")

    h1n = l1_norm(0)
    for ti in range(len(tiles_c)):
        h1c = h1n
        if ti + 1 < len(tiles_c):
            h1n = l1_norm(ti + 1)
        l1_proj(ti, h1c)
    P.memset(ucx[:, :, 0:3], 0.0)
    P.memset(ucx[:, :, CTXL + 3:CTXL + 6], 0.0)
    P.release(m_l1)

    prm = lru_params(C, dr)
    hs1 = P.sb([128, 8, 2], F32, "hs1")
    P.ts(hs1, prm["s1"], 0.5, None, ALU.mult)
    hba = P.sb([128, 8, 2], F32, "hba")
    P.ts(hba, prm["l_ba"], 0.5, None, ALU.mult)
    hbx = P.sb([128, 8, 2], F32, "hbx")
    P.ts(hbx, prm["l_bx"], 0.5, None, ALU.mult)
    qtr = P.sb([128, 1], F32, "qtr")
    P.memset(qtr, 0.25)
    hx = P.sb([128, 8, 6], F32, "hx")
    P.copy(hx[:, :, 0:3], uu[:, :, 3:6])
    P.copy(hx[:, :, 3:6], uu[:, :, TOK:TOK + 3])
    P.dma(hx_in_t, hx.re("p c k -> p (c k)"))
    allgather(hx_in_t, hx_all_t)
    hxa = P.sb([128, 4, 8, 6], F32, "hxa")
    P.dma(hxa.re("p r c k -> p r (c k)"), T(hx_all.rearrange("(r p) f -> p r f", p=128), hx_all_t.bufs))
    halo = P.sb([128, 8, 6], F32, "halo")
    P.memset(halo, 0.0)
    for j in range(4):
        P.stt(halo[:, :, 0:3], hxa[:, j, :, 3:6], hsel[:, 0, j:j + 1], halo[:, :, 0:3], ALU.mult, ALU.add)
        P.stt(halo[:, :, 3:6], hxa[:, j, :, 0:3], hsel[:, 1, j:j + 1], halo[:, :, 3:6], ALU.mult, ALU.add)
    P.copy(uu[:, :, 0:3], halo[:, :, 0:3])
    P.copy(uu[:, :, TOK + 3:TOK + 6], halo[:, :, 3:6])

    PW = HALF

    cur_dg = [None, None]

    def get_diag(c):
        if cur_dg[0] != c:
            dg = P.tmp("l_dg", [128, 2, 4, 128], BF16, n=2)
            for d_ in range(2):
                for k in range(4):
                    P.ts(dg[:, d_, k, :], C.ident, prm["cw"][:, c, d_, k:k + 1], None, ALU.mult)
            cur_dg[0], cur_dg[1] = c, dg
        return cur_dg[1]

    def stageA1a(win, W, c, d):
        dg = get_diag(c)
        o0 = 0 if d == 0 else 3
        psc = P.ps()
        for k in range(4):
            P.mm(psc[:, 0:W], dg[:, d, k, :], win[:, o0 + k:o0 + k + W], start=(k == 0), stop=(k == 3))
        uc = P.tmp("l_uc", [128, PW], F32, n=4)[:, 0:W]
        P.ts(uc, psc[:, 0:W], prm["l_cb"][:, c, d:d + 1], None, ALU.add)
        ucb = P.tmp("l_ucb", [128, PW], BF16, n=2)[:, 0:W]
        P.copy(ucb, uc, eng="dve")
        return dict(uc=uc, ucb=ucb)

    def stageA1b(st, W, c, d):
        ps = P.ps()
        P.mm(ps[:, 0:W], prm["wa"][:, d, c, :], st["ucb"])
        ps2 = P.ps()
        P.mm(ps2[:, 0:W], prm["wx"][:, d, c, :], st["ucb"])
        st["ps"], st["ps2"] = ps, ps2

    def stageA2(st, W, c, d):
        tr = P.tmp("l_tr", [128, PW], F32, n=2)[:, 0:W]
        ti = P.tmp("l_ti", [128, PW], F32, n=4)[:, 0:W]
        P.act(tr, st["ps"][:, 0:W], AF.Tanh, bias=hba[:, c, d:d + 1], scale=0.5)
        P.act(ti, st["ps2"][:, 0:W], AF.Tanh, bias=hbx[:, c, d:d + 1], scale=0.5)
        st["tr"], st["ti"] = tr, ti

    def stageEX(st, W, c, d):
        a = P.tmp("l_a", [128, PW], F32, n=3)[:, 0:W]
        P.act(a, st["tr"], AF.Exp, bias=hs1[:, c, d:d + 1], scale=hs1[:, c, d:d + 1])
        t = P.tmp("l_t", [128, PW], F32, n=3)[:, 0:W]
        P.act(t, st["tr"], AF.Exp, bias=prm["s1"][:, c, d:d + 1], scale=prm["s1"][:, c, d:d + 1])
        st["a"], st["t"] = a, t

    def stageSQ(st, W):
        P.act(st["t"], st["t"], AF.Sqrt, bias=qtr, scale=-0.25)

    def stageB2(st, W, c, d, init, out, want_A=None):
        uc, ti, a, t = st["uc"], st["ti"], st["a"], st["t"]
        P.stt(ti, ti, 1.0, uc, ALU.add, ALU.mult)
        P.tt(ti, ti, t, ALU.mult)
        if d == 0:
            P.scan(out, a, ti, init)
        else:
            P.scan(out[:, ::-1], a[:, ::-1], ti[:, ::-1], init)
        if want_A is not None:
            P.reduce(want_A, a, ALU.mult)

    def run_units(units):
        n = len(units)
        if n == 0:
            return
        sts = [None] * n

        def A1(i):
            u = units[i]
            sts[i] = stageA1a(u["win"], u["W"], u["c"], u["d"])

        def A1b(i):
            u = units[i]
            stageA1b(sts[i], u["W"], u["c"], u["d"])

        def A2(i):
            u = units[i]
            stageA2(sts[i], u["W"], u["c"], u["d"])

        def EX(i):
            u = units[i]
            stageEX(sts[i], u["W"], u["c"], u["d"])

        def B2(i):
            u = units[i]
            stageB2(sts[i], u["W"], u["c"], u["d"], u["init"](), u["out"], u.get("want_A"))
            if u.get("after"):
                u["after"]()
            sts[i] = None

        A1(0)
        if n > 1:
            A1(1)
        A1b(0)
        if n > 1:
            A1b(1)
        A2(0)
        if n > 1:
            A2(1)
        i = 0
        while i < n:
            two = i + 1 < n
            for j in (i + 2, i + 3):
                if j < n:
                    A1(j)
            for j in (i + 2, i + 3):
                if j < n:
                    A1b(j)
            EX(i)
            if two:
                EX(i + 1)
            stageSQ(sts[i], units[i]["W"])
            if two:
                stageSQ(sts[i + 1], units[i + 1]["W"])
            for j in (i + 2, i + 3):
                if j < n:
                    A2(j)
            B2(i)
            if two:
                B2(i + 1)
            i += 2

    NQ = TOK // HALF
    summ = P.sb([128, 8, 2, 3], F32, "summ")
    apq = P.sb([128, 8, 2, NQ], F32, "apq")
    carry = {}
    units = []
    for c in range(8):
        for d in range(2):
            hcx = P.tmp("l_hx", [128, PW], F32, n=2)[:, 0:CTXL]

            def after_ctx(c=c, d=d, hcx=hcx):
                P.copy(summ[:, c, d, 2:3], hcx[:, CTXL - 1:CTXL] if d == 0 else hcx[:, 0:1])
            units.append(dict(win=ucx[:, c, :], W=CTXL, c=c, d=d, init=lambda: 0.0, out=hcx, after=after_ctx))
            order = list(range(NQ)) if d == 0 else list(range(NQ - 1, -1, -1))
            for n_, k in enumerate(order):
                ho = P.tmp("l_hx", [128, PW], F32, n=2)[:, 0:HALF]
                key = (c, d)

                def init_fn(key=key, n_=n_):
                    return 0.0 if n_ == 0 else carry[key]

                def after_q(key=key, ho=ho, d=d, n_=n_, c=c):
                    if n_ == NQ - 1:
                        P.copy(summ[:, c, d, 1:2], ho[:, HALF - 1:HALF] if d == 0 else ho[:, 0:1])
                    else:
                        cr = P.tmp("l_cr", [128, 1], F32, n=4)
                        P.copy(cr, ho[:, HALF - 1:HALF] if d == 0 else ho[:, 0:1])
                        carry[key] = cr
                units.append(dict(win=uu[:, c, k * HALF:k * HALF + HALF + 6], W=HALF, c=c, d=d, init=init_fn, out=ho,
                                  after=after_q, want_A=apq[:, c, d, k:k + 1]))
    run_units(units)
    P.tt(summ[:, :, :, 0], apq[:, :, :, 0], apq[:, :, :, 1], ALU.mult)
    for k in range(2, NQ):
        P.tt(summ[:, :, :, 0], summ[:, :, :, 0], apq[:, :, :, k], ALU.mult)
    P.dma(sm_in_t, summ.re("p c d k -> p (c d k)"))
    allgather(sm_in_t, sm_all_t)
    sm = P.sb([128, 4, 8, 2, 3], F32, "sm")
    P.dma(sm.re("p r c d k -> p r (c d k)"), T(sm_all.rearrange("(r p) f -> p r f", p=128), sm_all_t.bufs))
    h0 = P.sb([128, 8, 2], F32, "h0")
    cand = P.sb([128, 8], F32, "cand")
    for d in range(2):
        P.copy(h0[:, :, d], summ[:, :, d, 2])
        order = range(4) if d == 0 else range(3, -1, -1)
        for j in order:
            P.tt(cand, sm[:, j, :, d, 0], h0[:, :, d], ALU.mult)
            P.tt(cand, cand, sm[:, j, :, d, 1], ALU.add)
            P.tt(cand, cand, h0[:, :, d], ALU.subtract)
            P.stt(h0[:, :, d], cand, jm[:, d, j:j + 1], h0[:, :, d], ALU.mult, ALU.add)
    hst = [T(None, None)] * NQ
    hfull = [P.sb([128, HALF], F32, f"hfull{k}") for k in range(NQ)]
    units = []
    carry2 = {}
    for c in range(8):
        dirs = [0, 1] if c % 2 == 0 else [1, 0]
        for di, d in enumerate(dirs):
            order = list(range(NQ)) if d == 0 else list(range(NQ - 1, -1, -1))
            for n_, k in enumerate(order):
                out = hfull[k] if di == 0 else P.tmp("l_hx", [128, PW], F32, n=2)[:, 0:HALF]
                key = (c, d)

                def init_fn(key=key, n_=n_, c=c, d=d):
                    return h0[:, c, d:d + 1] if n_ == 0 else carry2[key]

                def after_q(key=key, out=out, d=d, di=di, k=k, c=c):
                    cr = P.tmp("l_cr", [128, 1], F32, n=4)
                    P.copy(cr, out[:, HALF - 1:HALF] if d == 0 else out[:, 0:1])
                    carry2[key] = cr
                    if di == 1:
                        P.tt(out, out, hfull[k], ALU.add)
                        P.tt(yg[:, c, k * HALF:(k + 1) * HALF], out, yg[:, c, k * HALF:(k + 1) * HALF], ALU.mult)
                units.append(dict(win=uu[:, c, k * HALF:k * HALF + HALF + 6], W=HALF, c=c, d=d, init=init_fn, out=out, after=after_q))
    run_units(units)
    P.release(m_base)
    w_out1 = P.sb([128, 8, D], BF16, "w_out1")
    for kc in range(8):
        P.dma(w_out1[:, kc, :], D_(dr["od_w_out"][kc * 128:(kc + 1) * 128, :]), q="pool")
    for ti, (xf, W, _) in enumerate(tiles):
        for dc in range(8):
            ps = P.ps()
            for kc in range(8):
                P.mm(ps[:, 0:W], w_out1[:, kc, dc * 128:(dc + 1) * 128], yg[:, kc, ti * 512:(ti + 1) * 512],
                     start=(kc == 0), stop=(kc == 7))
            P.stt(xf(dc), ps[:, 0:W], G1n[:, dc:dc + 1], xf(dc), ALU.mult, ALU.add)
    P.release(m_base)
    P.top_release(top_x)
    moe_layer(C, dr, 1, tiles, A2n, Sh2n, G2n)
    P.add("sp", lambda e: e.dma_start(out=o_out.rearrange("c p t -> p c t"), in_=xres.ap), reads=[xres], writes=[outb], dma=True)
    LRU_PW[0] = 2048
    P.emit(final_reads=[outb])
    return nc, P


def fused_inputs(r, inp, cf, cb, cosT, sinT, lin):
    b, j = r // 4, r % 4
    d = l1_inputs(r, inp, cf, cb, cosT, sinT)
    del d["ada_w"], d["ada_bT"]
    d["ada_wq"] = np.ascontiguousarray(inp["ada_w"][:, :, 1536 * j:1536 * (j + 1)])
    d["ada_bq"] = np.ascontiguousarray(inp["ada_b"].reshape(2, 48, 128)[:, 12 * j:12 * (j + 1), :].transpose(2, 0, 1))
    jm = np.zeros((128, 2, 4), np.float32)
    hs = np.zeros((128, 2, 4), np.float32)
    for jj in range(4):
        jm[:, 0, jj] = 1.0 if jj < j else 0.0
        jm[:, 1, jj] = 1.0 if jj > j else 0.0
        hs[:, 0, jj] = 1.0 if jj == j - 1 else 0.0
        hs[:, 1, jj] = 1.0 if jj == j + 1 else 0.0
    d.update(dict(
        nffnT=np.stack([chunkT(inp["norm_ffn"][l]) for l in range(2)]),
        lamp=np.ascontiguousarray(np.stack([inp["ev_lam_q1"][0], inp["ev_lam_k1"][0], inp["ev_lam_q2"][0], inp["ev_lam_k2"][0]], axis=-1)),
        subn=np.ascontiguousarray(inp["ev_sub_norm"][0].reshape(128, 1)),
        w_out=inp["ev_w_out"][0], od_w_in=inp["od_w_in"][0], od_w_out=inp["od_w_out"][0], jmask=jm, hsel=hs))
    d.update(lin)
    m0, m1 = moe_inputs(inp, 0), moe_inputs(inp, 1)
    d.update(dict(moe_wr=np.concatenate([m0["moe_wr"], m1["moe_wr"]], 0), moe_rb=np.concatenate([m0["moe_rb"], m1["moe_rb"]], 0),
                  sel=m0["sel"], moe_wg=inp["moe_w_gate"], moe_wu=inp["moe_w_up"], moe_wd=inp["moe_w_down"]))
    return d


def kernel(**inp):
    inp = {k: np.asarray(v) for k, v in inp.items()}
    cf, cb = host_consts()
    cosT, sinT = rope_tables()
    cores = list(range(NCORE))
    lin = lru_inputs(inp)
    ncf, _ = _get("F", build_fused)
    ims = [fused_inputs(r, inp, cf, cb, cosT, sinT, lin) for r in cores]
    res = run_bass_kernel_spmd(ncf, ims, core_ids=cores).results
    out = np.empty((2, SEQ, D), np.float32)
    for r in cores:
        b, j = r // 4, r % 4
        out[b, j * TOK:(j + 1) * TOK, :] = res[r]["o_out"].reshape(D, TOK).T
    return out
```
